# Optimizing a Trainium2 kernel written in Bass

```python
import jax
import jax.numpy as jnp
from jax import lax
import numpy as np

D_MODEL = 1024
BATCH = 8
SEQ = 4096
DEPTH = 2

GRID_W = 64
CTX_LEN = 256

NA_HEADS = 8
NA_HEAD_DIM = 64
NA_WIDTH = NA_HEADS * NA_HEAD_DIM
NA_WIN_ROWS = 8
NA_WIN_COLS = 16
FNET_GROUPS = 4
FNET_GROUP_DIM = 128
FNET_WIDTH = FNET_GROUPS * FNET_GROUP_DIM
RET_HEADS = 4
RET_QK_DIM = 128
RET_V_DIM = 256
RET_QK_WIDTH = RET_HEADS * RET_QK_DIM
RET_V_WIDTH = RET_HEADS * RET_V_DIM
RET_CHUNK = 128
ROPE_BASE = 10000.0

N_BRANCHES = 3
IN_SPLITS = (NA_WIDTH, NA_WIDTH, NA_WIDTH, FNET_WIDTH, RET_QK_WIDTH, RET_QK_WIDTH, RET_V_WIDTH, RET_V_WIDTH, N_BRANCHES * D_MODEL)
IN_WIDTH = sum(IN_SPLITS)

N_EXPERTS = 256
TOP_K = 8
N_GROUPS = 8
TOPK_GROUPS = 4
EXPERTS_PER_GROUP = N_EXPERTS // N_GROUPS
EXPERT_DIM = 256
SHARED_DIM = 256
ROUTED_SCALE = 2.5
MOE_BLOCK = 128

DN_ALPHA = (2.0 * DEPTH) ** 0.25
DN_BETA = (8.0 * DEPTH) ** -0.25
LN_EPS = 1e-6
GN_EPS = 1e-5

kernel_name = 'hybrid_natten_fnet_retention_moe_dit'


def layer_norm(x, w=None, b=None):
    xf = x.astype(jnp.float32)
    mu = jnp.mean(xf, axis=-1, keepdims=True)
    var = jnp.mean(jnp.square(xf - mu), axis=-1, keepdims=True)
    y = (xf - mu) * lax.rsqrt(var + LN_EPS)
    if w is not None:
        y = y * w.astype(jnp.float32) + b.astype(jnp.float32)
    return y.astype(x.dtype)


def modulate(x, shift, scale):
    return layer_norm(x) * (1.0 + scale) + shift


def swiglu(x, w_gate, w_up, w_down):
    return (jax.nn.silu(x @ w_gate) * (x @ w_up)) @ w_down


def split_heads(t, n_heads):
    return t.reshape(t.shape[0], t.shape[1], n_heads, -1)


def axial_rope(t, row, col):
    half = t.shape[-1] // 2
    n_pairs = half // 2
    inv_freq = ROPE_BASE ** (-jnp.arange(n_pairs, dtype=jnp.float32) / n_pairs)

    def rotate(u, pos):
        ang = pos.astype(jnp.float32)[:, None] * inv_freq[None, :]
        cos = jnp.cos(ang)[None, :, None, :].astype(u.dtype)
        sin = jnp.sin(ang)[None, :, None, :].astype(u.dtype)
        u1, u2 = u[..., :n_pairs], u[..., n_pairs:]
        return jnp.concatenate([u1 * cos - u2 * sin, u1 * sin + u2 * cos], axis=-1)

    return jnp.concatenate([rotate(t[..., :half], row), rotate(t[..., half:], col)], axis=-1)


def neighbourhood_attention(q, k, v, k_ctx, v_ctx, rpb, rows):
    B, N, H, Dh = q.shape
    kh = min(NA_WIN_ROWS, rows)
    scale = Dh ** -0.5
    q = q.reshape(B, rows, GRID_W, H, Dh)
    k = k.reshape(B, rows, GRID_W, H, Dh)
    v = v.reshape(B, rows, GRID_W, H, Dh)
    r = jnp.arange(rows)
    r0 = jnp.clip(r - kh // 2, 0, rows - kh)
    key_rows = r0[:, None] + jnp.arange(kh)[None, :]
    k_band = k[:, key_rows]
    v_band = v[:, key_rows]
    cidx = jnp.arange(GRID_W)
    c0 = jnp.clip(cidx - NA_WIN_COLS // 2, 0, GRID_W - NA_WIN_COLS)
    col_ok = (cidx[None, :] >= c0[:, None]) & (cidx[None, :] < c0[:, None] + NA_WIN_COLS)
    dr = key_rows - r[:, None] + (NA_WIN_ROWS - 1)
    dc = jnp.clip(cidx[None, :] - cidx[:, None], -(NA_WIN_COLS - 1), NA_WIN_COLS - 1) + (NA_WIN_COLS - 1)
    bias = rpb[:, dr[:, None, :, None], dc[None, :, None, :]].astype(jnp.float32)
    s_win = jnp.einsum('brqhd,brkwhd->bhrqkw', q, k_band).astype(jnp.float32) * scale + bias[None]
    s_win = jnp.where(col_ok[:, None, :], s_win, -jnp.inf)
    s_ctx = jnp.einsum('brqhd,blhd->bhrql', q, k_ctx).astype(jnp.float32) * scale
    n_win = kh * GRID_W
    s = jnp.concatenate([s_win.reshape(B, H, rows, GRID_W, n_win), s_ctx], axis=-1)
    p = jax.nn.softmax(s, axis=-1).astype(v.dtype)
    p_win = p[..., :n_win].reshape(B, H, rows, GRID_W, kh, GRID_W)
    p_ctx = p[..., n_win:]
    o = jnp.einsum('bhrqkw,brkwhd->brqhd', p_win, v_band) + jnp.einsum('bhrql,blhd->brqhd', p_ctx, v_ctx)
    return o.reshape(B, N, H * Dh)


def context_attention(q, k, v):
    B, L, H, Dh = q.shape
    s = jnp.einsum('blhd,bmhd->bhlm', q, k).astype(jnp.float32) * (Dh ** -0.5)
    p = jax.nn.softmax(s, axis=-1).astype(v.dtype)
    return jnp.einsum('bhlm,bmhd->blhd', p, v).reshape(B, L, H * Dh)


def fourier_mix(u):
    B, N, _ = u.shape
    ug = u.reshape(B, N, FNET_GROUPS, FNET_GROUP_DIM).astype(jnp.float32)
    f = jnp.fft.fft2(ug, axes=(1, 3), norm='ortho').real
    return f.reshape(B, N, FNET_WIDTH).astype(u.dtype)


def retention_chunkwise(q, k, v, log_g, s0, inclusive):
    B, H, N, Dk = q.shape
    Dv = v.shape[-1]
    C = RET_CHUNK
    nc = N // C
    qc = q.astype(jnp.float32).reshape(B, H, nc, C, Dk)
    kc = k.astype(jnp.float32).reshape(B, H, nc, C, Dk)
    vc = v.astype(jnp.float32).reshape(B, H, nc, C, Dv)
    lg = log_g.astype(jnp.float32)[:, None]
    pos = jnp.arange(C, dtype=jnp.float32)
    diff = pos[:, None] - pos[None, :]
    in_band = (diff >= 0) if inclusive else (diff > 0)
    decay_in = jnp.where(in_band, jnp.exp(lg[:, :, None] * jnp.maximum(diff, 0.0)), 0.0)
    scores = jnp.einsum('bhncd,bhnmd->bhncm', qc, kc) * decay_in[None, :, None]
    o_inner = jnp.einsum('bhncm,bhnme->bhnce', scores, vc)
    k_decay = jnp.exp(lg * (C - 1 - pos))
    q_decay = jnp.exp(lg * (pos + 1))
    chunk_decay = jnp.exp(lg[:, 0] * C)[None, :, None, None]
    kv = jnp.einsum('bhnmd,bhnme->nbhde', kc * k_decay[None, :, None, :, None], vc)

    def step(state, kv_chunk):
        return chunk_decay * state + kv_chunk, state

    s_final, s_prev = lax.scan(step, s0.astype(jnp.float32), kv)
    o_cross = jnp.einsum('bhncd,nbhde->bhnce', qc * q_decay[None, :, None, :, None], s_prev)
    return (o_inner + o_cross).reshape(B, H, N, Dv), s_final


def bidirectional_retention(q, k, v, log_g_fwd, log_g_bwd, s0_fwd, s0_bwd):
    o_f, s_f = retention_chunkwise(q, k, v, log_g_fwd, s0_fwd, True)
    o_b, s_b = retention_chunkwise(q[:, :, ::-1], k[:, :, ::-1], v[:, :, ::-1], log_g_bwd, s0_bwd, False)
    return o_f + o_b[:, :, ::-1], s_f, s_b


def retention_output(o, gate, gn_w):
    B, H, N, Dv = o.shape
    mu = jnp.mean(o, axis=-1, keepdims=True)
    var = jnp.mean(jnp.square(o - mu), axis=-1, keepdims=True)
    y = ((o - mu) * lax.rsqrt(var + GN_EPS)).transpose(0, 2, 1, 3).reshape(B, N, H * Dv)
    y = y * gn_w.astype(jnp.float32)
    return y.astype(gate.dtype) * jax.nn.silu(gate)


def merge_branches(gate_logits, y_na, y_fn, y_ret, w_o_na, w_fourier, w_o_ret, w_out):
    gates = jax.nn.sigmoid(gate_logits.reshape(gate_logits.shape[0], gate_logits.shape[1], N_BRANCHES, D_MODEL))
    y = (gates[..., 0, :] * (y_na @ w_o_na) + gates[..., 1, :] * (y_fn @ w_fourier)
         + gates[..., 2, :] * (y_ret @ w_o_ret))
    return y @ w_out


def token_mixer(h, h_ctx, w_in, na_rpb, w_o_na, w_fourier, log_g_fwd, log_g_bwd, ret_gn_w, w_o_ret, w_out, update_ctx):
    B, N, _ = h.shape
    rows = N // GRID_W
    split_at = [int(s) for s in np.cumsum(IN_SPLITS)[:-1]]
    qa, ka, va, ub, qr, kr, vr, gr, gl = jnp.split(h @ w_in, split_at, axis=-1)
    qa_c, ka_c, va_c, ub_c, qr_c, kr_c, vr_c, gr_c, gl_c = jnp.split(h_ctx @ w_in, split_at, axis=-1)

    y_na = neighbourhood_attention(split_heads(qa, NA_HEADS), split_heads(ka, NA_HEADS), split_heads(va, NA_HEADS),
                                   split_heads(ka_c, NA_HEADS), split_heads(va_c, NA_HEADS), na_rpb, rows)
    y_fn = fourier_mix(ub)
    t = jnp.arange(N)
    row, col = t // GRID_W, t % GRID_W
    q_scale = RET_QK_DIM ** -0.5

    def ret_heads(qx, kx, vx, with_rope):
        qx, kx, vx = split_heads(qx, RET_HEADS), split_heads(kx, RET_HEADS), split_heads(vx, RET_HEADS)
        if with_rope:
            qx, kx = axial_rope(qx, row, col), axial_rope(kx, row, col)
        return (qx * q_scale).transpose(0, 2, 1, 3), kx.transpose(0, 2, 1, 3), vx.transpose(0, 2, 1, 3)

    zero_state = jnp.zeros((B, RET_HEADS, RET_QK_DIM, RET_V_DIM), jnp.float32)
    qc_h, kc_h, vc_h = ret_heads(qr_c, kr_c, vr_c, False)
    o_ret_c, s_fwd, s_bwd = bidirectional_retention(qc_h, kc_h, vc_h, log_g_fwd, log_g_bwd, zero_state, zero_state)
    q_h, k_h, v_h = ret_heads(qr, kr, vr, True)
    o_ret, _, _ = bidirectional_retention(q_h, k_h, v_h, log_g_fwd, log_g_bwd, s_fwd, s_bwd)
    y_ret = retention_output(o_ret, gr, ret_gn_w)
    y = merge_branches(gl, y_na, y_fn, y_ret, w_o_na, w_fourier, w_o_ret, w_out)
    if not update_ctx:
        return y, None
    y_na_c = context_attention(split_heads(qa_c, NA_HEADS), split_heads(ka_c, NA_HEADS), split_heads(va_c, NA_HEADS))
    y_fn_c = fourier_mix(ub_c)
    y_ret_c = retention_output(o_ret_c, gr_c, ret_gn_w)
    y_c = merge_branches(gl_c, y_na_c, y_fn_c, y_ret_c, w_o_na, w_fourier, w_o_ret, w_out)
    return y, y_c


def routed_experts(h, top_e, top_w, w_gate, w_up, w_down):
    T, D = h.shape
    A = T * TOP_K
    n_slots = -(-(A + N_EXPERTS * (MOE_BLOCK - 1)) // MOE_BLOCK) * MOE_BLOCK
    n_blocks = n_slots // MOE_BLOCK
    e_flat = top_e.reshape(A)
    order = jnp.argsort(e_flat)
    e_sorted = e_flat[order]
    tok_sorted = (order // TOP_K).astype(jnp.int32)
    w_sorted = top_w.reshape(A)[order]
    counts = jnp.bincount(e_flat, length=N_EXPERTS)
    padded = (counts + MOE_BLOCK - 1) // MOE_BLOCK * MOE_BLOCK
    pad_end = jnp.cumsum(padded)
    pad_start = pad_end - padded
    seg_start = jnp.cumsum(counts) - counts
    dest = pad_start[e_sorted] + (jnp.arange(A) - seg_start[e_sorted])
    slot_tok = jnp.full((n_slots,), T, jnp.int32).at[dest].set(tok_sorted)
    slot_w = jnp.zeros((n_slots,), h.dtype).at[dest].set(w_sorted)
    block_e = jnp.minimum(jnp.searchsorted(pad_end, jnp.arange(n_blocks) * MOE_BLOCK, side='right'), N_EXPERTS - 1)
    h_pad = jnp.concatenate([h, jnp.zeros((1, D), h.dtype)], axis=0)

    def expert_block(args):
        tok, wt, e = args
        return swiglu(h_pad[tok], w_gate[e], w_up[e], w_down[e]) * wt[:, None]

    y = lax.map(expert_block, (slot_tok.reshape(n_blocks, MOE_BLOCK), slot_w.reshape(n_blocks, MOE_BLOCK), block_e))
    return jax.ops.segment_sum(y.reshape(n_slots, D), slot_tok, num_segments=T + 1)[:T]


def moe_ffn(h, router_w, router_bias, exp_w_gate, exp_w_up, exp_w_down, sh_w_gate, sh_w_up, sh_w_down):
    T = h.shape[0]
    scores = jax.nn.sigmoid((h @ router_w).astype(jnp.float32))
    sel = scores + router_bias.astype(jnp.float32)
    grp_score = lax.top_k(sel.reshape(T, N_GROUPS, EXPERTS_PER_GROUP), 2)[0].sum(-1)
    _, top_g = lax.top_k(grp_score, TOPK_GROUPS)
    g_mask = jnp.any(top_g[..., None] == jnp.arange(N_GROUPS), axis=-2)
    sel = jnp.where(jnp.repeat(g_mask, EXPERTS_PER_GROUP, axis=-1), sel, -jnp.inf)
    _, top_e = lax.top_k(sel, TOP_K)
    w = jnp.take_along_axis(scores, top_e, axis=-1)
    w = w / jnp.sum(w, axis=-1, keepdims=True) * ROUTED_SCALE
    routed = routed_experts(h, top_e, w.astype(h.dtype), exp_w_gate, exp_w_up, exp_w_down)
    return routed + swiglu(h, sh_w_gate, sh_w_up, sh_w_down)


def setup_inputs(seed: int = 0) -> dict:
    key = jax.random.key(seed)
    ks = jax.random.split(key, 27)
    f32 = jnp.float32
    D = D_MODEL

    def nrm(k, shape, std):
        return jax.random.normal(k, shape, f32) * std

    base = 1.0 - 2.0 ** (-5.0 - np.arange(RET_HEADS))
    decay_logit = jnp.asarray(np.log(base / (1.0 - base)), f32)
    return {
        'x': nrm(ks[0], (BATCH, SEQ, D), 1.0),
        'c': nrm(ks[1], (BATCH, D), 1.0),
        'ctx': nrm(ks[2], (BATCH, CTX_LEN, D), 1.0),
        'c_ctx': nrm(ks[3], (D,), 1.0),
        'ada_w': nrm(ks[4], (DEPTH, D, 6 * D), 0.5 * D ** -0.5),
        'ada_b': nrm(ks[5], (DEPTH, 6 * D), 0.02),
        'w_in': nrm(ks[6], (DEPTH, D, IN_WIDTH), D ** -0.5),
        'na_rpb': nrm(ks[7], (DEPTH, NA_HEADS, 2 * NA_WIN_ROWS - 1, 2 * NA_WIN_COLS - 1), 0.1),
        'w_o_na': nrm(ks[8], (DEPTH, NA_WIDTH, D), DN_BETA * NA_WIDTH ** -0.5),
        'w_fourier': nrm(ks[9], (DEPTH, FNET_WIDTH, D), DN_BETA * FNET_WIDTH ** -0.5),
        'ret_decay_fwd': decay_logit + nrm(ks[10], (DEPTH, RET_HEADS), 0.01),
        'ret_decay_bwd': decay_logit + nrm(ks[11], (DEPTH, RET_HEADS), 0.01),
        'ret_gn_w': 1.0 + nrm(ks[12], (DEPTH, RET_V_WIDTH), 0.02),
        'w_o_ret': nrm(ks[13], (DEPTH, RET_V_WIDTH, D), DN_BETA * RET_V_WIDTH ** -0.5),
        'w_out': nrm(ks[14], (DEPTH, D, D), DN_BETA * D ** -0.5),
        'ln_mix_w': 1.0 + nrm(ks[15], (DEPTH, D), 0.02),
        'ln_mix_b': nrm(ks[16], (DEPTH, D), 0.02),
        'router_w': nrm(ks[17], (DEPTH, D, N_EXPERTS), D ** -0.5),
        'router_bias': nrm(ks[18], (DEPTH, N_EXPERTS), 0.01),
        'exp_w_gate': nrm(ks[19], (DEPTH, N_EXPERTS, D, EXPERT_DIM), D ** -0.5),
        'exp_w_up': nrm(ks[20], (DEPTH, N_EXPERTS, D, EXPERT_DIM), D ** -0.5),
        'exp_w_down': nrm(ks[21], (DEPTH, N_EXPERTS, EXPERT_DIM, D), DN_BETA * EXPERT_DIM ** -0.5),
        'sh_w_gate': nrm(ks[22], (DEPTH, D, SHARED_DIM), D ** -0.5),
        'sh_w_up': nrm(ks[23], (DEPTH, D, SHARED_DIM), D ** -0.5),
        'sh_w_down': nrm(ks[24], (DEPTH, SHARED_DIM, D), DN_BETA * SHARED_DIM ** -0.5),
        'ln_ffn_w': 1.0 + nrm(ks[25], (DEPTH, D), 0.02),
        'ln_ffn_b': nrm(ks[26], (DEPTH, D), 0.02),
    }


def reference(x, c, ctx, c_ctx, ada_w, ada_b, w_in, na_rpb, w_o_na, w_fourier, ret_decay_fwd, ret_decay_bwd,
              ret_gn_w, w_o_ret, w_out, ln_mix_w, ln_mix_b, router_w, router_bias, exp_w_gate, exp_w_up,
              exp_w_down, sh_w_gate, sh_w_up, sh_w_down, ln_ffn_w, ln_ffn_b):
    B, N, D = x.shape
    L = ctx.shape[1]
    xc = ctx
    for l in range(DEPTH):
        update_ctx = l < DEPTH - 1
        mod = jax.nn.silu(c) @ ada_w[l] + ada_b[l]
        mod_c = jax.nn.silu(c_ctx) @ ada_w[l] + ada_b[l]
        sh1, sc1, g1, sh2, sc2, g2 = jnp.split(mod[:, None, :], 6, axis=-1)
        sh1_c, sc1_c, g1_c, sh2_c, sc2_c, g2_c = jnp.split(mod_c, 6, axis=-1)
        log_g_fwd = jax.nn.log_sigmoid(ret_decay_fwd[l].astype(jnp.float32))
        log_g_bwd = jax.nn.log_sigmoid(ret_decay_bwd[l].astype(jnp.float32))
        y, y_c = token_mixer(modulate(x, sh1, sc1), modulate(xc, sh1_c, sc1_c), w_in[l], na_rpb[l], w_o_na[l],
                             w_fourier[l], log_g_fwd, log_g_bwd, ret_gn_w[l], w_o_ret[l], w_out[l], update_ctx)
        x = layer_norm(DN_ALPHA * x + g1 * y, ln_mix_w[l], ln_mix_b[l])
        h = modulate(x, sh2, sc2).reshape(B * N, D)
        moe_args = (router_w[l], router_bias[l], exp_w_gate[l], exp_w_up[l], exp_w_down[l],
                    sh_w_gate[l], sh_w_up[l], sh_w_down[l])
        if update_ctx:
            xc = layer_norm(DN_ALPHA * xc + g1_c * y_c, ln_mix_w[l], ln_mix_b[l])
            h_c = modulate(xc, sh2_c, sc2_c).reshape(B * L, D)
            f = moe_ffn(jnp.concatenate([h, h_c], axis=0), *moe_args)
            f_lat = f[:B * N].reshape(B, N, D)
            xc = layer_norm(DN_ALPHA * xc + g2_c * f[B * N:].reshape(B, L, D), ln_ffn_w[l], ln_ffn_b[l])
        else:
            f_lat = moe_ffn(h, *moe_args).reshape(B, N, D)
        x = layer_norm(DN_ALPHA * x + g2 * f_lat, ln_ffn_w[l], ln_ffn_b[l])
    return x
```

```python
import numpy as np
import concourse.bass as bass
import concourse.mybir as mybir
from concourse.bass_utils import run_bass_kernel_spmd

F32 = mybir.dt.float32
BF16 = mybir.dt.bfloat16
I32 = mybir.dt.int32
AF = mybir.ActivationFunctionType
ALU = mybir.AluOpType
AX = mybir.AxisListType

D = 1024
SEQ = 4096
CTX = 256
NT = SEQ + CTX
NTILE = NT // 128
DEPTH = 2
INW = 8192
LN_EPS = 1e-6
DN_ALPHA = (2.0 * DEPTH) ** 0.25


class P:
    def __init__(self, nc):
        self.nc = nc
        self.ops = {k: [] for k in ("pe", "act", "dve", "pool", "sp")}
        self.cnt = {k: 0 for k in self.ops}
        self.sem = {}
        self.waited = {k: {} for k in self.ops}
        self.last_w = {}
        self.readers = {}
        self.dma_sems = {"sp": [], "pool": []}
        self.dma_rr = {"sp": 0, "pool": 0}
        self.dma_val = {}
        self.dma_last = {}
        self.final_tokens = []
        self.pending = {k: [] for k in self.ops}

    def setup_sems(self, stack):
        for k in self.ops:
            self.sem[k] = stack.enter_context(self.nc.semaphore("e_" + k))
        for q in ("sp", "pool"):
            for i in range(12):
                s = stack.enter_context(self.nc.semaphore(f"d_{q}{i}"))
                self.dma_sems[q].append(s)
                self.dma_val[id(s)] = 0
                self.dma_last[id(s)] = None

    def _deps(self, eng, reads, writes):
        toks = []
        for r in reads:
            toks.extend(self._lw(r))
        for w in writes:
            toks.extend(self._lw(w))
            toks.extend(self.readers.get(w, ()))
        need = {}
        for (s, v, own) in toks:
            if own == eng and eng == "pe":
                continue
            key = id(s)
            if self.waited[eng].get(key, 0) >= v:
                continue
            if key not in need or need[key][1] < v:
                need[key] = (s, v)
        for key, (s, v) in need.items():
            self.waited[eng][key] = v
        return list(need.values())

    def _lw(self, key):
        t = self.last_w.get(key)
        if t is None:
            return []
        return t if isinstance(t, list) else [t]

    def _commit(self, tok, reads, writes):
        is_dma = tok[2].startswith("dma_")
        for w in writes:
            prev = self._lw(w)
            if is_dma and prev and all(t[2].startswith("dma_") for t in prev) and not self.readers.get(w):
                self.last_w[w] = prev + [tok]
            else:
                self.last_w[w] = [tok]
            self.readers[w] = []
        for r in reads:
            self.readers.setdefault(r, []).append(tok)

    def barrier(self):
        for eng in self.ops:
            w = []
            for k in self.ops:
                if k != eng and self.cnt[k] > 0 and self.waited[eng].get(id(self.sem[k]), 0) < self.cnt[k]:
                    w.append((self.sem[k], self.cnt[k]))
                    self.waited[eng][id(self.sem[k])] = self.cnt[k]
            for q in ("sp", "pool"):
                for ds in self.dma_sems[q]:
                    v = self.dma_val[id(ds)]
                    if v > 0 and self.waited[eng].get(id(ds), 0) < v:
                        w.append((ds, v))
                        self.waited[eng][id(ds)] = v
            self.pending[eng].extend(w)
        self.last_w = {}
        self.readers = {}

    def op(self, eng, fn, reads=(), writes=()):
        waits = self.pending[eng] + self._deps(eng, reads, writes)
        self.pending[eng] = []
        self.cnt[eng] += 1
        v = self.cnt[eng]
        s = self.sem[eng]
        self.ops[eng].append((waits, fn, s, 1))
        tok = (s, v, eng)
        self._commit(tok, reads, writes)
        return tok

    def dma(self, q, fn, reads=(), writes=(), inc=16):
        waits = self.pending[q] + self._deps(q, reads, writes)
        self.pending[q] = []
        i = self.dma_rr[q]
        self.dma_rr[q] = (i + 1) % len(self.dma_sems[q])
        s = self.dma_sems[q][i]
        prev = self.dma_val[id(s)]
        if prev > 0 and self.waited[q].get(id(s), 0) < prev:
            waits.append((s, prev))
            self.waited[q][id(s)] = prev
        self.dma_val[id(s)] = prev + inc
        v = prev + inc
        self.ops[q].append((waits, fn, s, inc))
        tok = (s, v, "dma_" + q)
        self._commit(tok, reads, writes)
        return tok

    def emit(self, block):
        def run(eng_name):
            def body(e):
                for waits, fn, s, inc in self.ops[eng_name]:
                    for (ws, wv) in waits:
                        e.wait_ge(ws, wv)
                    fn(e).then_inc(s, inc)
                if eng_name == "sp":
                    for q in ("sp", "pool"):
                        for ds in self.dma_sems[q]:
                            v = self.dma_val[id(ds)]
                            if v > 0:
                                e.wait_ge(ds, v)
                    for k in self.ops:
                        if k != "sp" and self.cnt[k] > 0:
                            e.wait_ge(self.sem[k], self.cnt[k])
            return body
        block.tensor(run("pe"))
        block.scalar(run("act"))
        block.vector(run("dve"))
        block.gpsimd(run("pool"))
        block.sync(run("sp"))


def build_program(cfg):
    from contextlib import ExitStack
    nc = bass.Bass("TRN2", target_bir_lowering=False)
    p = P(nc)
    stages = cfg.get("stages", 99)
    dbg = cfg.get("debug", ())

    def din(name, shape, dt=F32):
        return nc.dram_tensor(name, list(shape), dt, kind="ExternalInput")

    x_in = din("x", [SEQ, D])
    ctx_in = din("ctx", [CTX, D])
    cvec = din("cvec", [2, D])
    WD = cfg.get("wdepth", DEPTH)
    ada_w = din("ada_w", [WD, D, 6 * D])
    ada_b = din("ada_b", [WD, 6 * D])
    w_in = din("w_in", [WD, D, INW])
    ret_decay = din("ret_decay", [WD, 8])
    ret_gn_w = din("ret_gn_w", [WD, 1024])
    rope_t = din("rope", [SEQ, 256])
    na_bias = din("na_bias", [WD, 8, 128, 3200])
    na_mask = din("na_mask", [128, 3200])
    w_o_na = din("w_o_na", [WD, 512, D])
    w_fourier = din("w_fourier", [WD, 512, D])
    w_o_ret = din("w_o_ret", [WD, 1024, D])
    w_out = din("w_out", [WD, D, D])
    ln_mix_w = din("ln_mix_w", [WD, D])
    ln_mix_b = din("ln_mix_b", [WD, D])
    router_w = din("router_w", [WD, D, 256])
    router_bias = din("router_bias", [WD, 256])
    exp_w_gate = [din(f"exp_w_gate{i}", [256 * 128, 2048]) for i in range(WD)]
    exp_w_up = [din(f"exp_w_up{i}", [256 * 128, 2048]) for i in range(WD)]
    exp_w_down = [din(f"exp_w_down{i}", [256 * 128, 2048]) for i in range(WD)]
    sh_w_gate = din("sh_w_gate", [WD, D, 256])
    sh_w_up = din("sh_w_up", [WD, D, 256])
    sh_w_down = din("sh_w_down", [WD, 256, D])
    ln_ffn_w = din("ln_ffn_w", [WD, D])
    ln_ffn_b = din("ln_ffn_b", [WD, D])
    out_t = nc.dram_tensor("out", [SEQ, D], F32, kind="ExternalOutput")

    def scratch(name, shape, dt=BF16):
        kind = "ExternalOutput" if name in dbg else "Internal"
        return nc.dram_tensor(name, list(shape), dt, kind=kind)

    QAT = scratch("QAT", [512, NT])
    KAT = scratch("KAT", [512, NT])
    UBT = scratch("UBT", [512, NT])
    VA = scratch("VA", [NT, 512])
    QR = scratch("QR", [NT, 512])
    KR = scratch("KR", [NT, 512])
    VR = scratch("VR", [NT, 1024])
    GR = scratch("GR", [NT, 1024])
    GL = scratch("GL", [NT, 3072])
    MODS = scratch("MODS", [2, 6 * D], F32)
    XRES = scratch("XRES", [NT, D], F32)

    with ExitStack() as st:
        p.setup_sems(st)

        def sb(name, shape, dt):
            return st.enter_context(nc.sbuf_tensor(name, list(shape), dt))

        def ps(name, shape, dt=F32):
            return st.enter_context(nc.psum_tensor(name, list(shape), dt))


        ident = sb("ident", [128, 128], BF16)
        ones_f = sb("ones_f", [128, 128], F32)
        mods = sb("mods", [128, 2, 6 * D], F32)
        ARENA_N = 79 * 1024
        arena = sb("arena", [128, ARENA_N], BF16)
        pbig = [ps(f"pbig{i}", [128, 1024], F32) for i in range(2)]
        pb45 = [ps(f"pb{i}", [128, 512], F32) for i in (4, 5)]
        pbank = [pbig[0][:, 0:512], pbig[0][:, 512:1024], pbig[1][:, 0:512], pbig[1][:, 512:1024], pb45[0][:, :], pb45[1][:, :]]
        ptr = [ps(f"ptr{i}", [128, 1024], BF16) for i in range(2)]
        aoff = [0]

        def areset():
            p.barrier()
            aoff[0] = 0

        def alloc(shape, dt, parts=128):
            n = 1
            for d_ in shape:
                n *= d_
            n16 = n * (2 if dt in (F32, I32) else 1)
            assert aoff[0] + n16 <= ARENA_N, (aoff[0], n16)
            v = arena[0:parts, aoff[0]:aoff[0] + n16]
            aoff[0] += (n16 + 31) // 32 * 32
            if dt != BF16:
                v = v.bitcast(dt)
            if len(shape) == 2:
                v = v.rearrange("q (a b) -> q a b", a=shape[0])
            elif len(shape) == 3:
                v = v.rearrange("q (a b c) -> q a b c", a=shape[0], b=shape[1])
            return v

        p.op("pool", lambda e: e.memset(ones_f[:], 1.0), writes=["ones_f"])
        p.op("pool", lambda e: e.memset(ident[:], 1.0), writes=["ident"])
        p.op("pool", lambda e: e.affine_select(out=ident[:], in_=ident[:], pattern=[[-1, 128]],
                                              compare_op=ALU.is_equal, fill=0.0, base=0,
                                              channel_multiplier=1),
             reads=["ident"], writes=["ident"])

        ev = [0]

        def copy_any(out_ap, in_ap, reads, writes):
            ev[0] += 1
            if ev[0] % 2 == 0:
                p.op("act", lambda e: e.copy(out=out_ap, in_=in_ap), reads=reads, writes=writes)
            else:
                p.op("dve", lambda e: e.tensor_copy(out=out_ap, in_=in_ap), reads=reads, writes=writes)

        bank_rr = [0]

        def next_bank():
            bank_rr[0] = (bank_rr[0] + 1) % 6
            i = bank_rr[0]
            return pbank[i], f"pb{i}"

        def phase0(l):
            areset()
            csb = alloc([2, 8], F32)
            crep = alloc([2, 8, 128], F32)
            adaw = [alloc([8, 512], F32) for _ in range(2)]
            bia = [alloc([512], F32, parts=1) for _ in range(2)]
            for j in range(2):
                p.dma("sp", lambda e, j=j: e.dma_start(out=csb[:, j, :], in_=cvec.ap()[j, :].rearrange("(c q) -> q c", q=128),
                                                       allow_slow_non_contiguous=True), writes=["csb"])
            p.op("act", lambda e: e.activation(out=csb, in_=csb, func=AF.Silu), reads=["csb"], writes=["csb"])
            for j in range(2):
                for k in range(8):
                    p.op("dve", lambda e, j=j, k=k: e.tensor_scalar_mul(
                        out=crep[:, j, k, :], in0=ones_f[:], scalar1=csb[:, j, k:k + 1]),
                        reads=["csb", "ones_f"], writes=["crep"])
            for nb in range(12):
                wb = adaw[nb % 2]
                bb = bia[nb % 2]
                p.dma("sp", lambda e, nb=nb, wb=wb: e.dma_start(
                    out=wb, in_=ada_w.ap()[l, :, nb * 512:(nb + 1) * 512].rearrange("(c q) n -> q c n", q=128)),
                    writes=[f"adaw{nb % 2}"])
                p.dma("sp", lambda e, nb=nb, bb=bb: e.dma_start(
                    out=bb, in_=ada_b.ap()[l:l + 1, nb * 512:(nb + 1) * 512]), writes=[f"bia{nb % 2}"])
                for j in range(2):
                    bank, bkey = next_bank()
                    for k in range(8):
                        p.op("pe", lambda e, j=j, k=k, wb=wb, bank=bank: e.matmul(
                            bank, lhsT=crep[:, j, k, :], rhs=wb[:, k, :], start=(k == 0), stop=False),
                            reads=["crep", f"adaw{nb % 2}"], writes=[bkey])
                    p.op("pe", lambda e, bb=bb, bank=bank: e.matmul(
                        bank, lhsT=ones_f[0:1, :], rhs=bb, start=False, stop=True),
                        reads=["ones_f", f"bia{nb % 2}"], writes=[bkey])
                    is_scale = (nb // 2) in (1, 4)
                    if is_scale:
                        p.op("dve", lambda e, j=j, nb=nb, bank=bank: e.tensor_scalar_add(
                            out=mods[:, j, nb * 512:(nb + 1) * 512], in0=bank, scalar1=1.0),
                            reads=[bkey], writes=["mods"])
                    else:
                        p.op("dve", lambda e, j=j, nb=nb, bank=bank: e.tensor_copy(
                            out=mods[:, j, nb * 512:(nb + 1) * 512], in_=bank),
                            reads=[bkey], writes=["mods"])
            if "MODS" in dbg:
                p.dma("sp", lambda e: e.dma_start(out=MODS.ap(), in_=mods[0:1, :, :]), reads=["mods"], writes=["MODS"])

        def ln_tile(xt_ap, stats_ap, mv_ap, rstd_ap, kx, ks):
            for c in range(2):
                p.op("dve", lambda e, c=c: e.bn_stats(out=stats_ap[:, c, :], in_=xt_ap[:, c * 512:(c + 1) * 512]),
                     reads=[kx], writes=[ks])
            p.op("dve", lambda e: e.bn_aggr(out=mv_ap, in_=stats_ap), reads=[ks], writes=[ks + "mv"])
            p.op("dve", lambda e: e.tensor_scalar_add(out=rstd_ap, in0=mv_ap[:, 1:2], scalar1=LN_EPS),
                 reads=[ks + "mv"], writes=[ks + "r"])
            p.op("act", lambda e: e.sqrt(out=rstd_ap, in_=rstd_ap), reads=[ks + "r"], writes=[ks + "r"])
            p.op("dve", lambda e: e.reciprocal(out=rstd_ap, in_=rstd_ap), reads=[ks + "r"], writes=[ks + "r"])
            p.op("dve", lambda e: e.tensor_scalar(out=xt_ap, in0=xt_ap, scalar1=mv_ap[:, 0:1],
                                                  scalar2=rstd_ap[:, 0:1], op0=ALU.subtract, op1=ALU.mult),
                 reads=[kx, ks + "mv", ks + "r"], writes=[kx])

        def phase1(l, xres):
            areset()
            hT = alloc([8, NT], BF16)
            wbuf = [alloc([8, 1024], BF16) for _ in range(2)]
            xt = [alloc([1024], F32) for _ in range(2)]
            xn = [alloc([1024], BF16) for _ in range(2)]
            stats = [alloc([2, 6], F32) for _ in range(2)]
            mv = [alloc([2], F32) for _ in range(2)]
            rstd = [alloc([1], F32) for _ in range(2)]
            stg = [alloc([1024], BF16) for _ in range(4)]

            def load_w(g):
                wb = wbuf[g % 2]
                p.dma("pool", lambda e: e.dma_start(
                    out=wb, in_=w_in.ap()[l, :, g * 1024:(g + 1) * 1024].rearrange("(c q) n -> q c n", q=128)),
                    writes=[f"wbuf{g % 2}"])

            if stages >= 2:
                load_w(0)
            for t in range(NTILE):
                b = t % 2
                j = 0 if t < 32 else 1
                src = xres[t * 128:(t + 1) * 128, :]
                p.dma("sp", lambda e, b=b, src=src: e.dma_start(out=xt[b], in_=src), reads=["xres"], writes=[f"xt{b}"])
                if not cfg.get("noln"):
                    ln_tile(xt[b], stats[b], mv[b], rstd[b], f"xt{b}", f"st{b}")
                p.op("dve", lambda e, b=b, j=j: e.tensor_tensor(out=xt[b], in0=xt[b], in1=mods[:, j, D:2 * D], op=ALU.mult),
                     reads=[f"xt{b}", "mods"], writes=[f"xt{b}"])
                p.op("dve", lambda e, b=b, j=j: e.tensor_tensor(out=xn[b], in0=xt[b], in1=mods[:, j, 0:D], op=ALU.add),
                     reads=[f"xt{b}", "mods"], writes=[f"xn{b}"])
                bv = ptr[b]
                if cfg.get("notr"):
                    continue
                for k in range(8):
                    p.op("pe", lambda e, k=k, b=b, bv=bv: e.transpose(
                        bv[:, k * 128:(k + 1) * 128], xn[b][:, k * 128:(k + 1) * 128], ident[:]),
                        reads=[f"xn{b}", "ident"], writes=[f"ptr{b}"])
                if cfg.get("nocp"):
                    continue
                copy_any(hT[:, :, t * 128:(t + 1) * 128], bv[:, :].rearrange("q (k n) -> q k n", k=8),
                         [f"ptr{b}"], [f"hT{t}a", f"hT{t}b"])

            stg_rr = [0]

            def next_stg():
                stg_rr[0] = (stg_rr[0] + 1) % 4
                return stg[stg_rr[0]], f"stg{stg_rr[0]}"

            def gemm_tm(g, col0, ncols, dst, dst_col0):
                wb = wbuf[g % 2]
                for t in range(NTILE):
                    sg, sk = next_stg()
                    for cb in range(ncols // 512):
                        bank, bkey = next_bank()
                        for k in range(8):
                            p.op("pe", lambda e, k=k, cb=cb, bank=bank, t=t: e.matmul(
                                bank, lhsT=hT[:, k, t * 128:(t + 1) * 128],
                                rhs=wb[:, k, col0 + cb * 512:col0 + (cb + 1) * 512], start=(k == 0), stop=(k == 7)),
                                reads=[f"hT{t}a", f"hT{t}b", f"wbuf{g % 2}"], writes=[bkey])
                        copy_any(sg[:, cb * 512:(cb + 1) * 512], bank, [bkey], [sk])
                    p.dma("sp", lambda e, sg=sg, t=t: e.dma_start(
                        out=dst.ap()[t * 128:(t + 1) * 128, dst_col0:dst_col0 + ncols], in_=sg[:, 0:ncols]),
                        reads=[sk], writes=[dst.name])

            def gemm_fm(g, col0, ncols, dst):
                wb = wbuf[g % 2]
                for fb in range(ncols // 128):
                    for tg in range(0, NT, 1024):
                        ntok = min(1024, NT - tg)
                        sg, sk = next_stg()
                        for tb in range(0, ntok, 512):
                            nn = min(512, ntok - tb)
                            bank, bkey = next_bank()
                            rk = []
                            for tt in range((tg + tb) // 128, (tg + tb + nn) // 128):
                                rk += [f"hT{tt}a", f"hT{tt}b"]
                            for k in range(8):
                                p.op("pe", lambda e, k=k, bank=bank, tb=tb, nn=nn, tg=tg, fb=fb: e.matmul(
                                    bank[:, 0:nn], lhsT=wb[:, k, col0 + fb * 128:col0 + (fb + 1) * 128],
                                    rhs=hT[:, k, tg + tb:tg + tb + nn], start=(k == 0), stop=(k == 7)),
                                    reads=rk + [f"wbuf{g % 2}"], writes=[bkey])
                            copy_any(sg[:, tb:tb + nn], bank[:, 0:nn], [bkey], [sk])
                        p.dma("sp", lambda e, sg=sg, tg=tg, ntok=ntok, fb=fb: e.dma_start(
                            out=dst.ap()[fb * 128:(fb + 1) * 128, tg:tg + ntok], in_=sg[:, 0:ntok]),
                            reads=[sk], writes=[dst.name])

            if stages < 2:
                return
            plan = [
                [("fm", 0, 512, QAT, 0), ("fm", 512, 512, KAT, 0)],
                [("tm", 0, 512, VA, 0), ("fm", 512, 512, UBT, 0)],
                [("tm", 0, 512, QR, 0), ("tm", 512, 512, KR, 0)],
                [("tm", 0, 1024, VR, 0)],
                [("tm", 0, 1024, GR, 0)],
                [("tm", 0, 1024, GL, 0)],
                [("tm", 0, 1024, GL, 1024)],
                [("tm", 0, 1024, GL, 2048)],
            ]
            for g in range(8):
                if g + 1 < min(8, cfg.get("ngroups", 8)):
                    load_w(g + 1)
                if g >= cfg.get("ngroups", 8):
                    break
                for (mode, c0, ncol, dst, dc0) in plan[g]:
                    if mode == "tm":
                        gemm_tm(g, c0, ncol, dst, dc0)
                    else:
                        gemm_fm(g, c0, ncol, dst)


        YFNT = scratch("YFNT", [512, NT])
        MAGIC = 12582912.0

        def gen_cs(dst_c, dst_s, cols, nval, nvs, nmod, tmps, tk, dkeys):
            y, r, t_ = tmps
            p.op("dve", lambda e: e.tensor_scalar(out=y, in0=cols, scalar1=nvs, scalar2=MAGIC, op0=ALU.mult, op1=ALU.add),
                 reads=["fconst"], writes=[tk + "y"])
            p.op("dve", lambda e: e.tensor_scalar(out=r, in0=y, scalar1=MAGIC, scalar2=float(nmod), op0=ALU.subtract, op1=ALU.mult),
                 reads=[tk + "y"], writes=[tk + "r"])
            p.op("dve", lambda e: e.scalar_tensor_tensor(out=t_, in0=cols, scalar=nval, in1=r, op0=ALU.mult, op1=ALU.subtract),
                 reads=[tk + "r", "fconst"], writes=[tk + "t"])
            p.op("dve", lambda e: e.scalar_tensor_tensor(out=y, in0=t_, scalar=-1.0, in1=t_, op0=ALU.mult, op1=ALU.max),
                 reads=[tk + "t"], writes=[tk + "y"])
            p.op("act", lambda e: e.activation(out=dst_s, in_=t_, func=AF.Sin, scale=float(2 * np.pi / nmod)),
                 reads=[tk + "t"], writes=[dkeys[1]])
            p.op("act", lambda e: e.activation(out=dst_c, in_=y, func=AF.Sin, scale=float(-2 * np.pi / nmod), bias=float(np.pi / 2)),
                 reads=[tk + "y"], writes=[dkeys[0]])

        def fourier(N, tok0):
            areset()
            nch = N // 128
            W = min(512, N)
            ubt = alloc([4, N], BF16)
            ucs = alloc([nch, 4, 2 * 128], BF16)
            cs128 = alloc([256], BF16)
            colf = alloc([N], F32)
            nval = alloc([nch], F32)
            nvs = alloc([nch], F32)
            nvs128 = alloc([1], F32)
            tmps = [[alloc([512], F32) for _ in range(3)] for _ in range(2)]
            cblk = [alloc([512], BF16) for _ in range(2)]
            sblk = [alloc([512], BF16) for _ in range(2)]
            fstg = [alloc([512], BF16) for _ in range(4)]
            coli = alloc([N], I32)
            nvi = alloc([nch], I32)
            p.op("pool", lambda e: e.iota(coli, pattern=[[1, N]], base=0, channel_multiplier=0), writes=["coli"])
            p.op("pool", lambda e: e.iota(nvi, pattern=[[128, nch]], base=0, channel_multiplier=1), writes=["nvi"])
            p.op("dve", lambda e: e.tensor_copy(out=colf, in_=coli), reads=["coli"], writes=["fconst"])
            p.op("dve", lambda e: e.tensor_copy(out=nval, in_=nvi), reads=["nvi"], writes=["fconst"])
            p.op("dve", lambda e: e.tensor_scalar_mul(out=nvs, in0=nval, scalar1=1.0 / N), reads=["fconst"], writes=["fconst"])
            p.op("dve", lambda e: e.tensor_scalar_mul(out=nvs128, in0=nval[:, 0:1], scalar1=1.0 / 128), reads=["fconst"], writes=["fconst"])
            for g in range(4):
                p.dma("sp", lambda e, g=g: e.dma_start(out=ubt[:, g, :], in_=UBT.ap()[g * 128:(g + 1) * 128, tok0:tok0 + N]),
                      reads=["UBT"], writes=[f"ubt{g}"])
            gen_cs(cs128[:, 0:128], cs128[:, 128:256], colf[:, 0:128], nval[:, 0:1], nvs128[:, 0:1], 128,
                   [tm[:, 0:128] for tm in tmps[0]], "gt0", ["cs128", "cs128"])
            for i in range(nch):
                for gp in range(2):
                    bank, bkey = next_bank()
                    for gg in range(2):
                        g = gp * 2 + gg
                        p.op("pe", lambda e, g=g, gg=gg, i=i, bank=bank: e.matmul(
                            bank[:, gg * 256:(gg + 1) * 256], lhsT=ubt[:, g, i * 128:(i + 1) * 128], rhs=cs128,
                            start=True, stop=True), reads=[f"ubt{g}", "cs128"], writes=[bkey])
                    bv = bank.rearrange("q (g s c) -> q g s c", g=2, s=2)
                    ov = ucs[:, i, gp * 2:gp * 2 + 2, :].rearrange("q g (s c) -> q g s c", s=2)
                    p.op("dve", lambda e, bv=bv, ov=ov: e.tensor_copy(out=ov[:, :, 0, :], in_=bv[:, :, 0, :]),
                         reads=[bkey], writes=[f"ucs{i}c{gp}"])
                    p.op("dve", lambda e, bv=bv, ov=ov: e.tensor_scalar_mul(out=ov[:, :, 1, :], in0=bv[:, :, 1, :], scalar1=-1.0),
                         reads=[bkey], writes=[f"ucs{i}s{gp}"])
            scale = float(1.0 / np.sqrt(N * 128.0))
            blk = 0
            for mg in range(N // W):
                for i in range(nch):
                    b = blk % 2
                    blk += 1
                    gen_cs(cblk[b][:, 0:W], sblk[b][:, 0:W], colf[:, mg * W:(mg + 1) * W], nval[:, i:i + 1], nvs[:, i:i + 1], N,
                           [tm[:, 0:W] for tm in tmps[b]], f"gt{b}", [f"cblk{b}", f"sblk{b}"])
                    for g in range(4):
                        rk = [f"ucs{i}c{g // 2}", f"ucs{i}s{g // 2}", f"cblk{b}", f"sblk{b}"]
                        p.op("pe", lambda e, g=g, i=i, b=b: e.matmul(
                            pbank[g][:, 0:W], lhsT=ucs[:, i, g, 0:128], rhs=cblk[b][:, 0:W], start=(i == 0), stop=False),
                            reads=rk, writes=[f"pb{g}"])
                        p.op("pe", lambda e, g=g, i=i, b=b: e.matmul(
                            pbank[g][:, 0:W], lhsT=ucs[:, i, g, 128:256], rhs=sblk[b][:, 0:W], start=False, stop=(i == nch - 1)),
                            reads=rk, writes=[f"pb{g}"])
                for g in range(4):
                    if g % 2 == 0:
                        p.op("act", lambda e, g=g: e.mul(out=fstg[g][:, 0:W], in_=pbank[g][:, 0:W], mul=scale),
                             reads=[f"pb{g}"], writes=[f"fstg{g}"])
                    else:
                        p.op("dve", lambda e, g=g: e.tensor_scalar_mul(out=fstg[g][:, 0:W], in0=pbank[g][:, 0:W], scalar1=scale),
                             reads=[f"pb{g}"], writes=[f"fstg{g}"])
                    p.dma("sp", lambda e, g=g, mg=mg: e.dma_start(
                        out=YFNT.ap()[g * 128:(g + 1) * 128, tok0 + mg * W:tok0 + (mg + 1) * W], in_=fstg[g][:, 0:W]),
                        reads=[f"fstg{g}"], writes=["YFNT"])


        YRETT = scratch("YRETT", [1024, NT])
        QS = 128.0 ** -0.5
        LNQS = float(np.log(QS))
        GN_EPS = 1e-5

        def retention(l):
            areset()
            dec = alloc([8], F32)
            lg = alloc([8], F32)
            gC = alloc([8], F32)
            diffi = alloc([128], I32)
            diff = alloc([128], F32)
            rp = alloc([128], F32)
            rn = alloc([128], F32)
            ef = alloc([128], F32)
            eb = alloc([128], F32)
            pci = alloc([2], I32)
            pc = alloc([2], F32)
            cri = alloc([2, 128], I32)
            cr = alloc([2, 128], F32)
            dmask = alloc([4, 128], F32)
            qdf = alloc([4, 128], F32)
            qdb = alloc([4, 128], F32)
            kdf = alloc([4], F32)
            kdb = alloc([4], F32)
            gnw = alloc([1024], F32)
            Sf = alloc([4, 256], F32)
            Sb = alloc([4, 256], F32)
            Sf16 = alloc([4, 256], BF16)
            Sbprev = alloc([32, 1024], BF16)
            qt = [alloc([512], BF16) for _ in range(2)]
            kt = [alloc([512], BF16) for _ in range(2)]
            vt = [alloc([1024], BF16) for _ in range(2)]
            gt = [alloc([1024], BF16) for _ in range(2)]
            rt = [alloc([256], F32) for _ in range(2)]
            t1 = alloc([512], F32)
            t2 = alloc([512], F32)
            q16 = alloc([512], BF16)
            k16 = alloc([512], BF16)
            ks16 = alloc([512], BF16)
            qkT = alloc([8, 128], BF16)
            PT = alloc([4, 128], BF16)
            qfT = alloc([4, 128], BF16)
            qbT = alloc([4, 128], BF16)
            rstat = alloc([4, 6], F32)
            rmv = alloc([4, 2], F32)
            rr = alloc([4], F32)
            yn = alloc([1024], F32)
            sg = alloc([1024], F32)
            y16 = alloc([1024], BF16)
            yT = alloc([8, 128], BF16)

            p.dma("sp", lambda e: e.dma_start(out=dec, in_=ret_decay.ap()[l, :].partition_broadcast(128)), writes=["dec"])
            p.dma("sp", lambda e: e.dma_start(out=gnw, in_=ret_gn_w.ap()[l, :].partition_broadcast(128)), writes=["gnw"])
            p.op("act", lambda e: e.activation(out=lg, in_=dec, func=AF.Exp, scale=-1.0), reads=["dec"], writes=["lg"])
            p.op("act", lambda e: e.activation(out=lg, in_=lg, func=AF.Ln, bias=1.0), reads=["lg"], writes=["lg"])
            p.op("dve", lambda e: e.tensor_scalar_mul(out=lg, in0=lg, scalar1=-1.0), reads=["lg"], writes=["lg"])
            p.op("act", lambda e: e.activation(out=gC, in_=lg, func=AF.Exp, scale=128.0), reads=["lg"], writes=["gC"])
            p.op("pool", lambda e: e.iota(diffi, pattern=[[1, 128]], base=0, channel_multiplier=-1), writes=["diffi"])
            p.op("pool", lambda e: e.iota(pci[:, 0:1], pattern=[[0, 1]], base=127, channel_multiplier=-1), writes=["pci"])
            p.op("pool", lambda e: e.iota(pci[:, 1:2], pattern=[[0, 1]], base=0, channel_multiplier=1), reads=["pci"], writes=["pci"])
            p.op("pool", lambda e: e.iota(cri[:, 0, :], pattern=[[1, 128]], base=1, channel_multiplier=0), writes=["cri"])
            p.op("pool", lambda e: e.iota(cri[:, 1, :], pattern=[[-1, 128]], base=128, channel_multiplier=0), reads=["cri"], writes=["cri"])
            p.op("dve", lambda e: e.tensor_copy(out=diff, in_=diffi), reads=["diffi"], writes=["diff"])
            p.op("dve", lambda e: e.tensor_copy(out=pc, in_=pci), reads=["pci"], writes=["pc"])
            p.op("dve", lambda e: e.tensor_copy(out=cr, in_=cri), reads=["cri"], writes=["cr"])
            p.op("dve", lambda e: e.tensor_scalar_max(out=rp, in0=diff, scalar1=0.0), reads=["diff"], writes=["rp"])
            p.op("dve", lambda e: e.tensor_tensor(out=rn, in0=rp, in1=diff, op=ALU.subtract), reads=["rp", "diff"], writes=["rn"])
            for h in range(4):
                p.op("act", lambda e, h=h: e.activation(out=kdf[:, h:h + 1], in_=pc[:, 0:1], func=AF.Exp, scale=lg[:, h:h + 1]),
                     reads=["pc", "lg"], writes=["kdf"])
                p.op("act", lambda e, h=h: e.activation(out=kdb[:, h:h + 1], in_=pc[:, 1:2], func=AF.Exp, scale=lg[:, 4 + h:5 + h]),
                     reads=["pc", "lg"], writes=["kdb"])
                p.op("act", lambda e, h=h: e.activation(out=qdf[:, h, :], in_=cr[:, 0, :], func=AF.Exp, scale=lg[:, h:h + 1], bias=LNQS),
                     reads=["cr", "lg"], writes=["qdf"])
                p.op("act", lambda e, h=h: e.activation(out=qdb[:, h, :], in_=cr[:, 1, :], func=AF.Exp, scale=lg[:, 4 + h:5 + h], bias=LNQS),
                     reads=["cr", "lg"], writes=["qdb"])
                p.op("act", lambda e, h=h: e.activation(out=ef, in_=rp, func=AF.Exp, scale=lg[:, h:h + 1], bias=LNQS),
                     reads=["rp", "lg"], writes=["ef"])
                p.op("act", lambda e, h=h: e.activation(out=eb, in_=rn, func=AF.Exp, scale=lg[:, 4 + h:5 + h], bias=LNQS),
                     reads=["rn", "lg"], writes=["eb"])
                p.op("pool", lambda e: e.affine_select(out=ef, in_=ef, pattern=[[1, 128]], compare_op=ALU.is_ge, fill=0.0,
                                                      base=0, channel_multiplier=-1), reads=["ef"], writes=["ef"])
                p.op("pool", lambda e: e.affine_select(out=eb, in_=eb, pattern=[[-1, 128]], compare_op=ALU.is_gt, fill=0.0,
                                                      base=0, channel_multiplier=1), reads=["eb"], writes=["eb"])
                p.op("dve", lambda e, h=h: e.tensor_tensor(out=dmask[:, h, :], in0=ef, in1=eb, op=ALU.add),
                     reads=["ef", "eb"], writes=["dmask"])
            p.op("pool", lambda e: e.memset(Sf, 0.0), writes=["Sf"])
            p.op("pool", lambda e: e.memset(Sb, 0.0), writes=["Sb"])
            p.op("pool", lambda e: e.memset(Sf16, 0.0), writes=["Sf16"])

            pbS = pbank[0]
            pbO = [pbank[1], pbank[2]]
            pbK = [pbank[3], pbank[4]]

            def rope(src, dst, rtile, dk):
                sv = src.rearrange("q (h r u d) -> q (h r) u d", h=4, r=2, u=2)
                Cb = rtile[:, 0:128].unsqueeze(1).broadcast_to([128, 4, 128])
                Sv = rtile[:, 128:256].rearrange("q (r u d) -> q r u d", r=2, u=2)
                t1v = t1.rearrange("q (h x) -> q h x", h=4)
                t2v = t2.rearrange("q (h r u d) -> q h r u d", h=4, r=2, u=2)
                s5 = src.rearrange("q (h r u d) -> q h r u d", h=4, r=2, u=2)
                p.op("dve", lambda e: e.tensor_tensor(out=t1v, in0=src.rearrange("q (h x) -> q h x", h=4), in1=Cb, op=ALU.mult),
                     reads=[dk + "src", dk + "rt"], writes=["t1"])
                for u in range(2):
                    for r in range(2):
                        p.op("dve", lambda e, u=u, r=r: e.tensor_tensor(
                            out=t2v[:, :, r, u, :], in0=s5[:, :, r, 1 - u, :],
                            in1=Sv[:, r, u, :].unsqueeze(1).broadcast_to([128, 4, 32]), op=ALU.mult),
                            reads=[dk + "src", dk + "rt"], writes=["t2"])
                t1w = t1.rearrange("q (h r u d) -> q (h r) u d", h=4, r=2, u=2)
                t2w = t2.rearrange("q (h r u d) -> q (h r) u d", h=4, r=2, u=2)
                dw = dst.rearrange("q (h r u d) -> q (h r) u d", h=4, r=2, u=2)
                p.op("dve", lambda e: e.tensor_tensor(out=dw[:, :, 0, :], in0=t1w[:, :, 0, :], in1=t2w[:, :, 0, :], op=ALU.subtract),
                     reads=["t1", "t2"], writes=[dk])
                p.op("dve", lambda e: e.tensor_tensor(out=dw[:, :, 1, :], in0=t1w[:, :, 1, :], in1=t2w[:, :, 1, :], op=ALU.add),
                     reads=["t1", "t2"], writes=[dk])

            def load_k_v(n, tok, b, use_rope, need_q):
                p.dma("sp", lambda e: e.dma_start(out=kt[b], in_=KR.ap()[tok:tok + 128, :]), reads=["KR"], writes=[f"kt{b}"])
                p.dma("sp", lambda e: e.dma_start(out=vt[b], in_=VR.ap()[tok:tok + 128, :]), reads=["VR"], writes=[f"vt{b}"])
                if use_rope:
                    p.dma("sp", lambda e: e.dma_start(out=rt[b], in_=rope_t.ap()[tok:tok + 128, :]), writes=[f"rt{b}"])
                if need_q:
                    p.dma("sp", lambda e: e.dma_start(out=qt[b], in_=QR.ap()[tok:tok + 128, :]), reads=["QR"], writes=[f"qt{b}"])
                    p.dma("sp", lambda e: e.dma_start(out=gt[b], in_=GR.ap()[tok:tok + 128, :]), reads=["GR"], writes=[f"gt{b}"])

            def prep_k(b, use_rope):
                if use_rope:
                    p.last_w["k16src"] = p.last_w.get(f"kt{b}")
                    p.last_w["k16rt"] = p.last_w.get(f"rt{b}")
                    rope(kt[b], k16, rt[b], "k16")
                    p.readers.setdefault(f"kt{b}", []).extend(p._lw("k16"))
                    p.readers.setdefault(f"rt{b}", []).extend(p._lw("k16"))
                else:
                    p.op("dve", lambda e: e.tensor_copy(out=k16, in_=kt[b]), reads=[f"kt{b}"], writes=["k16"])

            def prep_q(b, use_rope):
                if use_rope:
                    p.last_w["q16src"] = p.last_w.get(f"qt{b}")
                    p.last_w["q16rt"] = p.last_w.get(f"rt{b}")
                    rope(qt[b], q16, rt[b], "q16")
                    p.readers.setdefault(f"qt{b}", []).extend(p._lw("q16"))
                    p.readers.setdefault(f"rt{b}", []).extend(p._lw("q16"))
                else:
                    p.op("dve", lambda e: e.tensor_copy(out=q16, in_=qt[b]), reads=[f"qt{b}"], writes=["q16"])

            def kv_update(b, kd, S, gcol, skey):
                for h in range(4):
                    p.op("dve", lambda e, h=h: e.tensor_scalar_mul(out=ks16[:, h * 128:(h + 1) * 128], in0=k16[:, h * 128:(h + 1) * 128],
                                                                   scalar1=kd[:, h:h + 1]), reads=["k16", "kdf", "kdb"], writes=["ks16"])
                for h in range(4):
                    p.op("pe", lambda e, h=h: e.matmul(pbK[h // 2][:, (h % 2) * 256:(h % 2) * 256 + 256], lhsT=ks16[:, h * 128:(h + 1) * 128],
                                                       rhs=vt[b][:, h * 256:(h + 1) * 256], start=True, stop=True),
                         reads=["ks16", f"vt{b}"], writes=[f"pb{3 + h // 2}"])
                for h in range(4):
                    p.op("dve", lambda e, h=h: e.scalar_tensor_tensor(
                        out=S[:, h, :], in0=S[:, h, :], scalar=gC[:, gcol + h:gcol + h + 1],
                        in1=pbK[h // 2][:, (h % 2) * 256:(h % 2) * 256 + 256], op0=ALU.mult, op1=ALU.add),
                        reads=[skey, "gC", f"pb{3 + h // 2}"], writes=[skey])


            def pass1_chunk(n, b, tok, use_rope):
                if True:
                    load_k_v(n, tok, b, use_rope, False)
                    prep_k(b, use_rope)
                    p.op("act", lambda e, n=n: e.copy(out=Sbprev[:, n, :], in_=Sb.rearrange("q h d -> q (h d)")),
                         reads=["Sb"], writes=[f"Sbprev{n}"])
                    kv_update(b, kdb, Sb, 4, "Sb")
            def pass2_chunk(n, b, tok, use_rope):
                if True:
                    load_k_v(n, tok, b, use_rope, True)
                    prep_k(b, use_rope)
                    prep_q(b, use_rope)
                    for h in range(4):
                        p.op("pe", lambda e, h=h: e.transpose(ptr[0][:, h * 128:(h + 1) * 128], q16[:, h * 128:(h + 1) * 128], ident[:]),
                             reads=["q16", "ident"], writes=["ptr0"])
                        p.op("pe", lambda e, h=h: e.transpose(ptr[0][:, (4 + h) * 128:(5 + h) * 128], k16[:, h * 128:(h + 1) * 128], ident[:]),
                             reads=["k16", "ident"], writes=["ptr0"])
                    p.op("act", lambda e: e.copy(out=qkT, in_=ptr[0][:, :].rearrange("q (k n) -> q k n", k=8)),
                         reads=["ptr0"], writes=["qkT"])
                    for h in range(4):
                        p.op("pe", lambda e, h=h: e.matmul(pbS[:, h * 128:(h + 1) * 128], lhsT=qkT[:, 4 + h, :], rhs=qkT[:, h, :],
                                                           start=True, stop=True), reads=["qkT"], writes=["pb0"])
                    p.op("dve", lambda e: e.tensor_tensor(out=PT, in0=pbS[:, :].rearrange("q (h c) -> q h c", h=4), in1=dmask, op=ALU.mult),
                         reads=["pb0", "dmask"], writes=["PT"])
                    p.op("dve", lambda e: e.tensor_tensor(out=qfT, in0=qkT[:, 0:4, :], in1=qdf, op=ALU.mult),
                         reads=["qkT", "qdf"], writes=["qfT"])
                    p.op("dve", lambda e: e.tensor_tensor(out=qbT, in0=qkT[:, 0:4, :], in1=qdb, op=ALU.mult),
                         reads=["qkT", "qdb"], writes=["qbT"])
                    for h in range(4):
                        ob = pbO[h // 2][:, (h % 2) * 256:(h % 2) * 256 + 256]
                        ok = f"pb{1 + h // 2}"
                        p.op("pe", lambda e, h=h, ob=ob: e.matmul(ob, lhsT=PT[:, h, :], rhs=vt[b][:, h * 256:(h + 1) * 256], start=True, stop=False),
                             reads=["PT", f"vt{b}"], writes=[ok])
                        p.op("pe", lambda e, h=h, ob=ob: e.matmul(ob, lhsT=qfT[:, h, :], rhs=Sf16[:, h, :], start=False, stop=False),
                             reads=["qfT", "Sf16"], writes=[ok])
                        p.op("pe", lambda e, h=h, ob=ob, n=n: e.matmul(ob, lhsT=qbT[:, h, :], rhs=Sbprev[:, n, h * 256:(h + 1) * 256],
                                                                       start=False, stop=True),
                             reads=["qbT", f"Sbprev{n}"], writes=[ok])
                    kv_update(b, kdf, Sf, 0, "Sf")
                    p.op("act", lambda e: e.copy(out=Sf16, in_=Sf), reads=["Sf"], writes=["Sf16"])
                    for h in range(4):
                        ob = pbO[h // 2][:, (h % 2) * 256:(h % 2) * 256 + 256]
                        ok = f"pb{1 + h // 2}"
                        p.op("dve", lambda e, h=h, ob=ob: e.bn_stats(out=rstat[:, h, :], in_=ob), reads=[ok], writes=["rstat"])
                    for h in range(4):
                        p.op("dve", lambda e, h=h: e.bn_aggr(out=rmv[:, h, :], in_=rstat[:, h, :]), reads=["rstat"], writes=["rmv"])
                    p.op("dve", lambda e: e.tensor_scalar_add(out=rr, in0=rmv[:, :, 1], scalar1=GN_EPS), reads=["rmv"], writes=["rr"])
                    p.op("act", lambda e: e.sqrt(out=rr, in_=rr), reads=["rr"], writes=["rr"])
                    p.op("dve", lambda e: e.reciprocal(out=rr, in_=rr), reads=["rr"], writes=["rr"])
                    for h in range(4):
                        ob = pbO[h // 2][:, (h % 2) * 256:(h % 2) * 256 + 256]
                        ok = f"pb{1 + h // 2}"
                        p.op("dve", lambda e, h=h, ob=ob: e.tensor_scalar(out=yn[:, h * 256:(h + 1) * 256], in0=ob, scalar1=rmv[:, h, 0:1],
                                                                          scalar2=rr[:, h:h + 1], op0=ALU.subtract, op1=ALU.mult),
                             reads=[ok, "rmv", "rr"], writes=["yn"])
                    p.op("act", lambda e: e.activation(out=sg, in_=gt[b], func=AF.Silu), reads=[f"gt{b}"], writes=["sg"])
                    p.op("dve", lambda e: e.tensor_tensor(out=yn, in0=yn, in1=gnw, op=ALU.mult), reads=["yn", "gnw"], writes=["yn"])
                    p.op("dve", lambda e: e.tensor_tensor(out=y16, in0=yn, in1=sg, op=ALU.mult), reads=["yn", "sg"], writes=["y16"])
                    for k in range(8):
                        p.op("pe", lambda e, k=k: e.transpose(ptr[1][:, k * 128:(k + 1) * 128], y16[:, k * 128:(k + 1) * 128], ident[:]),
                             reads=["y16", "ident"], writes=["ptr1"])
                    p.op("act", lambda e: e.copy(out=yT, in_=ptr[1][:, :].rearrange("q (k n) -> q k n", k=8)), reads=["ptr1"], writes=["yT"])
                    p.dma("sp", lambda e, tok=tok: e.dma_start(out=YRETT.ap()[:, tok:tok + 128].rearrange("(k q) t -> q k t", q=128), in_=yT),
                          reads=["yT"], writes=["YRETT"])

            def segment(tok0, nchunks, use_rope):
                for n in range(nchunks - 1, -1, -1):
                    pass1_chunk(n, n % 2, tok0 + n * 128, use_rope)
                for n in range(nchunks):
                    pass2_chunk(n, n % 2, tok0 + n * 128, use_rope)

            segment(SEQ, 2, False)
            segment(0, 32, True)


        YNAT = scratch("YNAT", [512, NT])

        def nattn(l, with_ctx_q):
            areset()
            qT = alloc([NT], BF16)
            kT = alloc([NT], BF16)
            va_aug = alloc([NTILE, 8, 65], BF16)
            y_tm = alloc([NTILE, 512], BF16)
            tmpv = alloc([17, 512], BF16)
            nabt = alloc([3200], F32)
            maskt = alloc([3200], F32)
            emb = alloc([5, 5, 128], BF16)
            PTs = [alloc([7, 128], BF16) for _ in range(2)]
            rec = alloc([4], F32)
            nstg = alloc([4, 128], BF16)
            p.dma("sp", lambda e: e.dma_start(out=maskt, in_=na_mask.ap()), writes=["maskt"])
            p.op("pool", lambda e: e.memset(va_aug[:, :, :, 64:65], 1.0), writes=["va_ones"])
            for half in range(2):
                p.dma("sp", lambda e, half=half: e.dma_start(
                    out=tmpv, in_=VA.ap()[half * 17 * 128:(half + 1) * 17 * 128, :].rearrange("(t q) d -> q t d", q=128)),
                    reads=["VA"], writes=["tmpv"])
                p.op("dve", lambda e, half=half: e.tensor_copy(
                    out=va_aug[:, half * 17:(half + 1) * 17, :, 0:64], in_=tmpv.rearrange("q t (h d) -> q t h d", h=8)),
                    reads=["tmpv"], writes=[f"va{half}"])
            slot_rr = [0]

            def one(h, tq, keytiles, cls, off):
                bi = tq % 2
                big = pbig[bi][:, :].rearrange("q (j n) -> q j n", j=8)
                PT = PTs[bi]
                nk = len(keytiles)
                for j, ktile in enumerate(keytiles):
                    p.op("pe", lambda e, j=j, ktile=ktile: e.matmul(
                        big[:, j, :], lhsT=kT[off:off + 64, ktile * 128:(ktile + 1) * 128],
                        rhs=qT[off:off + 64, tq * 128:(tq + 1) * 128], start=True, stop=True),
                        reads=["qT", "kT"], writes=[f"pbig{bi}"])
                n0 = min(nk, 4)
                p.op("act", lambda e: e.activation(out=PT[:, 0:n0, :], in_=big[:, 0:n0, :], func=AF.Exp, scale=0.125),
                     reads=[f"pbig{bi}"], writes=[f"PT{bi}"])
                if nk > 4:
                    p.op("act", lambda e: e.activation(out=PT[:, 4:nk, :], in_=big[:, 4:nk, :], func=AF.Exp, scale=0.125),
                         reads=[f"pbig{bi}"], writes=[f"PT{bi}"])
                if cls is not None:
                    p.op("dve", lambda e: e.tensor_tensor(out=PT[:, 0:5, :], in0=PT[:, 0:5, :], in1=emb[:, cls, :, :], op=ALU.mult),
                         reads=[f"PT{bi}", "emb"], writes=[f"PT{bi}"])
                slot_rr[0] = (slot_rr[0] + 1) % 8
                sl = slot_rr[0]
                po = pb45[sl // 4][:, (sl % 4) * 128:(sl % 4) * 128 + 65]
                pk = f"po{sl}"
                for j, ktile in enumerate(keytiles):
                    p.op("pe", lambda e, j=j, ktile=ktile: e.matmul(
                        po, lhsT=PT[:, j, :], rhs=va_aug[:, ktile, h, :], start=(j == 0), stop=(j == nk - 1)),
                        reads=[f"PT{bi}", "va0", "va1", "va_ones"], writes=[pk])
                rc = rec[:, sl % 4:sl % 4 + 1]
                p.op("dve", lambda e: e.reciprocal(out=rc, in_=po[:, 64:65]), reads=[pk], writes=[f"rec{sl % 4}"])
                p.op("dve", lambda e: e.tensor_scalar_mul(out=y_tm[:, tq, h * 64:(h + 1) * 64], in0=po[:, 0:64], scalar1=rc),
                     reads=[pk, f"rec{sl % 4}"], writes=[f"ytm{tq}"])

            for h in range(8):
                pair, off = h // 2, (h % 2) * 64
                if h % 2 == 0:
                    p.dma("sp", lambda e, pair=pair: e.dma_start(out=qT, in_=QAT.ap()[pair * 128:(pair + 1) * 128, :]),
                          reads=["QAT"], writes=["qT"])
                    p.dma("sp", lambda e, pair=pair: e.dma_start(out=kT, in_=KAT.ap()[pair * 128:(pair + 1) * 128, :]),
                          reads=["KAT"], writes=["kT"])
                p.dma("sp", lambda e, h=h: e.dma_start(out=nabt, in_=na_bias.ap()[l, h, :, :]), writes=["nabt"])
                p.op("act", lambda e: e.activation(out=nabt, in_=nabt, func=AF.Exp), reads=["nabt"], writes=["nabt"])
                p.op("dve", lambda e: e.tensor_tensor(out=emb.rearrange("q c j n -> q (c j n)"), in0=nabt, in1=maskt, op=ALU.mult),
                     reads=["nabt", "maskt"], writes=["emb"])
                for tq in range(32):
                    cls = {0: 0, 1: 1, 30: 3, 31: 4}.get(tq, 2)
                    k0 = min(max(tq - 2, 0), 27)
                    one(h, tq, [k0 + j for j in range(5)] + [32, 33], cls, off)
                if with_ctx_q:
                    for tq in (32, 33):
                        one(h, tq, [32, 33], None, off)
            for t in range(NTILE if with_ctx_q else 32):
                for k in range(4):
                    p.op("pe", lambda e, k=k, t=t: e.transpose(ptr[t % 2][:, k * 128:(k + 1) * 128], y_tm[:, t, k * 128:(k + 1) * 128], ident[:]),
                         reads=[f"ytm{t}", "ident"], writes=[f"ptr{t % 2}"])
                copy_any(nstg, ptr[t % 2][:, 0:512].rearrange("q (k n) -> q k n", k=4), [f"ptr{t % 2}"], ["nstg"])
                p.dma("sp", lambda e, t=t: e.dma_start(out=YNAT.ap()[:, t * 128:(t + 1) * 128].rearrange("(k q) n -> q k n", q=128), in_=nstg),
                      reads=["nstg"], writes=["YNAT"])


        def merge(l, ntiles):
            areset()
            wna = alloc([4, 1024], BF16)
            wfn = alloc([4, 1024], BF16)
            wret = alloc([8, 1024], BF16)
            wout = alloc([8, 1024], BF16)
            lnw = alloc([1024], F32)
            lnb = alloc([1024], F32)
            glt = [alloc([3072], BF16) for _ in range(2)]
            gates = alloc([3072], F32)
            ynT = [alloc([4, 128], BF16) for _ in range(2)]
            yfT = [alloc([4, 128], BF16) for _ in range(2)]
            yrT = [alloc([8, 128], BF16) for _ in range(2)]
            xt = [alloc([1024], F32) for _ in range(2)]
            ysum = alloc([1024], F32)
            ytmp = alloc([1024], F32)
            y16 = alloc([1024], BF16)
            ysT = alloc([8, 128], BF16)
            xo = alloc([1024], F32)
            stats = alloc([2, 6], F32)
            mv = alloc([2], F32)
            rstd = alloc([1], F32)
            p.dma("pool", lambda e: e.dma_start(out=wna, in_=w_o_na.ap()[l].rearrange("(c q) n -> q c n", q=128)), writes=["wna"])
            p.dma("pool", lambda e: e.dma_start(out=wfn, in_=w_fourier.ap()[l].rearrange("(c q) n -> q c n", q=128)), writes=["wfn"])
            p.dma("pool", lambda e: e.dma_start(out=wret, in_=w_o_ret.ap()[l].rearrange("(c q) n -> q c n", q=128)), writes=["wret"])
            p.dma("pool", lambda e: e.dma_start(out=wout, in_=w_out.ap()[l].rearrange("(c q) n -> q c n", q=128)), writes=["wout"])
            p.dma("sp", lambda e: e.dma_start(out=lnw, in_=ln_mix_w.ap()[l, :].partition_broadcast(128)), writes=["lnw"])
            p.dma("sp", lambda e: e.dma_start(out=lnb, in_=ln_mix_b.ap()[l, :].partition_broadcast(128)), writes=["lnb"])

            def tile_fn(t, b, j):
                tok = t * 128
                p.dma("sp", lambda e: e.dma_start(out=glt[b], in_=GL.ap()[tok:tok + 128, :]), reads=["GL"], writes=[f"glt{b}"])
                p.dma("sp", lambda e: e.dma_start(out=ynT[b], in_=YNAT.ap()[:, tok:tok + 128].rearrange("(k q) n -> q k n", q=128)),
                      reads=["YNAT"], writes=[f"ynT{b}"])
                p.dma("sp", lambda e: e.dma_start(out=yfT[b], in_=YFNT.ap()[:, tok:tok + 128].rearrange("(k q) n -> q k n", q=128)),
                      reads=["YFNT"], writes=[f"yfT{b}"])
                p.dma("sp", lambda e: e.dma_start(out=yrT[b], in_=YRETT.ap()[:, tok:tok + 128].rearrange("(k q) n -> q k n", q=128)),
                      reads=["YRETT"], writes=[f"yrT{b}"])
                p.dma("sp", lambda e: e.dma_start(out=xt[b], in_=XRES.ap()[tok:tok + 128, :]), reads=["xres"], writes=[f"mxt{b}"])
                p.op("act", lambda e: e.activation(out=gates, in_=glt[b], func=AF.Sigmoid), reads=[f"glt{b}"], writes=["gates"])
                branches = [(ynT[b], wna, 4, f"ynT{b}", "wna"), (yfT[b], wfn, 4, f"yfT{b}", "wfn"), (yrT[b], wret, 8, f"yrT{b}", "wret")]
                for nb in range(2):
                    cs = slice(nb * 512, (nb + 1) * 512)
                    for bi, (yT, w, nk, yk, wk) in enumerate(branches):
                        bank, bkey = next_bank()
                        for k in range(nk):
                            p.op("pe", lambda e, k=k, yT=yT, w=w, bank=bank, nk=nk, cs=cs: e.matmul(
                                bank, lhsT=yT[:, k, :], rhs=w[:, k, cs], start=(k == 0), stop=(k == nk - 1)),
                                reads=[yk, wk], writes=[bkey])
                        gsl = gates[:, bi * 1024 + nb * 512: bi * 1024 + (nb + 1) * 512]
                        if bi == 0:
                            p.op("dve", lambda e, bank=bank, gsl=gsl, cs=cs: e.tensor_tensor(out=ysum[:, cs], in0=bank, in1=gsl, op=ALU.mult),
                                 reads=[bkey, "gates"], writes=[f"ysum{nb}"])
                        else:
                            p.op("dve", lambda e, bank=bank, gsl=gsl, cs=cs: e.tensor_tensor(out=ytmp[:, cs], in0=bank, in1=gsl, op=ALU.mult),
                                 reads=[bkey, "gates"], writes=[f"ytmp{nb}"])
                            dst = y16 if bi == 2 else ysum
                            p.op("dve", lambda e, dst=dst, cs=cs: e.tensor_tensor(out=dst[:, cs], in0=ysum[:, cs], in1=ytmp[:, cs], op=ALU.add),
                                 reads=[f"ysum{nb}", f"ytmp{nb}"], writes=[f"ysum{nb}", f"y16{nb}"])
                for k in range(8):
                    p.op("pe", lambda e, k=k: e.transpose(ptr[b][:, k * 128:(k + 1) * 128], y16[:, k * 128:(k + 1) * 128], ident[:]),
                         reads=["y160", "y161", "ident"], writes=[f"ptr{b}"])
                copy_any(ysT, ptr[b][:, :].rearrange("q (k n) -> q k n", k=8), [f"ptr{b}"], ["ysT"])
                for nb in range(2):
                    cs = slice(nb * 512, (nb + 1) * 512)
                    bank, bkey = next_bank()
                    for k in range(8):
                        p.op("pe", lambda e, k=k, bank=bank, cs=cs: e.matmul(bank, lhsT=ysT[:, k, :], rhs=wout[:, k, cs], start=(k == 0), stop=(k == 7)),
                             reads=["ysT", "wout"], writes=[bkey])
                    p.op("dve", lambda e, bank=bank, cs=cs, nb=nb: e.tensor_tensor(out=xo[:, cs], in0=bank, in1=mods[:, j, 2 * D + nb * 512:2 * D + (nb + 1) * 512],
                                                                     op=ALU.mult), reads=[bkey, "mods"], writes=["xo"])
                p.op("dve", lambda e: e.scalar_tensor_tensor(out=xo, in0=xt[b], scalar=DN_ALPHA, in1=xo, op0=ALU.mult, op1=ALU.add),
                     reads=[f"mxt{b}", "xo"], writes=["xo"])
                ln_tile(xo, stats, mv, rstd, "xo", "mst")
                p.op("dve", lambda e: e.tensor_tensor(out=xo, in0=xo, in1=lnw, op=ALU.mult), reads=["xo", "lnw"], writes=["xo"])
                p.op("dve", lambda e: e.tensor_tensor(out=xo, in0=xo, in1=lnb, op=ALU.add), reads=["xo", "lnb"], writes=["xo"])
                p.dma("sp", lambda e: e.dma_start(out=XRES.ap()[tok:tok + 128, :], in_=xo), reads=["xo"], writes=["xres_w"])

            for t in range(ntiles):
                tile_fn(t, t % 2, 0 if t < 32 else 1)


        NBLK = 526
        NROW = 640
        NSLOT = NROW * 128
        H16 = scratch("H16", [NT + 1, D])
        SHO = scratch("SHO", [NT, D])
        TBL = scratch("TBL", [NSLOT, 1], F32)
        OUTS = scratch("OUTS", [NSLOT, D])
        ident_f = sb("ident_f", [128, 128], F32)
        p.op("pool", lambda e: e.memset(ident_f[:], 1.0), writes=["ident_f"])
        p.op("pool", lambda e: e.affine_select(out=ident_f[:], in_=ident_f[:], pattern=[[-1, 128]],
                                              compare_op=ALU.is_equal, fill=0.0, base=0, channel_multiplier=1),
             reads=["ident_f"], writes=["ident_f"])

        def moe(l, ntiles):
            areset()
            rw = alloc([8, 256], F32)
            rb = alloc([256], F32)
            shgu = alloc([8, 512], BF16)
            shd = alloc([2, 1024], BF16)
            triU = alloc([128], BF16)
            ones16 = alloc([128], BF16)
            onesr = alloc([256], F32)
            cum = alloc([256], F32)
            eidi = alloc([256], I32)
            eidx = alloc([256], F32)
            qci = alloc([1], I32)
            qcf = alloc([1], F32)
            toki = alloc([NTILE], I32)
            tokf = alloc([NTILE], F32)
            W8 = alloc([NTILE, 8], F32)
            E8 = alloc([NTILE, 8], F32)
            R8 = alloc([NTILE, 8], F32)
            D8 = alloc([NTILE, 8], I32)
            BE = alloc([NROW], F32)
            WIDX = alloc([NROW], I32)
            padded = alloc([256], F32)
            pad_end = alloc([256], F32)
            pad_start = alloc([256], F32)
            xt = [alloc([1024], F32) for _ in range(2)]
            h16 = alloc([1024], BF16)
            hT16 = alloc([8, 128], BF16)
            hT32 = alloc([8, 128], F32)
            hm16 = alloc([256], BF16)
            sgt = alloc([256], F32)
            hmT = alloc([2, 128], BF16)
            sho16 = alloc([1024], BF16)
            sc = alloc([256], F32)
            sel = alloc([256], F32)
            selm = alloc([256], F32)
            m8 = alloc([8, 8], F32)
            gs = alloc([8], F32)
            g8 = alloc([8], F32)
            pen = alloc([8], F32)
            v8 = alloc([8], F32)
            A = alloc([256], F32)
            A16 = alloc([256], BF16)
            rankd = alloc([256], F32)
            junk = alloc([256], F32)
            d8f = alloc([8], F32)
            w8 = alloc([8], F32)
            wsum = alloc([1], F32)
            stats = alloc([2, 6], F32)
            mv = alloc([2], F32)
            rstd = alloc([1], F32)
            tfill = alloc([NROW], F32)
            zrow = alloc([1024], BF16)

            p.dma("sp", lambda e: e.dma_start(out=rw, in_=router_w.ap()[l].rearrange("(c q) n -> q c n", q=128)), writes=["rw"])
            p.dma("sp", lambda e: e.dma_start(out=rb, in_=router_bias.ap()[l, :].partition_broadcast(128)), writes=["rb"])
            p.dma("pool", lambda e: e.dma_start(out=shgu[:, :, 0:256], in_=sh_w_gate.ap()[l].rearrange("(c q) n -> q c n", q=128)), writes=["shgu_a"])
            p.dma("pool", lambda e: e.dma_start(out=shgu[:, :, 256:512], in_=sh_w_up.ap()[l].rearrange("(c q) n -> q c n", q=128)), writes=["shgu_b"])
            p.dma("pool", lambda e: e.dma_start(out=shd, in_=sh_w_down.ap()[l].rearrange("(c q) n -> q c n", q=128)), writes=["shd"])
            p.op("pool", lambda e: e.memset(triU, 1.0), writes=["triU"])
            p.op("pool", lambda e: e.affine_select(out=triU, in_=triU, pattern=[[1, 128]], compare_op=ALU.is_gt, fill=0.0,
                                                  base=0, channel_multiplier=-1), reads=["triU"], writes=["triU"])
            p.op("pool", lambda e: e.memset(ones16, 1.0), writes=["ones16"])
            p.op("pool", lambda e: e.memset(onesr, 1.0), writes=["onesr"])
            p.op("pool", lambda e: e.memset(cum, 0.0), writes=["cum"])
            p.op("pool", lambda e: e.iota(eidi, pattern=[[1, 256]], base=0, channel_multiplier=0), writes=["eidi"])
            p.op("dve", lambda e: e.tensor_copy(out=eidx, in_=eidi), reads=["eidi"], writes=["eidx"])
            p.op("pool", lambda e: e.iota(qci, pattern=[[0, 1]], base=0, channel_multiplier=1), writes=["qci"])
            p.op("dve", lambda e: e.tensor_copy(out=qcf, in_=qci), reads=["qci"], writes=["qcf"])
            p.op("pool", lambda e: e.iota(toki, pattern=[[128, NTILE]], base=0, channel_multiplier=1), writes=["toki"])
            p.op("dve", lambda e: e.tensor_copy(out=tokf, in_=toki), reads=["toki"], writes=["tokf"])
            p.op("pool", lambda e: e.memset(tfill, float(NT)), writes=["tfill"])
            p.op("pool", lambda e: e.memset(zrow, 0.0), writes=["zrow"])
            p.dma("sp", lambda e: e.dma_start(out=TBL.ap().rearrange("(q f) o -> q (f o)", q=128), in_=tfill), reads=["tfill"], writes=["TBL"])
            p.dma("sp", lambda e: e.dma_start(out=H16.ap()[NT:NT + 1, :], in_=zrow[0:1, :]), reads=["zrow"], writes=["H16"])

            def m1_tile(t, b, j):
                tok = t * 128
                p.dma("sp", lambda e: e.dma_start(out=xt[b], in_=XRES.ap()[tok:tok + 128, :]), reads=["xres", "xres_w"], writes=[f"xt{b}"])
                ln_tile(xt[b], stats, mv, rstd, f"xt{b}", "st")
                p.op("dve", lambda e: e.tensor_tensor(out=xt[b], in0=xt[b], in1=mods[:, j, 4 * D:5 * D], op=ALU.mult),
                     reads=[f"xt{b}", "mods"], writes=[f"xt{b}"])
                p.op("dve", lambda e: e.tensor_tensor(out=xt[b], in0=xt[b], in1=mods[:, j, 3 * D:4 * D], op=ALU.add),
                     reads=[f"xt{b}", "mods"], writes=[f"xt{b}"])
                p.op("act", lambda e: e.copy(out=h16, in_=xt[b]), reads=[f"xt{b}"], writes=["h16"])
                p.dma("sp", lambda e: e.dma_start(out=H16.ap()[tok:tok + 128, :], in_=h16), reads=["h16"], writes=["H16"])
                for k in range(8):
                    p.op("pe", lambda e, k=k: e.transpose(ptr[0][:, k * 128:(k + 1) * 128], h16[:, k * 128:(k + 1) * 128], ident[:]),
                         reads=["h16", "ident"], writes=["ptr0"])
                p.op("act", lambda e: e.copy(out=hT16, in_=ptr[0][:, :].rearrange("q (k n) -> q k n", k=8)), reads=["ptr0"], writes=["hT16"])
                for k in range(8):
                    p.op("pe", lambda e, k=k: e.matmul(pb45[0][:, :], lhsT=hT16[:, k, :], rhs=shgu[:, k, :], start=(k == 0), stop=(k == 7)),
                         reads=["hT16", "shgu_a", "shgu_b"], writes=["pb4"])
                p.op("act", lambda e: e.activation(out=sgt, in_=pb45[0][:, 0:256], func=AF.Silu), reads=["pb4"], writes=["sgt"])
                p.op("dve", lambda e: e.tensor_tensor(out=hm16, in0=sgt, in1=pb45[0][:, 256:512], op=ALU.mult), reads=["sgt", "pb4"], writes=["hm16"])
                for k in range(2):
                    p.op("pe", lambda e, k=k: e.transpose(ptr[1][:, k * 128:(k + 1) * 128], hm16[:, k * 128:(k + 1) * 128], ident[:]),
                         reads=["hm16", "ident"], writes=["ptr1"])
                p.op("act", lambda e: e.copy(out=hmT, in_=ptr[1][:, 0:256].rearrange("q (k n) -> q k n", k=2)), reads=["ptr1"], writes=["hmT"])
                for nb in range(2):
                    bank = pbank[2 + nb]
                    for k in range(2):
                        p.op("pe", lambda e, k=k, nb=nb, bank=bank: e.matmul(bank, lhsT=hmT[:, k, :], rhs=shd[:, k, nb * 512:(nb + 1) * 512],
                                                                             start=(k == 0), stop=(k == 1)), reads=["hmT", "shd"], writes=[f"pb{2 + nb}"])
                    copy_any(sho16[:, nb * 512:(nb + 1) * 512], bank, [f"pb{2 + nb}"], ["sho16"])
                p.dma("sp", lambda e: e.dma_start(out=SHO.ap()[tok:tok + 128, :], in_=sho16), reads=["sho16"], writes=["SHO"])
                for k in range(8):
                    p.op("pe", lambda e, k=k: e.transpose(pbig[0][:, k * 128:(k + 1) * 128], xt[b][:, k * 128:(k + 1) * 128], ident_f[:]),
                         reads=[f"xt{b}", "ident_f"], writes=["pb0", "pb1"])
                p.op("dve", lambda e: e.tensor_copy(out=hT32, in_=pbig[0][:, :].rearrange("q (k n) -> q k n", k=8)), reads=["pb0", "pb1"], writes=["hT32"])
                for k in range(8):
                    p.op("pe", lambda e, k=k: e.matmul(pb45[1][:, 0:256], lhsT=hT32[:, k, :], rhs=rw[:, k, :], start=(k == 0), stop=(k == 7)),
                         reads=["hT32", "rw"], writes=["pb5"])
                p.op("act", lambda e: e.activation(out=sc, in_=pb45[1][:, 0:256], func=AF.Sigmoid), reads=["pb5"], writes=["sc"])
                p.op("dve", lambda e: e.tensor_tensor(out=sel, in0=sc, in1=rb, op=ALU.add), reads=["sc", "rb"], writes=["sel"])
                for g in range(8):
                    p.op("dve", lambda e, g=g: e.max(out=m8[:, g, :], in_=sel[:, g * 32:(g + 1) * 32]), reads=["sel"], writes=["m8"])
                p.op("dve", lambda e: e.tensor_tensor(out=gs, in0=m8[:, :, 0], in1=m8[:, :, 1], op=ALU.add), reads=["m8"], writes=["gs"])
                p.op("dve", lambda e: e.max(out=g8, in_=gs), reads=["gs"], writes=["g8"])
                p.op("dve", lambda e: e.tensor_scalar(out=pen, in0=gs, scalar1=g8[:, 3:4], scalar2=None, op0=ALU.is_ge), reads=["gs", "g8"], writes=["pen"])
                p.op("dve", lambda e: e.tensor_scalar(out=pen, in0=pen, scalar1=1.0, scalar2=1.0e4, op0=ALU.subtract, op1=ALU.mult),
                     reads=["pen"], writes=["pen"])
                p.op("dve", lambda e: e.tensor_tensor(out=selm.rearrange("q (g x) -> q g x", g=8), in0=sel.rearrange("q (g x) -> q g x", g=8),
                                                      in1=pen.unsqueeze(2).broadcast_to([128, 8, 32]), op=ALU.add), reads=["sel", "pen"], writes=["selm"])
                p.op("dve", lambda e: e.max(out=v8, in_=selm), reads=["selm"], writes=["v8"])
                p.op("dve", lambda e: e.tensor_scalar(out=A, in0=selm, scalar1=v8[:, 7:8], scalar2=None, op0=ALU.is_ge), reads=["selm", "v8"], writes=["A"])
                p.op("dve", lambda e: e.tensor_copy(out=A16, in_=A), reads=["A"], writes=["A16"])
                p.op("pe", lambda e: e.matmul(pbank[2][:, 0:256], lhsT=triU, rhs=A16, start=True, stop=True), reads=["triU", "A16"], writes=["pb2"])
                p.op("pe", lambda e: e.matmul(pbank[3][:, 0:256], lhsT=ones16, rhs=A16, start=True, stop=True), reads=["ones16", "A16"], writes=["pb3"])
                p.op("dve", lambda e: e.tensor_tensor(out=rankd, in0=pbank[2][:, 0:256], in1=cum, op=ALU.add), reads=["pb2", "cum"], writes=["rankd"])
                p.op("dve", lambda e: e.tensor_tensor(out=cum, in0=pbank[3][:, 0:256], in1=cum, op=ALU.add), reads=["pb3", "cum"], writes=["cum"])
                for k in range(8):
                    for (src, dstT, key) in ((rankd, R8, "R8"), (sc, w8.unsqueeze(1), "w8"), (eidx, E8, "E8")):
                        oap = dstT[:, t, k:k + 1] if key != "w8" else w8[:, k:k + 1]
                        p.op("dve", lambda e, k=k, src=src, oap=oap: e.scalar_tensor_tensor(
                            out=junk, in0=selm, scalar=v8[:, k:k + 1], in1=src, op0=ALU.is_equal, op1=ALU.mult, accum_out=oap),
                            reads=["selm", "v8", "rankd", "sc", "eidx"], writes=["junk", f"{key}_{t}"])
                p.op("dve", lambda e: e.reduce_sum(out=wsum, in_=w8, axis=AX.X), reads=[f"w8_{t}"], writes=["wsum"])
                p.op("dve", lambda e: e.reciprocal(out=wsum, in_=wsum), reads=["wsum"], writes=["wsum"])
                p.op("dve", lambda e: e.tensor_scalar(out=W8[:, t, :], in0=w8, scalar1=wsum[:, 0:1], scalar2=2.5, op0=ALU.mult, op1=ALU.mult),
                     reads=[f"w8_{t}", "wsum"], writes=[f"W8_{t}"])

            for t in range(ntiles):
                m1_tile(t, t % 2, 0 if t < 32 else 1)

            p.barrier()
            MAGIC_ = 12582912.0
            p.op("dve", lambda e: e.tensor_scalar(out=padded, in0=cum, scalar1=63.25, scalar2=1.0 / 128, op0=ALU.add, op1=ALU.mult),
                 reads=["cum"], writes=["padded"])
            p.op("dve", lambda e: e.tensor_scalar_add(out=padded, in0=padded, scalar1=MAGIC_), reads=["padded"], writes=["padded"])
            p.op("dve", lambda e: e.tensor_scalar(out=padded, in0=padded, scalar1=MAGIC_, scalar2=128.0, op0=ALU.subtract, op1=ALU.mult),
                 reads=["padded"], writes=["padded"])
            p.op("dve", lambda e: e.tensor_tensor_scan(out=pad_end, data0=onesr, data1=padded, initial=0.0, op0=ALU.mult, op1=ALU.add),
                 reads=["padded", "onesr"], writes=["pad_end"])
            p.op("dve", lambda e: e.tensor_tensor(out=pad_start, in0=pad_end, in1=padded, op=ALU.subtract), reads=["pad_end", "padded"], writes=["pad_start"])
            for bq in range(NROW):
                p.op("dve", lambda e, bq=bq: e.scalar_tensor_tensor(out=junk, in0=pad_end, scalar=float(128 * bq), in1=onesr, op0=ALU.is_le,
                                                                    op1=ALU.mult, accum_out=BE[:, bq:bq + 1]),
                     reads=["pad_end", "onesr"], writes=["junk", "BE"])
            p.op("dve", lambda e: e.tensor_scalar(out=BE, in0=BE, scalar1=255.0, scalar2=128.0, op0=ALU.min, op1=ALU.mult), reads=["BE"], writes=["BE"])
            p.op("dve", lambda e: e.tensor_scalar_add(out=BE, in0=BE, scalar1=qcf[:, 0:1]), reads=["BE", "qcf"], writes=["BE"])
            p.op("dve", lambda e: e.tensor_copy(out=WIDX, in_=BE), reads=["BE"], writes=["WIDX"])

            def m1b_tile(t):
                for k in range(8):
                    p.op("dve", lambda e, k=k: e.scalar_tensor_tensor(
                        out=junk, in0=eidx, scalar=E8[:, t, k:k + 1], in1=pad_start, op0=ALU.is_equal, op1=ALU.mult, accum_out=d8f[:, k:k + 1]),
                        reads=["eidx", f"E8_{t}", "pad_start"], writes=["junk", "d8f"])
                p.op("dve", lambda e: e.tensor_tensor(out=d8f, in0=d8f, in1=R8[:, t, :], op=ALU.add), reads=["d8f", f"R8_{t}"], writes=["d8f"])
                p.op("dve", lambda e: e.tensor_copy(out=D8[:, t, :], in_=d8f), reads=["d8f"], writes=[f"D8_{t}"])
                for k in range(8):
                    p.dma("pool", lambda e, k=k: e.indirect_dma_start(
                        out=TBL.ap()[:, :], out_offset=bass.IndirectOffsetOnAxis(ap=D8[:, t, k:k + 1], axis=0),
                        in_=tokf[:, t:t + 1], in_offset=None), reads=[f"D8_{t}", "tokf", "TBL"], writes=["TBLs"])

            for t in range(ntiles):
                m1b_tile(t)

            p.barrier()
            tbl_sb = alloc([5, 128], F32)
            idxc = alloc([NROW], I32)
            wg = [alloc([8, 256], BF16) for _ in range(3)]
            wu = [alloc([8, 256], BF16) for _ in range(3)]
            wd = [alloc([2, 1024], BF16) for _ in range(3)]
            xg = [alloc([1024], BF16) for _ in range(3)]
            XT = alloc([8, 128], BF16)
            ostg = [alloc([1024], BF16) for _ in range(2)]
            p.dma("sp", lambda e: e.dma_start(out=tbl_sb, in_=TBL.ap().rearrange("(c r q) o -> r c (q o)", c=5, r=128)),
                  reads=["TBL", "TBLs"], writes=["tbl_sb"])
            for c in range(5):
                p.op("pe", lambda e, c=c: e.transpose(pbig[0][:, c * 128:(c + 1) * 128], tbl_sb[:, c, :], ident_f[:]),
                     reads=["tbl_sb", "ident_f"], writes=["pb0", "pb1"])
            p.op("dve", lambda e: e.tensor_copy(out=idxc, in_=pbig[0][:, 0:NROW]), reads=["pb0", "pb1"], writes=["idxc"])

            def load_block_w(bq):
                b3 = bq % 3
                off = bass.IndirectOffsetOnAxis(ap=WIDX[:, bq:bq + 1], axis=0)
                p.dma("pool", lambda e: e.indirect_dma_start(out=wg[b3].rearrange("q c n -> q (c n)"), out_offset=None,
                                                             in_=exp_w_gate[l].ap(), in_offset=off), reads=["WIDX"], writes=[f"wg{b3}"])
                p.dma("pool", lambda e: e.indirect_dma_start(out=wu[b3].rearrange("q c n -> q (c n)"), out_offset=None,
                                                             in_=exp_w_up[l].ap(), in_offset=off), reads=["WIDX"], writes=[f"wu{b3}"])
                p.dma("pool", lambda e: e.indirect_dma_start(out=wd[b3].rearrange("q c n -> q (c n)"), out_offset=None,
                                                             in_=exp_w_down[l].ap(), in_offset=off), reads=["WIDX"], writes=[f"wd{b3}"])

            def expert_block(bq):
                b3 = bq % 3
                o2 = bq % 2
                p.dma("pool", lambda e: e.indirect_dma_start(
                    out=xg[b3], out_offset=None, in_=H16.ap()[:, :],
                    in_offset=bass.IndirectOffsetOnAxis(ap=idxc[:, bq:bq + 1], axis=0)),
                    reads=["idxc", "H16"], writes=[f"xg{b3}"])
                for k in range(8):
                    p.op("pe", lambda e, k=k: e.transpose(ptr[0][:, k * 128:(k + 1) * 128], xg[b3][:, k * 128:(k + 1) * 128], ident[:]),
                         reads=[f"xg{b3}", "ident"], writes=["ptr0"])
                copy_any(XT, ptr[0][:, :].rearrange("q (k n) -> q k n", k=8), ["ptr0"], ["XT"])
                for k in range(8):
                    p.op("pe", lambda e, k=k: e.matmul(pb45[0][:, 0:256], lhsT=XT[:, k, :], rhs=wg[b3][:, k, :], start=(k == 0), stop=(k == 7)),
                         reads=["XT", f"wg{b3}"], writes=["pb4"])
                for k in range(8):
                    p.op("pe", lambda e, k=k: e.matmul(pb45[1][:, 0:256], lhsT=XT[:, k, :], rhs=wu[b3][:, k, :], start=(k == 0), stop=(k == 7)),
                         reads=["XT", f"wu{b3}"], writes=["pb5"])
                p.op("act", lambda e: e.activation(out=sgt, in_=pb45[0][:, 0:256], func=AF.Silu), reads=["pb4"], writes=["sgt"])
                p.op("dve", lambda e: e.tensor_tensor(out=hm16, in0=sgt, in1=pb45[1][:, 0:256], op=ALU.mult), reads=["sgt", "pb5"], writes=["hm16"])
                for k in range(2):
                    p.op("pe", lambda e, k=k: e.transpose(ptr[1][:, k * 128:(k + 1) * 128], hm16[:, k * 128:(k + 1) * 128], ident[:]),
                         reads=["hm16", "ident"], writes=["ptr1"])
                p.op("act", lambda e: e.copy(out=hmT, in_=ptr[1][:, 0:256].rearrange("q (k n) -> q k n", k=2)), reads=["ptr1"], writes=["hmT"])
                for nb in range(2):
                    bank = pbank[2 * o2 + nb]
                    bkey = f"pb{2 * o2 + nb}"
                    for k in range(2):
                        p.op("pe", lambda e, k=k, nb=nb, bank=bank: e.matmul(bank, lhsT=hmT[:, k, :], rhs=wd[b3][:, k, nb * 512:(nb + 1) * 512],
                                                                             start=(k == 0), stop=(k == 1)), reads=["hmT", f"wd{b3}"], writes=[bkey])
                    copy_any(ostg[o2][:, nb * 512:(nb + 1) * 512], bank, [bkey], [f"ostg{o2}"])
                r0 = bq * 128
                p.dma("sp", lambda e: e.dma_start(out=OUTS.ap()[r0:r0 + 128, :], in_=ostg[o2]), reads=[f"ostg{o2}"], writes=["OUTS"])

            NB_ = cfg.get("nblk", NBLK)
            load_block_w(0)
            load_block_w(1)
            for bq in range(NB_):
                if bq + 2 < NB_:
                    load_block_w(bq + 2)
                expert_block(bq)

            p.barrier()
            og = [alloc([1024], BF16) for _ in range(3)]
            acc = alloc([1024], F32)
            lnw = alloc([1024], F32)
            lnb = alloc([1024], F32)
            sh_in = alloc([1024], BF16)
            xo = alloc([1024], F32)
            p.dma("sp", lambda e: e.dma_start(out=lnw, in_=ln_ffn_w.ap()[l, :].partition_broadcast(128)), writes=["lnw"])
            p.dma("sp", lambda e: e.dma_start(out=lnb, in_=ln_ffn_b.ap()[l, :].partition_broadcast(128)), writes=["lnb"])
            gc = [0]

            def m3_tile(t, b, j):
                tok = t * 128
                p.dma("sp", lambda e: e.dma_start(out=sh_in, in_=SHO.ap()[tok:tok + 128, :]), reads=["SHO"], writes=["sh_in"])
                p.dma("sp", lambda e: e.dma_start(out=xt[b], in_=XRES.ap()[tok:tok + 128, :]), reads=["xres", "xres_w"], writes=[f"xt{b}"])
                p.op("dve", lambda e: e.tensor_copy(out=acc, in_=sh_in), reads=["sh_in"], writes=["acc"])
                for k in range(8):
                    g3 = gc[0] % 3
                    gc[0] += 1
                    p.dma("pool", lambda e, k=k, g3=g3: e.indirect_dma_start(
                        out=og[g3], out_offset=None, in_=OUTS.ap()[:, :],
                        in_offset=bass.IndirectOffsetOnAxis(ap=D8[:, t, k:k + 1], axis=0)),
                        reads=[f"D8_{t}", "OUTS"], writes=[f"og{g3}"])
                    p.op("dve", lambda e, k=k, g3=g3: e.scalar_tensor_tensor(out=acc, in0=og[g3], scalar=W8[:, t, k:k + 1], in1=acc,
                                                                             op0=ALU.mult, op1=ALU.add),
                         reads=[f"og{g3}", f"W8_{t}", "acc"], writes=["acc"])
                p.op("dve", lambda e: e.tensor_tensor(out=xo, in0=acc, in1=mods[:, j, 5 * D:6 * D], op=ALU.mult), reads=["acc", "mods"], writes=["xo"])
                p.op("dve", lambda e: e.scalar_tensor_tensor(out=xo, in0=xt[b], scalar=DN_ALPHA, in1=xo, op0=ALU.mult, op1=ALU.add),
                     reads=[f"xt{b}", "xo"], writes=["xo"])
                ln_tile(xo, stats, mv, rstd, "xo", "st")
                p.op("dve", lambda e: e.tensor_tensor(out=xo, in0=xo, in1=lnw, op=ALU.mult), reads=["xo", "lnw"], writes=["xo"])
                p.op("dve", lambda e: e.tensor_tensor(out=xo, in0=xo, in1=lnb, op=ALU.add), reads=["xo", "lnb"], writes=["xo"])
                p.dma("sp", lambda e: e.dma_start(out=XRES.ap()[tok:tok + 128, :], in_=xo), reads=["xo"], writes=["xres_w2"])

            for t in range(ntiles):
                m3_tile(t, t % 2, 0 if t < 32 else 1)

        p.dma("sp", lambda e: e.dma_start(out=XRES.ap()[0:SEQ, :], in_=x_in.ap()), writes=["xres"])
        p.dma("sp", lambda e: e.dma_start(out=XRES.ap()[SEQ:NT, :], in_=ctx_in.ap()), writes=["xres"])
        for l in range(cfg.get("layers", DEPTH)):
            phase0(l)
            if stages >= 1 and not cfg.get("moe_only"):
                phase1(l, XRES.ap())
            if stages >= 5 and not cfg.get("moe_only"):
                nattn(l, l < DEPTH - 1)
            if stages >= 4 and not cfg.get("noret") and not cfg.get("moe_only"):
                retention(l)
            if stages >= 3 and not cfg.get("nofourier") and not cfg.get("moe_only"):
                fourier(SEQ, 0)
                if l < DEPTH - 1:
                    fourier(CTX, SEQ)
            if stages >= 6 and not cfg.get("moe_only"):
                merge(l, NTILE if l < DEPTH - 1 else 32)
            if stages >= 7:
                moe(l, NTILE if l < DEPTH - 1 else 32)

        p.barrier()
        for i in range(4):
            p.dma("sp", lambda e, i=i: e.dma_start(out=out_t.ap()[i * 1024:(i + 1) * 1024, :],
                                                   in_=XRES.ap()[i * 1024:(i + 1) * 1024, :]),
                  reads=["xres"], writes=["out"])
        p.barrier()
        with nc.Block() as block:
            p.emit(block)
    return nc


_NC_CACHE = {}


def _rope_table():
    t = np.arange(SEQ)
    row, col = t // 64, t % 64
    inv = (10000.0 ** (-np.arange(32, dtype=np.float32) / 32)).astype(np.float32)
    ar = row.astype(np.float32)[:, None] * inv[None, :]
    ac = col.astype(np.float32)[:, None] * inv[None, :]
    C = np.concatenate([np.cos(ar), np.cos(ar), np.cos(ac), np.cos(ac)], axis=1)
    S = np.concatenate([np.sin(ar), np.sin(ar), np.sin(ac), np.sin(ac)], axis=1)
    return np.ascontiguousarray(np.concatenate([C, S], axis=1), dtype=np.float32)


def _na_tables():
    DR = np.zeros((5, 128, 5, 128), np.int64)
    DC = np.zeros((5, 128, 5, 128), np.int64)
    M = np.zeros((5, 128, 5, 128), np.float32)
    pp = np.arange(128)
    for cls, tq in enumerate((0, 1, 2, 30, 31)):
        k0 = min(max(tq - 2, 0), 27)
        for j in range(5):
            kt = k0 + j
            kr = (2 * kt + pp // 64)[:, None]
            kc = (pp % 64)[:, None]
            qr = (2 * tq + pp // 64)[None, :]
            qc = (pp % 64)[None, :]
            r0 = np.clip(qr - 4, 0, 56)
            c0 = np.clip(qc - 8, 0, 48)
            valid = (kr >= r0) & (kr < r0 + 8) & (kc >= c0) & (kc < c0 + 16)
            DR[cls, :, j, :] = np.clip(kr - qr + 7, 0, 14)
            DC[cls, :, j, :] = np.clip(kc - qc, -15, 15) + 15
            M[cls, :, j, :] = valid
    return DR, DC, M


def _na_bias_layout(rpb):
    DR, DC, M = _na_tables()
    g = rpb[:, :, DR, DC]
    g = np.transpose(g, (0, 1, 3, 2, 4, 5))
    L = rpb.shape[0]
    return (np.ascontiguousarray(g.reshape(L, 8, 128, 3200), dtype=np.float32),
            np.ascontiguousarray(np.transpose(M, (1, 0, 2, 3)).reshape(128, 3200), dtype=np.float32))


def _wlay(w, c):
    L, E, K, N = w.shape
    return np.ascontiguousarray(w.reshape(L, E, c, 128, N).transpose(0, 1, 3, 2, 4)).reshape(L, E * 128, c * N)


def _run(inputs, batches, cfg=None):
    f32 = lambda a: np.ascontiguousarray(np.asarray(a), dtype=np.float32)
    g = {k: f32(v) for k, v in inputs.items()}
    key = str(sorted((cfg or {}).items()))
    if key not in _NC_CACHE:
        _NC_CACHE[key] = build_program(dict(cfg or {}))
    nc = _NC_CACHE[key]
    nab, nam = _na_bias_layout(g["na_rpb"])
    shared = {
        "ada_w": g["ada_w"], "ada_b": g["ada_b"], "w_in": g["w_in"],
        "ret_decay": np.ascontiguousarray(np.concatenate([g["ret_decay_fwd"], g["ret_decay_bwd"]], axis=1)),
        "ret_gn_w": g["ret_gn_w"], "rope": _rope_table(), "na_bias": nab, "na_mask": nam,
        "w_o_na": g["w_o_na"], "w_fourier": g["w_fourier"], "w_o_ret": g["w_o_ret"], "w_out": g["w_out"],
        "ln_mix_w": g["ln_mix_w"], "ln_mix_b": g["ln_mix_b"], "router_w": g["router_w"], "router_bias": g["router_bias"],
        "sh_w_gate": g["sh_w_gate"], "sh_w_up": g["sh_w_up"], "sh_w_down": g["sh_w_down"],
        "ln_ffn_w": g["ln_ffn_w"], "ln_ffn_b": g["ln_ffn_b"],
    }
    for nm, c in (("exp_w_gate", 8), ("exp_w_up", 8), ("exp_w_down", 2)):
        wl = _wlay(g[nm], c)
        for i in range(wl.shape[0]):
            shared[f"{nm}{i}"] = wl[i]
    in_maps = []
    for b in batches:
        m = dict(shared)
        m["x"] = g["x"][b]
        m["ctx"] = g["ctx"][b]
        m["cvec"] = np.ascontiguousarray(np.stack([g["c"][b], g["c_ctx"]]))
        in_maps.append(m)
    res = run_bass_kernel_spmd(nc, in_maps, core_ids=list(range(len(batches))))
    return np.stack([np.asarray(res.results[i]["out"], dtype=np.float32) for i in range(len(batches))], axis=0)


def kernel(**inputs):
    return _run(inputs, list(range(8)))
```

```python
import numpy as np
import concourse.bass as bass
import concourse.mybir as mybir
from concourse.bass_utils import run_bass_kernel_spmd

F32 = mybir.dt.float32
BF16 = mybir.dt.bfloat16
I32 = mybir.dt.int32
AF = mybir.ActivationFunctionType
ALU = mybir.AluOpType
AX = mybir.AxisListType

D = 1024
SEQ = 4096
CTX = 256
NT = SEQ + CTX
NTILE = NT // 128
DEPTH = 2
INW = 8192
LN_EPS = 1e-6
DN_ALPHA = (2.0 * DEPTH) ** 0.25


class P:
    def __init__(self, nc):
        self.nc = nc
        self.ops = {k: [] for k in ("pe", "act", "dve", "pool", "sp")}
        self.cnt = {k: 0 for k in self.ops}
        self.sem = {}
        self.waited = {k: {} for k in self.ops}
        self.last_w = {}
        self.readers = {}
        self.dma_sems = {"sp": [], "pool": []}
        self.dma_rr = {"sp": 0, "pool": 0}
        self.dma_val = {}
        self.dma_last = {}
        self.final_tokens = []
        self.pending = {k: [] for k in self.ops}

    def setup_sems(self, stack):
        for k in self.ops:
            self.sem[k] = stack.enter_context(self.nc.semaphore("e_" + k))
        for q in ("sp", "pool"):
            for i in range(12):
                s = stack.enter_context(self.nc.semaphore(f"d_{q}{i}"))
                self.dma_sems[q].append(s)
                self.dma_val[id(s)] = 0
                self.dma_last[id(s)] = None

    def _deps(self, eng, reads, writes):
        toks = []
        for r in reads:
            toks.extend(self._lw(r))
        for w in writes:
            toks.extend(self._lw(w))
            toks.extend(self.readers.get(w, ()))
        need = {}
        for (s, v, own) in toks:
            if own == eng and eng == "pe":
                continue
            key = id(s)
            if self.waited[eng].get(key, 0) >= v:
                continue
            if key not in need or need[key][1] < v:
                need[key] = (s, v)
        for key, (s, v) in need.items():
            self.waited[eng][key] = v
        return list(need.values())

    def _lw(self, key):
        t = self.last_w.get(key)
        if t is None:
            return []
        return t if isinstance(t, list) else [t]

    def _commit(self, tok, reads, writes):
        is_dma = tok[2].startswith("dma_")
        for w in writes:
            prev = self._lw(w)
            if is_dma and prev and all(t[2].startswith("dma_") for t in prev) and not self.readers.get(w):
                self.last_w[w] = prev + [tok]
            else:
                self.last_w[w] = [tok]
            self.readers[w] = []
        for r in reads:
            self.readers.setdefault(r, []).append(tok)

    def barrier(self):
        for eng in self.ops:
            w = []
            for k in self.ops:
                if k != eng and self.cnt[k] > 0 and self.waited[eng].get(id(self.sem[k]), 0) < self.cnt[k]:
                    w.append((self.sem[k], self.cnt[k]))
                    self.waited[eng][id(self.sem[k])] = self.cnt[k]
            for q in ("sp", "pool"):
                for ds in self.dma_sems[q]:
                    v = self.dma_val[id(ds)]
                    if v > 0 and self.waited[eng].get(id(ds), 0) < v:
                        w.append((ds, v))
                        self.waited[eng][id(ds)] = v
            self.pending[eng].extend(w)
        self.last_w = {}
        self.readers = {}

    def op(self, eng, fn, reads=(), writes=()):
        waits = self.pending[eng] + self._deps(eng, reads, writes)
        self.pending[eng] = []
        self.cnt[eng] += 1
        v = self.cnt[eng]
        s = self.sem[eng]
        self.ops[eng].append((waits, fn, s, 1))
        tok = (s, v, eng)
        self._commit(tok, reads, writes)
        return tok

    def dma(self, q, fn, reads=(), writes=(), inc=16):
        waits = self.pending[q] + self._deps(q, reads, writes)
        self.pending[q] = []
        i = self.dma_rr[q]
        self.dma_rr[q] = (i + 1) % len(self.dma_sems[q])
        s = self.dma_sems[q][i]
        prev = self.dma_val[id(s)]
        if prev > 0 and self.waited[q].get(id(s), 0) < prev:
            waits.append((s, prev))
            self.waited[q][id(s)] = prev
        self.dma_val[id(s)] = prev + inc
        v = prev + inc
        self.ops[q].append((waits, fn, s, inc))
        tok = (s, v, "dma_" + q)
        self._commit(tok, reads, writes)
        return tok

    def emit(self, block):
        def run(eng_name):
            def body(e):
                for waits, fn, s, inc in self.ops[eng_name]:
                    for (ws, wv) in waits:
                        e.wait_ge(ws, wv)
                    fn(e).then_inc(s, inc)
                if eng_name == "sp":
                    for q in ("sp", "pool"):
                        for ds in self.dma_sems[q]:
                            v = self.dma_val[id(ds)]
                            if v > 0:
                                e.wait_ge(ds, v)
                    for k in self.ops:
                        if k != "sp" and self.cnt[k] > 0:
                            e.wait_ge(self.sem[k], self.cnt[k])
            return body
        block.tensor(run("pe"))
        block.scalar(run("act"))
        block.vector(run("dve"))
        block.gpsimd(run("pool"))
        block.sync(run("sp"))


def build_program(cfg):
    from contextlib import ExitStack
    nc = bass.Bass("TRN2", target_bir_lowering=False)
    p = P(nc)
    stages = cfg.get("stages", 99)
    dbg = cfg.get("debug", ())

    def din(name, shape, dt=F32):
        return nc.dram_tensor(name, list(shape), dt, kind="ExternalInput")

    x_in = din("x", [SEQ, D])
    ctx_in = din("ctx", [CTX, D])
    cvec = din("cvec", [2, D])
    WD = cfg.get("wdepth", DEPTH)
    ada_w = din("ada_w", [WD, D, 6 * D])
    ada_b = din("ada_b", [WD, 6 * D])
    w_in = din("w_in", [WD, D, INW])
    ret_decay = din("ret_decay", [WD, 8])
    ret_gn_w = din("ret_gn_w", [WD, 1024])
    rope_t = din("rope", [SEQ, 256])
    na_bias = din("na_bias", [WD, 8, 128, 3200])
    na_mask = din("na_mask", [128, 3200])
    w_o_na = din("w_o_na", [WD, 512, D])
    w_fourier = din("w_fourier", [WD, 512, D])
    w_o_ret = din("w_o_ret", [WD, 1024, D])
    w_out = din("w_out", [WD, D, D])
    ln_mix_w = din("ln_mix_w", [WD, D])
    ln_mix_b = din("ln_mix_b", [WD, D])
    router_w = din("router_w", [WD, D, 256])
    router_bias = din("router_bias", [WD, 256])
    exp_w_gate = [din(f"exp_w_gate{i}", [256 * 128, 2048]) for i in range(WD)]
    exp_w_up = [din(f"exp_w_up{i}", [256 * 128, 2048]) for i in range(WD)]
    exp_w_down = [din(f"exp_w_down{i}", [256 * 128, 2048]) for i in range(WD)]
    sh_w_gate = din("sh_w_gate", [WD, D, 256])
    sh_w_up = din("sh_w_up", [WD, D, 256])
    sh_w_down = din("sh_w_down", [WD, 256, D])
    ln_ffn_w = din("ln_ffn_w", [WD, D])
    ln_ffn_b = din("ln_ffn_b", [WD, D])
    out_t = nc.dram_tensor("out", [SEQ, D], F32, kind="ExternalOutput")

    def scratch(name, shape, dt=BF16):
        kind = "ExternalOutput" if name in dbg else "Internal"
        return nc.dram_tensor(name, list(shape), dt, kind=kind)

    QAT = scratch("QAT", [512, NT])
    KAT = scratch("KAT", [512, NT])
    UBT = scratch("UBT", [512, NT])
    VA = scratch("VA", [NT, 512])
    QR = scratch("QR", [NT, 512])
    KR = scratch("KR", [NT, 512])
    VR = scratch("VR", [NT, 1024])
    GR = scratch("GR", [NT, 1024])
    GL = scratch("GL", [NT, 3072])
    MODS = scratch("MODS", [2, 6 * D], F32)
    XRES = scratch("XRES", [NT, D], F32)

    with ExitStack() as st:
        p.setup_sems(st)

        def sb(name, shape, dt):
            return st.enter_context(nc.sbuf_tensor(name, list(shape), dt))

        def ps(name, shape, dt=F32):
            return st.enter_context(nc.psum_tensor(name, list(shape), dt))


        ident = sb("ident", [128, 128], BF16)
        ones_f = sb("ones_f", [128, 128], F32)
        mods = sb("mods", [128, 2, 6 * D], F32)
        ARENA_N = 79 * 1024
        arena = sb("arena", [128, ARENA_N], BF16)
        pbig = [ps(f"pbig{i}", [128, 1024], F32) for i in range(2)]
        pb45 = [ps(f"pb{i}", [128, 512], F32) for i in (4, 5)]
        pbank = [pbig[0][:, 0:512], pbig[0][:, 512:1024], pbig[1][:, 0:512], pbig[1][:, 512:1024], pb45[0][:, :], pb45[1][:, :]]
        ptr = [ps(f"ptr{i}", [128, 1024], BF16) for i in range(2)]
        aoff = [0]

        def areset():
            p.barrier()
            aoff[0] = 0

        def alloc(shape, dt, parts=128):
            n = 1
            for d_ in shape:
                n *= d_
            n16 = n * (2 if dt in (F32, I32) else 1)
            assert aoff[0] + n16 <= ARENA_N, (aoff[0], n16)
            v = arena[0:parts, aoff[0]:aoff[0] + n16]
            aoff[0] += (n16 + 31) // 32 * 32
            if dt != BF16:
                v = v.bitcast(dt)
            if len(shape) == 2:
                v = v.rearrange("q (a b) -> q a b", a=shape[0])
            elif len(shape) == 3:
                v = v.rearrange("q (a b c) -> q a b c", a=shape[0], b=shape[1])
            return v

        p.op("pool", lambda e: e.memset(ones_f[:], 1.0), writes=["ones_f"])
        p.op("pool", lambda e: e.memset(ident[:], 1.0), writes=["ident"])
        p.op("pool", lambda e: e.affine_select(out=ident[:], in_=ident[:], pattern=[[-1, 128]],
                                              compare_op=ALU.is_equal, fill=0.0, base=0,
                                              channel_multiplier=1),
             reads=["ident"], writes=["ident"])

        ev = [0]

        def copy_any(out_ap, in_ap, reads, writes):
            ev[0] += 1
            if ev[0] % 2 == 0:
                p.op("act", lambda e: e.copy(out=out_ap, in_=in_ap), reads=reads, writes=writes)
            else:
                p.op("dve", lambda e: e.tensor_copy(out=out_ap, in_=in_ap), reads=reads, writes=writes)

        bank_rr = [0]

        def next_bank():
            bank_rr[0] = (bank_rr[0] + 1) % 6
            i = bank_rr[0]
            return pbank[i], f"pb{i}"

        def phase0(l):
            areset()
            csb = alloc([2, 8], F32)
            crep = alloc([2, 8, 128], F32)
            adaw = [alloc([8, 512], F32) for _ in range(2)]
            bia = [alloc([512], F32, parts=1) for _ in range(2)]
            for j in range(2):
                p.dma("sp", lambda e, j=j: e.dma_start(out=csb[:, j, :], in_=cvec.ap()[j, :].rearrange("(c q) -> q c", q=128),
                                                       allow_slow_non_contiguous=True), writes=["csb"])
            p.op("act", lambda e: e.activation(out=csb, in_=csb, func=AF.Silu), reads=["csb"], writes=["csb"])
            for j in range(2):
                for k in range(8):
                    p.op("dve", lambda e, j=j, k=k: e.tensor_scalar_mul(
                        out=crep[:, j, k, :], in0=ones_f[:], scalar1=csb[:, j, k:k + 1]),
                        reads=["csb", "ones_f"], writes=["crep"])
            for nb in range(12):
                wb = adaw[nb % 2]
                bb = bia[nb % 2]
                p.dma("sp", lambda e, nb=nb, wb=wb: e.dma_start(
                    out=wb, in_=ada_w.ap()[l, :, nb * 512:(nb + 1) * 512].rearrange("(c q) n -> q c n", q=128)),
                    writes=[f"adaw{nb % 2}"])
                p.dma("sp", lambda e, nb=nb, bb=bb: e.dma_start(
                    out=bb, in_=ada_b.ap()[l:l + 1, nb * 512:(nb + 1) * 512]), writes=[f"bia{nb % 2}"])
                for j in range(2):
                    bank, bkey = next_bank()
                    for k in range(8):
                        p.op("pe", lambda e, j=j, k=k, wb=wb, bank=bank: e.matmul(
                            bank, lhsT=crep[:, j, k, :], rhs=wb[:, k, :], start=(k == 0), stop=False),
                            reads=["crep", f"adaw{nb % 2}"], writes=[bkey])
                    p.op("pe", lambda e, bb=bb, bank=bank: e.matmul(
                        bank, lhsT=ones_f[0:1, :], rhs=bb, start=False, stop=True),
                        reads=["ones_f", f"bia{nb % 2}"], writes=[bkey])
                    is_scale = (nb // 2) in (1, 4)
                    if is_scale:
                        p.op("dve", lambda e, j=j, nb=nb, bank=bank: e.tensor_scalar_add(
                            out=mods[:, j, nb * 512:(nb + 1) * 512], in0=bank, scalar1=1.0),
                            reads=[bkey], writes=["mods"])
                    else:
                        p.op("dve", lambda e, j=j, nb=nb, bank=bank: e.tensor_copy(
                            out=mods[:, j, nb * 512:(nb + 1) * 512], in_=bank),
                            reads=[bkey], writes=["mods"])
            if "MODS" in dbg:
                p.dma("sp", lambda e: e.dma_start(out=MODS.ap(), in_=mods[0:1, :, :]), reads=["mods"], writes=["MODS"])

        def ln_tile(xt_ap, stats_ap, mv_ap, rstd_ap, kx, ks):
            for c in range(2):
                p.op("dve", lambda e, c=c: e.bn_stats(out=stats_ap[:, c, :], in_=xt_ap[:, c * 512:(c + 1) * 512]),
                     reads=[kx], writes=[ks])
            p.op("dve", lambda e: e.bn_aggr(out=mv_ap, in_=stats_ap), reads=[ks], writes=[ks + "mv"])
            p.op("dve", lambda e: e.tensor_scalar_add(out=rstd_ap, in0=mv_ap[:, 1:2], scalar1=LN_EPS),
                 reads=[ks + "mv"], writes=[ks + "r"])
            p.op("act", lambda e: e.sqrt(out=rstd_ap, in_=rstd_ap), reads=[ks + "r"], writes=[ks + "r"])
            p.op("dve", lambda e: e.reciprocal(out=rstd_ap, in_=rstd_ap), reads=[ks + "r"], writes=[ks + "r"])
            p.op("dve", lambda e: e.tensor_scalar(out=xt_ap, in0=xt_ap, scalar1=mv_ap[:, 0:1],
                                                  scalar2=rstd_ap[:, 0:1], op0=ALU.subtract, op1=ALU.mult),
                 reads=[kx, ks + "mv", ks + "r"], writes=[kx])

        def phase1(l, xres):
            areset()
            hT = alloc([8, NT], BF16)
            wbuf = [alloc([8, 1024], BF16) for _ in range(2)]
            xt = [alloc([1024], F32) for _ in range(2)]
            xn = [alloc([1024], BF16) for _ in range(2)]
            stats = [alloc([2, 6], F32) for _ in range(2)]
            mv = [alloc([2], F32) for _ in range(2)]
            rstd = [alloc([1], F32) for _ in range(2)]
            stg = [alloc([1024], BF16) for _ in range(4)]

            def load_w(g):
                wb = wbuf[g % 2]
                p.dma("pool", lambda e: e.dma_start(
                    out=wb, in_=w_in.ap()[l, :, g * 1024:(g + 1) * 1024].rearrange("(c q) n -> q c n", q=128)),
                    writes=[f"wbuf{g % 2}"])

            if stages >= 2:
                load_w(0)
            for t in range(NTILE):
                b = t % 2
                j = 0 if t < 32 else 1
                src = xres[t * 128:(t + 1) * 128, :]
                p.dma("sp", lambda e, b=b, src=src: e.dma_start(out=xt[b], in_=src), reads=["xres"], writes=[f"xt{b}"])
                if not cfg.get("noln"):
                    ln_tile(xt[b], stats[b], mv[b], rstd[b], f"xt{b}", f"st{b}")
                p.op("dve", lambda e, b=b, j=j: e.tensor_tensor(out=xt[b], in0=xt[b], in1=mods[:, j, D:2 * D], op=ALU.mult),
                     reads=[f"xt{b}", "mods"], writes=[f"xt{b}"])
                p.op("dve", lambda e, b=b, j=j: e.tensor_tensor(out=xn[b], in0=xt[b], in1=mods[:, j, 0:D], op=ALU.add),
                     reads=[f"xt{b}", "mods"], writes=[f"xn{b}"])
                bv = ptr[b]
                if cfg.get("notr"):
                    continue
                for k in range(8):
                    p.op("pe", lambda e, k=k, b=b, bv=bv: e.transpose(
                        bv[:, k * 128:(k + 1) * 128], xn[b][:, k * 128:(k + 1) * 128], ident[:]),
                        reads=[f"xn{b}", "ident"], writes=[f"ptr{b}"])
                if cfg.get("nocp"):
                    continue
                copy_any(hT[:, :, t * 128:(t + 1) * 128], bv[:, :].rearrange("q (k n) -> q k n", k=8),
                         [f"ptr{b}"], [f"hT{t}a", f"hT{t}b"])

            stg_rr = [0]

            def next_stg():
                stg_rr[0] = (stg_rr[0] + 1) % 4
                return stg[stg_rr[0]], f"stg{stg_rr[0]}"

            def gemm_tm(g, col0, ncols, dst, dst_col0):
                wb = wbuf[g % 2]
                for t in range(NTILE):
                    sg, sk = next_stg()
                    for cb in range(ncols // 512):
                        bank, bkey = next_bank()
                        for k in range(8):
                            p.op("pe", lambda e, k=k, cb=cb, bank=bank, t=t: e.matmul(
                                bank, lhsT=hT[:, k, t * 128:(t + 1) * 128],
                                rhs=wb[:, k, col0 + cb * 512:col0 + (cb + 1) * 512], start=(k == 0), stop=(k == 7)),
                                reads=[f"hT{t}a", f"hT{t}b", f"wbuf{g % 2}"], writes=[bkey])
                        copy_any(sg[:, cb * 512:(cb + 1) * 512], bank, [bkey], [sk])
                    p.dma("sp", lambda e, sg=sg, t=t: e.dma_start(
                        out=dst.ap()[t * 128:(t + 1) * 128, dst_col0:dst_col0 + ncols], in_=sg[:, 0:ncols]),
                        reads=[sk], writes=[dst.name])

            def gemm_fm(g, col0, ncols, dst):
                wb = wbuf[g % 2]
                for fb in range(ncols // 128):
                    for tg in range(0, NT, 1024):
                        ntok = min(1024, NT - tg)
                        sg, sk = next_stg()
                        for tb in range(0, ntok, 512):
                            nn = min(512, ntok - tb)
                            bank, bkey = next_bank()
                            rk = []
                            for tt in range((tg + tb) // 128, (tg + tb + nn) // 128):
                                rk += [f"hT{tt}a", f"hT{tt}b"]
                            for k in range(8):
                                p.op("pe", lambda e, k=k, bank=bank, tb=tb, nn=nn, tg=tg, fb=fb: e.matmul(
                                    bank[:, 0:nn], lhsT=wb[:, k, col0 + fb * 128:col0 + (fb + 1) * 128],
                                    rhs=hT[:, k, tg + tb:tg + tb + nn], start=(k == 0), stop=(k == 7)),
                                    reads=rk + [f"wbuf{g % 2}"], writes=[bkey])
                            copy_any(sg[:, tb:tb + nn], bank[:, 0:nn], [bkey], [sk])
                        p.dma("sp", lambda e, sg=sg, tg=tg, ntok=ntok, fb=fb: e.dma_start(
                            out=dst.ap()[fb * 128:(fb + 1) * 128, tg:tg + ntok], in_=sg[:, 0:ntok]),
                            reads=[sk], writes=[dst.name])

            if stages < 2:
                return
            plan = [
                [("fm", 0, 512, QAT, 0), ("fm", 512, 512, KAT, 0)],
                [("tm", 0, 512, VA, 0), ("fm", 512, 512, UBT, 0)],
                [("tm", 0, 512, QR, 0), ("tm", 512, 512, KR, 0)],
                [("tm", 0, 1024, VR, 0)],
                [("tm", 0, 1024, GR, 0)],
                [("tm", 0, 1024, GL, 0)],
                [("tm", 0, 1024, GL, 1024)],
                [("tm", 0, 1024, GL, 2048)],
            ]
            for g in range(8):
                if g + 1 < min(8, cfg.get("ngroups", 8)):
                    load_w(g + 1)
                if g >= cfg.get("ngroups", 8):
                    break
                for (mode, c0, ncol, dst, dc0) in plan[g]:
                    if mode == "tm":
                        gemm_tm(g, c0, ncol, dst, dc0)
                    else:
                        gemm_fm(g, c0, ncol, dst)


        YFNT = scratch("YFNT", [512, NT])
        MAGIC = 12582912.0

        def gen_cs(dst_c, dst_s, cols, nval, nvs, nmod, tmps, tk, dkeys):
            y, r, t_ = tmps
            p.op("dve", lambda e: e.tensor_scalar(out=y, in0=cols, scalar1=nvs, scalar2=MAGIC, op0=ALU.mult, op1=ALU.add),
                 reads=["fconst"], writes=[tk + "y"])
            p.op("dve", lambda e: e.tensor_scalar(out=r, in0=y, scalar1=MAGIC, scalar2=float(nmod), op0=ALU.subtract, op1=ALU.mult),
                 reads=[tk + "y"], writes=[tk + "r"])
            p.op("dve", lambda e: e.scalar_tensor_tensor(out=t_, in0=cols, scalar=nval, in1=r, op0=ALU.mult, op1=ALU.subtract),
                 reads=[tk + "r", "fconst"], writes=[tk + "t"])
            p.op("dve", lambda e: e.scalar_tensor_tensor(out=y, in0=t_, scalar=-1.0, in1=t_, op0=ALU.mult, op1=ALU.max),
                 reads=[tk + "t"], writes=[tk + "y"])
            p.op("act", lambda e: e.activation(out=dst_s, in_=t_, func=AF.Sin, scale=float(2 * np.pi / nmod)),
                 reads=[tk + "t"], writes=[dkeys[1]])
            p.op("act", lambda e: e.activation(out=dst_c, in_=y, func=AF.Sin, scale=float(-2 * np.pi / nmod), bias=float(np.pi / 2)),
                 reads=[tk + "y"], writes=[dkeys[0]])

        def fourier(N, tok0):
            areset()
            nch = N // 128
            W = min(512, N)
            ubt = alloc([4, N], BF16)
            ucs = alloc([nch, 4, 2 * 128], BF16)
            cs128 = alloc([256], BF16)
            colf = alloc([N], F32)
            nval = alloc([nch], F32)
            nvs = alloc([nch], F32)
            nvs128 = alloc([1], F32)
            tmps = [[alloc([512], F32) for _ in range(3)] for _ in range(2)]
            cblk = [alloc([512], BF16) for _ in range(2)]
            sblk = [alloc([512], BF16) for _ in range(2)]
            fstg = [alloc([512], BF16) for _ in range(4)]
            coli = alloc([N], I32)
            nvi = alloc([nch], I32)
            p.op("pool", lambda e: e.iota(coli, pattern=[[1, N]], base=0, channel_multiplier=0), writes=["coli"])
            p.op("pool", lambda e: e.iota(nvi, pattern=[[128, nch]], base=0, channel_multiplier=1), writes=["nvi"])
            p.op("dve", lambda e: e.tensor_copy(out=colf, in_=coli), reads=["coli"], writes=["fconst"])
            p.op("dve", lambda e: e.tensor_copy(out=nval, in_=nvi), reads=["nvi"], writes=["fconst"])
            p.op("dve", lambda e: e.tensor_scalar_mul(out=nvs, in0=nval, scalar1=1.0 / N), reads=["fconst"], writes=["fconst"])
            p.op("dve", lambda e: e.tensor_scalar_mul(out=nvs128, in0=nval[:, 0:1], scalar1=1.0 / 128), reads=["fconst"], writes=["fconst"])
            for g in range(4):
                p.dma("sp", lambda e, g=g: e.dma_start(out=ubt[:, g, :], in_=UBT.ap()[g * 128:(g + 1) * 128, tok0:tok0 + N]),
                      reads=["UBT"], writes=[f"ubt{g}"])
            gen_cs(cs128[:, 0:128], cs128[:, 128:256], colf[:, 0:128], nval[:, 0:1], nvs128[:, 0:1], 128,
                   [tm[:, 0:128] for tm in tmps[0]], "gt0", ["cs128", "cs128"])
            for i in range(nch):
                for gp in range(2):
                    bank, bkey = next_bank()
                    for gg in range(2):
                        g = gp * 2 + gg
                        p.op("pe", lambda e, g=g, gg=gg, i=i, bank=bank: e.matmul(
                            bank[:, gg * 256:(gg + 1) * 256], lhsT=ubt[:, g, i * 128:(i + 1) * 128], rhs=cs128,
                            start=True, stop=True), reads=[f"ubt{g}", "cs128"], writes=[bkey])
                    bv = bank.rearrange("q (g s c) -> q g s c", g=2, s=2)
                    ov = ucs[:, i, gp * 2:gp * 2 + 2, :].rearrange("q g (s c) -> q g s c", s=2)
                    p.op("dve", lambda e, bv=bv, ov=ov: e.tensor_copy(out=ov[:, :, 0, :], in_=bv[:, :, 0, :]),
                         reads=[bkey], writes=[f"ucs{i}c{gp}"])
                    p.op("dve", lambda e, bv=bv, ov=ov: e.tensor_scalar_mul(out=ov[:, :, 1, :], in0=bv[:, :, 1, :], scalar1=-1.0),
                         reads=[bkey], writes=[f"ucs{i}s{gp}"])
            scale = float(1.0 / np.sqrt(N * 128.0))
            blk = 0
            for mg in range(N // W):
                for i in range(nch):
                    b = blk % 2
                    blk += 1
                    gen_cs(cblk[b][:, 0:W], sblk[b][:, 0:W], colf[:, mg * W:(mg + 1) * W], nval[:, i:i + 1], nvs[:, i:i + 1], N,
                           [tm[:, 0:W] for tm in tmps[b]], f"gt{b}", [f"cblk{b}", f"sblk{b}"])
                    for g in range(4):
                        rk = [f"ucs{i}c{g // 2}", f"ucs{i}s{g // 2}", f"cblk{b}", f"sblk{b}"]
                        p.op("pe", lambda e, g=g, i=i, b=b: e.matmul(
                            pbank[g][:, 0:W], lhsT=ucs[:, i, g, 0:128], rhs=cblk[b][:, 0:W], start=(i == 0), stop=False),
                            reads=rk, writes=[f"pb{g}"])
                        p.op("pe", lambda e, g=g, i=i, b=b: e.matmul(
                            pbank[g][:, 0:W], lhsT=ucs[:, i, g, 128:256], rhs=sblk[b][:, 0:W], start=False, stop=(i == nch - 1)),
                            reads=rk, writes=[f"pb{g}"])
                for g in range(4):
                    if g % 2 == 0:
                        p.op("act", lambda e, g=g: e.mul(out=fstg[g][:, 0:W], in_=pbank[g][:, 0:W], mul=scale),
                             reads=[f"pb{g}"], writes=[f"fstg{g}"])
                    else:
                        p.op("dve", lambda e, g=g: e.tensor_scalar_mul(out=fstg[g][:, 0:W], in0=pbank[g][:, 0:W], scalar1=scale),
                             reads=[f"pb{g}"], writes=[f"fstg{g}"])
                    p.dma("sp", lambda e, g=g, mg=mg: e.dma_start(
                        out=YFNT.ap()[g * 128:(g + 1) * 128, tok0 + mg * W:tok0 + (mg + 1) * W], in_=fstg[g][:, 0:W]),
                        reads=[f"fstg{g}"], writes=["YFNT"])


        YRETT = scratch("YRETT", [1024, NT])
        QS = 128.0 ** -0.5
        LNQS = float(np.log(QS))
        GN_EPS = 1e-5

        def retention(l):
            areset()
            dec = alloc([8], F32)
            lg = alloc([8], F32)
            gC = alloc([8], F32)
            diffi = alloc([128], I32)
            diff = alloc([128], F32)
            rp = alloc([128], F32)
            rn = alloc([128], F32)
            ef = alloc([128], F32)
            eb = alloc([128], F32)
            pci = alloc([2], I32)
            pc = alloc([2], F32)
            cri = alloc([2, 128], I32)
            cr = alloc([2, 128], F32)
            dmask = alloc([4, 128], F32)
            qdf = alloc([4, 128], F32)
            qdb = alloc([4, 128], F32)
            kdf = alloc([4], F32)
            kdb = alloc([4], F32)
            gnw = alloc([1024], F32)
            Sf = alloc([4, 256], F32)
            Sb = alloc([4, 256], F32)
            Sf16 = alloc([4, 256], BF16)
            Sbprev = alloc([32, 1024], BF16)
            qt = [alloc([512], BF16) for _ in range(2)]
            kt = [alloc([512], BF16) for _ in range(2)]
            vt = [alloc([1024], BF16) for _ in range(2)]
            gt = [alloc([1024], BF16) for _ in range(2)]
            rt = [alloc([256], F32) for _ in range(2)]
            t1 = alloc([512], F32)
            t2 = alloc([512], F32)
            q16 = alloc([512], BF16)
            k16 = alloc([512], BF16)
            ks16 = alloc([512], BF16)
            qkT = alloc([8, 128], BF16)
            PT = alloc([4, 128], BF16)
            qfT = alloc([4, 128], BF16)
            qbT = alloc([4, 128], BF16)
            rstat = alloc([4, 6], F32)
            rmv = alloc([4, 2], F32)
            rr = alloc([4], F32)
            yn = alloc([1024], F32)
            sg = alloc([1024], F32)
            y16 = alloc([1024], BF16)
            yT = alloc([8, 128], BF16)

            p.dma("sp", lambda e: e.dma_start(out=dec, in_=ret_decay.ap()[l, :].partition_broadcast(128)), writes=["dec"])
            p.dma("sp", lambda e: e.dma_start(out=gnw, in_=ret_gn_w.ap()[l, :].partition_broadcast(128)), writes=["gnw"])
            p.op("act", lambda e: e.activation(out=lg, in_=dec, func=AF.Exp, scale=-1.0), reads=["dec"], writes=["lg"])
            p.op("act", lambda e: e.activation(out=lg, in_=lg, func=AF.Ln, bias=1.0), reads=["lg"], writes=["lg"])
            p.op("dve", lambda e: e.tensor_scalar_mul(out=lg, in0=lg, scalar1=-1.0), reads=["lg"], writes=["lg"])
            p.op("act", lambda e: e.activation(out=gC, in_=lg, func=AF.Exp, scale=128.0), reads=["lg"], writes=["gC"])
            p.op("pool", lambda e: e.iota(diffi, pattern=[[1, 128]], base=0, channel_multiplier=-1), writes=["diffi"])
            p.op("pool", lambda e: e.iota(pci[:, 0:1], pattern=[[0, 1]], base=127, channel_multiplier=-1), writes=["pci"])
            p.op("pool", lambda e: e.iota(pci[:, 1:2], pattern=[[0, 1]], base=0, channel_multiplier=1), reads=["pci"], writes=["pci"])
            p.op("pool", lambda e: e.iota(cri[:, 0, :], pattern=[[1, 128]], base=1, channel_multiplier=0), writes=["cri"])
            p.op("pool", lambda e: e.iota(cri[:, 1, :], pattern=[[-1, 128]], base=128, channel_multiplier=0), reads=["cri"], writes=["cri"])
            p.op("dve", lambda e: e.tensor_copy(out=diff, in_=diffi), reads=["diffi"], writes=["diff"])
            p.op("dve", lambda e: e.tensor_copy(out=pc, in_=pci), reads=["pci"], writes=["pc"])
            p.op("dve", lambda e: e.tensor_copy(out=cr, in_=cri), reads=["cri"], writes=["cr"])
            p.op("dve", lambda e: e.tensor_scalar_max(out=rp, in0=diff, scalar1=0.0), reads=["diff"], writes=["rp"])
            p.op("dve", lambda e: e.tensor_tensor(out=rn, in0=rp, in1=diff, op=ALU.subtract), reads=["rp", "diff"], writes=["rn"])
            for h in range(4):
                p.op("act", lambda e, h=h: e.activation(out=kdf[:, h:h + 1], in_=pc[:, 0:1], func=AF.Exp, scale=lg[:, h:h + 1]),
                     reads=["pc", "lg"], writes=["kdf"])
                p.op("act", lambda e, h=h: e.activation(out=kdb[:, h:h + 1], in_=pc[:, 1:2], func=AF.Exp, scale=lg[:, 4 + h:5 + h]),
                     reads=["pc", "lg"], writes=["kdb"])
                p.op("act", lambda e, h=h: e.activation(out=qdf[:, h, :], in_=cr[:, 0, :], func=AF.Exp, scale=lg[:, h:h + 1], bias=LNQS),
                     reads=["cr", "lg"], writes=["qdf"])
                p.op("act", lambda e, h=h: e.activation(out=qdb[:, h, :], in_=cr[:, 1, :], func=AF.Exp, scale=lg[:, 4 + h:5 + h], bias=LNQS),
                     reads=["cr", "lg"], writes=["qdb"])
                p.op("act", lambda e, h=h: e.activation(out=ef, in_=rp, func=AF.Exp, scale=lg[:, h:h + 1], bias=LNQS),
                     reads=["rp", "lg"], writes=["ef"])
                p.op("act", lambda e, h=h: e.activation(out=eb, in_=rn, func=AF.Exp, scale=lg[:, 4 + h:5 + h], bias=LNQS),
                     reads=["rn", "lg"], writes=["eb"])
                p.op("pool", lambda e: e.affine_select(out=ef, in_=ef, pattern=[[1, 128]], compare_op=ALU.is_ge, fill=0.0,
                                                      base=0, channel_multiplier=-1), reads=["ef"], writes=["ef"])
                p.op("pool", lambda e: e.affine_select(out=eb, in_=eb, pattern=[[-1, 128]], compare_op=ALU.is_gt, fill=0.0,
                                                      base=0, channel_multiplier=1), reads=["eb"], writes=["eb"])
                p.op("dve", lambda e, h=h: e.tensor_tensor(out=dmask[:, h, :], in0=ef, in1=eb, op=ALU.add),
                     reads=["ef", "eb"], writes=["dmask"])
            p.op("pool", lambda e: e.memset(Sf, 0.0), writes=["Sf"])
            p.op("pool", lambda e: e.memset(Sb, 0.0), writes=["Sb"])
            p.op("pool", lambda e: e.memset(Sf16, 0.0), writes=["Sf16"])

            pbS = pbank[0]
            pbO = [pbank[1], pbank[2]]
            pbK = [pbank[3], pbank[4]]

            def rope(src, dst, rtile, dk):
                sv = src.rearrange("q (h r u d) -> q (h r) u d", h=4, r=2, u=2)
                Cb = rtile[:, 0:128].unsqueeze(1).broadcast_to([128, 4, 128])
                Sv = rtile[:, 128:256].rearrange("q (r u d) -> q r u d", r=2, u=2)
                t1v = t1.rearrange("q (h x) -> q h x", h=4)
                t2v = t2.rearrange("q (h r u d) -> q h r u d", h=4, r=2, u=2)
                s5 = src.rearrange("q (h r u d) -> q h r u d", h=4, r=2, u=2)
                p.op("dve", lambda e: e.tensor_tensor(out=t1v, in0=src.rearrange("q (h x) -> q h x", h=4), in1=Cb, op=ALU.mult),
                     reads=[dk + "src", dk + "rt"], writes=["t1"])
                for u in range(2):
                    for r in range(2):
                        p.op("dve", lambda e, u=u, r=r: e.tensor_tensor(
                            out=t2v[:, :, r, u, :], in0=s5[:, :, r, 1 - u, :],
                            in1=Sv[:, r, u, :].unsqueeze(1).broadcast_to([128, 4, 32]), op=ALU.mult),
                            reads=[dk + "src", dk + "rt"], writes=["t2"])
                t1w = t1.rearrange("q (h r u d) -> q (h r) u d", h=4, r=2, u=2)
                t2w = t2.rearrange("q (h r u d) -> q (h r) u d", h=4, r=2, u=2)
                dw = dst.rearrange("q (h r u d) -> q (h r) u d", h=4, r=2, u=2)
                p.op("dve", lambda e: e.tensor_tensor(out=dw[:, :, 0, :], in0=t1w[:, :, 0, :], in1=t2w[:, :, 0, :], op=ALU.subtract),
                     reads=["t1", "t2"], writes=[dk])
                p.op("dve", lambda e: e.tensor_tensor(out=dw[:, :, 1, :], in0=t1w[:, :, 1, :], in1=t2w[:, :, 1, :], op=ALU.add),
                     reads=["t1", "t2"], writes=[dk])

            def load_k_v(n, tok, b, use_rope, need_q):
                p.dma("sp", lambda e: e.dma_start(out=kt[b], in_=KR.ap()[tok:tok + 128, :]), reads=["KR"], writes=[f"kt{b}"])
                p.dma("sp", lambda e: e.dma_start(out=vt[b], in_=VR.ap()[tok:tok + 128, :]), reads=["VR"], writes=[f"vt{b}"])
                if use_rope:
                    p.dma("sp", lambda e: e.dma_start(out=rt[b], in_=rope_t.ap()[tok:tok + 128, :]), writes=[f"rt{b}"])
                if need_q:
                    p.dma("sp", lambda e: e.dma_start(out=qt[b], in_=QR.ap()[tok:tok + 128, :]), reads=["QR"], writes=[f"qt{b}"])
                    p.dma("sp", lambda e: e.dma_start(out=gt[b], in_=GR.ap()[tok:tok + 128, :]), reads=["GR"], writes=[f"gt{b}"])

            def prep_k(b, use_rope):
                if use_rope:
                    p.last_w["k16src"] = p.last_w.get(f"kt{b}")
                    p.last_w["k16rt"] = p.last_w.get(f"rt{b}")
                    rope(kt[b], k16, rt[b], "k16")
                    p.readers.setdefault(f"kt{b}", []).extend(p._lw("k16"))
                    p.readers.setdefault(f"rt{b}", []).extend(p._lw("k16"))
                else:
                    p.op("dve", lambda e: e.tensor_copy(out=k16, in_=kt[b]), reads=[f"kt{b}"], writes=["k16"])

            def prep_q(b, use_rope):
                if use_rope:
                    p.last_w["q16src"] = p.last_w.get(f"qt{b}")
                    p.last_w["q16rt"] = p.last_w.get(f"rt{b}")
                    rope(qt[b], q16, rt[b], "q16")
                    p.readers.setdefault(f"qt{b}", []).extend(p._lw("q16"))
                    p.readers.setdefault(f"rt{b}", []).extend(p._lw("q16"))
                else:
                    p.op("dve", lambda e: e.tensor_copy(out=q16, in_=qt[b]), reads=[f"qt{b}"], writes=["q16"])

            def kv_update(b, kd, S, gcol, skey):
                for h in range(4):
                    p.op("dve", lambda e, h=h: e.tensor_scalar_mul(out=ks16[:, h * 128:(h + 1) * 128], in0=k16[:, h * 128:(h + 1) * 128],
                                                                   scalar1=kd[:, h:h + 1]), reads=["k16", "kdf", "kdb"], writes=["ks16"])
                for h in range(4):
                    p.op("pe", lambda e, h=h: e.matmul(pbK[h // 2][:, (h % 2) * 256:(h % 2) * 256 + 256], lhsT=ks16[:, h * 128:(h + 1) * 128],
                                                       rhs=vt[b][:, h * 256:(h + 1) * 256], start=True, stop=True),
                         reads=["ks16", f"vt{b}"], writes=[f"pb{3 + h // 2}"])
                for h in range(4):
                    p.op("dve", lambda e, h=h: e.scalar_tensor_tensor(
                        out=S[:, h, :], in0=S[:, h, :], scalar=gC[:, gcol + h:gcol + h + 1],
                        in1=pbK[h // 2][:, (h % 2) * 256:(h % 2) * 256 + 256], op0=ALU.mult, op1=ALU.add),
                        reads=[skey, "gC", f"pb{3 + h // 2}"], writes=[skey])


            def pass1_chunk(n, b, tok, use_rope):
                if True:
                    load_k_v(n, tok, b, use_rope, False)
                    prep_k(b, use_rope)
                    p.op("act", lambda e, n=n: e.copy(out=Sbprev[:, n, :], in_=Sb.rearrange("q h d -> q (h d)")),
                         reads=["Sb"], writes=[f"Sbprev{n}"])
                    kv_update(b, kdb, Sb, 4, "Sb")
            def pass2_chunk(n, b, tok, use_rope):
                if True:
                    load_k_v(n, tok, b, use_rope, True)
                    prep_k(b, use_rope)
                    prep_q(b, use_rope)
                    for h in range(4):
                        p.op("pe", lambda e, h=h: e.transpose(ptr[0][:, h * 128:(h + 1) * 128], q16[:, h * 128:(h + 1) * 128], ident[:]),
                             reads=["q16", "ident"], writes=["ptr0"])
                        p.op("pe", lambda e, h=h: e.transpose(ptr[0][:, (4 + h) * 128:(5 + h) * 128], k16[:, h * 128:(h + 1) * 128], ident[:]),
                             reads=["k16", "ident"], writes=["ptr0"])
                    p.op("act", lambda e: e.copy(out=qkT, in_=ptr[0][:, :].rearrange("q (k n) -> q k n", k=8)),
                         reads=["ptr0"], writes=["qkT"])
                    for h in range(4):
                        p.op("pe", lambda e, h=h: e.matmul(pbS[:, h * 128:(h + 1) * 128], lhsT=qkT[:, 4 + h, :], rhs=qkT[:, h, :],
                                                           start=True, stop=True), reads=["qkT"], writes=["pb0"])
                    p.op("dve", lambda e: e.tensor_tensor(out=PT, in0=pbS[:, :].rearrange("q (h c) -> q h c", h=4), in1=dmask, op=ALU.mult),
                         reads=["pb0", "dmask"], writes=["PT"])
                    p.op("dve", lambda e: e.tensor_tensor(out=qfT, in0=qkT[:, 0:4, :], in1=qdf, op=ALU.mult),
                         reads=["qkT", "qdf"], writes=["qfT"])
                    p.op("dve", lambda e: e.tensor_tensor(out=qbT, in0=qkT[:, 0:4, :], in1=qdb, op=ALU.mult),
                         reads=["qkT", "qdb"], writes=["qbT"])
                    for h in range(4):
                        ob = pbO[h // 2][:, (h % 2) * 256:(h % 2) * 256 + 256]
                        ok = f"pb{1 + h // 2}"
                        p.op("pe", lambda e, h=h, ob=ob: e.matmul(ob, lhsT=PT[:, h, :], rhs=vt[b][:, h * 256:(h + 1) * 256], start=True, stop=False),
                             reads=["PT", f"vt{b}"], writes=[ok])
                        p.op("pe", lambda e, h=h, ob=ob: e.matmul(ob, lhsT=qfT[:, h, :], rhs=Sf16[:, h, :], start=False, stop=False),
                             reads=["qfT", "Sf16"], writes=[ok])
                        p.op("pe", lambda e, h=h, ob=ob, n=n: e.matmul(ob, lhsT=qbT[:, h, :], rhs=Sbprev[:, n, h * 256:(h + 1) * 256],
                                                                       start=False, stop=True),
                             reads=["qbT", f"Sbprev{n}"], writes=[ok])
                    kv_update(b, kdf, Sf, 0, "Sf")
                    p.op("act", lambda e: e.copy(out=Sf16, in_=Sf), reads=["Sf"], writes=["Sf16"])
                    for h in range(4):
                        ob = pbO[h // 2][:, (h % 2) * 256:(h % 2) * 256 + 256]
                        ok = f"pb{1 + h // 2}"
                        p.op("dve", lambda e, h=h, ob=ob: e.bn_stats(out=rstat[:, h, :], in_=ob), reads=[ok], writes=["rstat"])
                    for h in range(4):
                        p.op("dve", lambda e, h=h: e.bn_aggr(out=rmv[:, h, :], in_=rstat[:, h, :]), reads=["rstat"], writes=["rmv"])
                    p.op("dve", lambda e: e.tensor_scalar_add(out=rr, in0=rmv[:, :, 1], scalar1=GN_EPS), reads=["rmv"], writes=["rr"])
                    p.op("act", lambda e: e.sqrt(out=rr, in_=rr), reads=["rr"], writes=["rr"])
                    p.op("dve", lambda e: e.reciprocal(out=rr, in_=rr), reads=["rr"], writes=["rr"])
                    for h in range(4):
                        ob = pbO[h // 2][:, (h % 2) * 256:(h % 2) * 256 + 256]
                        ok = f"pb{1 + h // 2}"
                        p.op("dve", lambda e, h=h, ob=ob: e.tensor_scalar(out=yn[:, h * 256:(h + 1) * 256], in0=ob, scalar1=rmv[:, h, 0:1],
                                                                          scalar2=rr[:, h:h + 1], op0=ALU.subtract, op1=ALU.mult),
                             reads=[ok, "rmv", "rr"], writes=["yn"])
                    p.op("act", lambda e: e.activation(out=sg, in_=gt[b], func=AF.Silu), reads=[f"gt{b}"], writes=["sg"])
                    p.op("dve", lambda e: e.tensor_tensor(out=yn, in0=yn, in1=gnw, op=ALU.mult), reads=["yn", "gnw"], writes=["yn"])
                    p.op("dve", lambda e: e.tensor_tensor(out=y16, in0=yn, in1=sg, op=ALU.mult), reads=["yn", "sg"], writes=["y16"])
                    for k in range(8):
                        p.op("pe", lambda e, k=k: e.transpose(ptr[1][:, k * 128:(k + 1) * 128], y16[:, k * 128:(k + 1) * 128], ident[:]),
                             reads=["y16", "ident"], writes=["ptr1"])
                    p.op("act", lambda e: e.copy(out=yT, in_=ptr[1][:, :].rearrange("q (k n) -> q k n", k=8)), reads=["ptr1"], writes=["yT"])
                    p.dma("sp", lambda e, tok=tok: e.dma_start(out=YRETT.ap()[:, tok:tok + 128].rearrange("(k q) t -> q k t", q=128), in_=yT),
                          reads=["yT"], writes=["YRETT"])

            def segment(tok0, nchunks, use_rope):
                for n in range(nchunks - 1, -1, -1):
                    pass1_chunk(n, n % 2, tok0 + n * 128, use_rope)
                for n in range(nchunks):
                    pass2_chunk(n, n % 2, tok0 + n * 128, use_rope)

            segment(SEQ, 2, False)
            segment(0, 32, True)


        YNAT = scratch("YNAT", [512, NT])

        def nattn(l, with_ctx_q):
            areset()
            qT = alloc([NT], BF16)
            kT = alloc([NT], BF16)
            va_aug = alloc([NTILE, 8, 65], BF16)
            y_tm = alloc([NTILE, 512], BF16)
            tmpv = alloc([17, 512], BF16)
            nabt = alloc([3200], F32)
            maskt = alloc([3200], F32)
            emb = alloc([5, 5, 128], BF16)
            PTs = [alloc([7, 128], BF16) for _ in range(2)]
            rec = alloc([4], F32)
            nstg = alloc([4, 128], BF16)
            p.dma("sp", lambda e: e.dma_start(out=maskt, in_=na_mask.ap()), writes=["maskt"])
            p.op("pool", lambda e: e.memset(va_aug[:, :, :, 64:65], 1.0), writes=["va_ones"])
            for half in range(2):
                p.dma("sp", lambda e, half=half: e.dma_start(
                    out=tmpv, in_=VA.ap()[half * 17 * 128:(half + 1) * 17 * 128, :].rearrange("(t q) d -> q t d", q=128)),
                    reads=["VA"], writes=["tmpv"])
                p.op("dve", lambda e, half=half: e.tensor_copy(
                    out=va_aug[:, half * 17:(half + 1) * 17, :, 0:64], in_=tmpv.rearrange("q t (h d) -> q t h d", h=8)),
                    reads=["tmpv"], writes=[f"va{half}"])
            slot_rr = [0]

            def one(h, tq, keytiles, cls, off):
                bi = tq % 2
                big = pbig[bi][:, :].rearrange("q (j n) -> q j n", j=8)
                PT = PTs[bi]
                nk = len(keytiles)
                for j, ktile in enumerate(keytiles):
                    p.op("pe", lambda e, j=j, ktile=ktile: e.matmul(
                        big[:, j, :], lhsT=kT[off:off + 64, ktile * 128:(ktile + 1) * 128],
                        rhs=qT[off:off + 64, tq * 128:(tq + 1) * 128], start=True, stop=True),
                        reads=["qT", "kT"], writes=[f"pbig{bi}"])
                n0 = min(nk, 4)
                p.op("act", lambda e: e.activation(out=PT[:, 0:n0, :], in_=big[:, 0:n0, :], func=AF.Exp, scale=0.125),
                     reads=[f"pbig{bi}"], writes=[f"PT{bi}"])
                if nk > 4:
                    p.op("act", lambda e: e.activation(out=PT[:, 4:nk, :], in_=big[:, 4:nk, :], func=AF.Exp, scale=0.125),
                         reads=[f"pbig{bi}"], writes=[f"PT{bi}"])
                if cls is not None:
                    p.op("dve", lambda e: e.tensor_tensor(out=PT[:, 0:5, :], in0=PT[:, 0:5, :], in1=emb[:, cls, :, :], op=ALU.mult),
                         reads=[f"PT{bi}", "emb"], writes=[f"PT{bi}"])
                slot_rr[0] = (slot_rr[0] + 1) % 8
                sl = slot_rr[0]
                po = pb45[sl // 4][:, (sl % 4) * 128:(sl % 4) * 128 + 65]
                pk = f"po{sl}"
                for j, ktile in enumerate(keytiles):
                    p.op("pe", lambda e, j=j, ktile=ktile: e.matmul(
                        po, lhsT=PT[:, j, :], rhs=va_aug[:, ktile, h, :], start=(j == 0), stop=(j == nk - 1)),
                        reads=[f"PT{bi}", "va0", "va1", "va_ones"], writes=[pk])
                rc = rec[:, sl % 4:sl % 4 + 1]
                p.op("dve", lambda e: e.reciprocal(out=rc, in_=po[:, 64:65]), reads=[pk], writes=[f"rec{sl % 4}"])
                p.op("dve", lambda e: e.tensor_scalar_mul(out=y_tm[:, tq, h * 64:(h + 1) * 64], in0=po[:, 0:64], scalar1=rc),
                     reads=[pk, f"rec{sl % 4}"], writes=[f"ytm{tq}"])

            for h in range(8):
                pair, off = h // 2, (h % 2) * 64
                if h % 2 == 0:
                    p.dma("sp", lambda e, pair=pair: e.dma_start(out=qT, in_=QAT.ap()[pair * 128:(pair + 1) * 128, :]),
                          reads=["QAT"], writes=["qT"])
                    p.dma("sp", lambda e, pair=pair: e.dma_start(out=kT, in_=KAT.ap()[pair * 128:(pair + 1) * 128, :]),
                          reads=["KAT"], writes=["kT"])
                p.dma("sp", lambda e, h=h: e.dma_start(out=nabt, in_=na_bias.ap()[l, h, :, :]), writes=["nabt"])
                p.op("act", lambda e: e.activation(out=nabt, in_=nabt, func=AF.Exp), reads=["nabt"], writes=["nabt"])
                p.op("dve", lambda e: e.tensor_tensor(out=emb.rearrange("q c j n -> q (c j n)"), in0=nabt, in1=maskt, op=ALU.mult),
                     reads=["nabt", "maskt"], writes=["emb"])
                for tq in range(32):
                    cls = {0: 0, 1: 1, 30: 3, 31: 4}.get(tq, 2)
                    k0 = min(max(tq - 2, 0), 27)
                    one(h, tq, [k0 + j for j in range(5)] + [32, 33], cls, off)
                if with_ctx_q:
                    for tq in (32, 33):
                        one(h, tq, [32, 33], None, off)
            for t in range(NTILE if with_ctx_q else 32):
                for k in range(4):
                    p.op("pe", lambda e, k=k, t=t: e.transpose(ptr[t % 2][:, k * 128:(k + 1) * 128], y_tm[:, t, k * 128:(k + 1) * 128], ident[:]),
                         reads=[f"ytm{t}", "ident"], writes=[f"ptr{t % 2}"])
                copy_any(nstg, ptr[t % 2][:, 0:512].rearrange("q (k n) -> q k n", k=4), [f"ptr{t % 2}"], ["nstg"])
                p.dma("sp", lambda e, t=t: e.dma_start(out=YNAT.ap()[:, t * 128:(t + 1) * 128].rearrange("(k q) n -> q k n", q=128), in_=nstg),
                      reads=["nstg"], writes=["YNAT"])


        def merge(l, ntiles):
            areset()
            wna = alloc([4, 1024], BF16)
            wfn = alloc([4, 1024], BF16)
            wret = alloc([8, 1024], BF16)
            wout = alloc([8, 1024], BF16)
            lnw = alloc([1024], F32)
            lnb = alloc([1024], F32)
            glt = [alloc([3072], BF16) for _ in range(2)]
            gates = alloc([3072], F32)
            ynT = [alloc([4, 128], BF16) for _ in range(2)]
            yfT = [alloc([4, 128], BF16) for _ in range(2)]
            yrT = [alloc([8, 128], BF16) for _ in range(2)]
            xt = [alloc([1024], F32) for _ in range(2)]
            ysum = alloc([1024], F32)
            ytmp = alloc([1024], F32)
            y16 = alloc([1024], BF16)
            ysT = alloc([8, 128], BF16)
            xo = alloc([1024], F32)
            stats = alloc([2, 6], F32)
            mv = alloc([2], F32)
            rstd = alloc([1], F32)
            p.dma("pool", lambda e: e.dma_start(out=wna, in_=w_o_na.ap()[l].rearrange("(c q) n -> q c n", q=128)), writes=["wna"])
            p.dma("pool", lambda e: e.dma_start(out=wfn, in_=w_fourier.ap()[l].rearrange("(c q) n -> q c n", q=128)), writes=["wfn"])
            p.dma("pool", lambda e: e.dma_start(out=wret, in_=w_o_ret.ap()[l].rearrange("(c q) n -> q c n", q=128)), writes=["wret"])
            p.dma("pool", lambda e: e.dma_start(out=wout, in_=w_out.ap()[l].rearrange("(c q) n -> q c n", q=128)), writes=["wout"])
            p.dma("sp", lambda e: e.dma_start(out=lnw, in_=ln_mix_w.ap()[l, :].partition_broadcast(128)), writes=["lnw"])
            p.dma("sp", lambda e: e.dma_start(out=lnb, in_=ln_mix_b.ap()[l, :].partition_broadcast(128)), writes=["lnb"])

            def tile_fn(t, b, j):
                tok = t * 128
                p.dma("sp", lambda e: e.dma_start(out=glt[b], in_=GL.ap()[tok:tok + 128, :]), reads=["GL"], writes=[f"glt{b}"])
                p.dma("sp", lambda e: e.dma_start(out=ynT[b], in_=YNAT.ap()[:, tok:tok + 128].rearrange("(k q) n -> q k n", q=128)),
                      reads=["YNAT"], writes=[f"ynT{b}"])
                p.dma("sp", lambda e: e.dma_start(out=yfT[b], in_=YFNT.ap()[:, tok:tok + 128].rearrange("(k q) n -> q k n", q=128)),
                      reads=["YFNT"], writes=[f"yfT{b}"])
                p.dma("sp", lambda e: e.dma_start(out=yrT[b], in_=YRETT.ap()[:, tok:tok + 128].rearrange("(k q) n -> q k n", q=128)),
                      reads=["YRETT"], writes=[f"yrT{b}"])
                p.dma("sp", lambda e: e.dma_start(out=xt[b], in_=XRES.ap()[tok:tok + 128, :]), reads=["xres"], writes=[f"mxt{b}"])
                p.op("act", lambda e: e.activation(out=gates, in_=glt[b], func=AF.Sigmoid), reads=[f"glt{b}"], writes=["gates"])
                branches = [(ynT[b], wna, 4, f"ynT{b}", "wna"), (yfT[b], wfn, 4, f"yfT{b}", "wfn"), (yrT[b], wret, 8, f"yrT{b}", "wret")]
                for nb in range(2):
                    cs = slice(nb * 512, (nb + 1) * 512)
                    for bi, (yT, w, nk, yk, wk) in enumerate(branches):
                        bank, bkey = next_bank()
                        for k in range(nk):
                            p.op("pe", lambda e, k=k, yT=yT, w=w, bank=bank, nk=nk, cs=cs: e.matmul(
                                bank, lhsT=yT[:, k, :], rhs=w[:, k, cs], start=(k == 0), stop=(k == nk - 1)),
                                reads=[yk, wk], writes=[bkey])
                        gsl = gates[:, bi * 1024 + nb * 512: bi * 1024 + (nb + 1) * 512]
                        if bi == 0:
                            p.op("dve", lambda e, bank=bank, gsl=gsl, cs=cs: e.tensor_tensor(out=ysum[:, cs], in0=bank, in1=gsl, op=ALU.mult),
                                 reads=[bkey, "gates"], writes=[f"ysum{nb}"])
                        else:
                            p.op("dve", lambda e, bank=bank, gsl=gsl, cs=cs: e.tensor_tensor(out=ytmp[:, cs], in0=bank, in1=gsl, op=ALU.mult),
                                 reads=[bkey, "gates"], writes=[f"ytmp{nb}"])
                            dst = y16 if bi == 2 else ysum
                            p.op("dve", lambda e, dst=dst, cs=cs: e.tensor_tensor(out=dst[:, cs], in0=ysum[:, cs], in1=ytmp[:, cs], op=ALU.add),
                                 reads=[f"ysum{nb}", f"ytmp{nb}"], writes=[f"ysum{nb}", f"y16{nb}"])
                for k in range(8):
                    p.op("pe", lambda e, k=k: e.transpose(ptr[b][:, k * 128:(k + 1) * 128], y16[:, k * 128:(k + 1) * 128], ident[:]),
                         reads=["y160", "y161", "ident"], writes=[f"ptr{b}"])
                copy_any(ysT, ptr[b][:, :].rearrange("q (k n) -> q k n", k=8), [f"ptr{b}"], ["ysT"])
                for nb in range(2):
                    cs = slice(nb * 512, (nb + 1) * 512)
                    bank, bkey = next_bank()
                    for k in range(8):
                        p.op("pe", lambda e, k=k, bank=bank, cs=cs: e.matmul(bank, lhsT=ysT[:, k, :], rhs=wout[:, k, cs], start=(k == 0), stop=(k == 7)),
                             reads=["ysT", "wout"], writes=[bkey])
                    p.op("dve", lambda e, bank=bank, cs=cs, nb=nb: e.tensor_tensor(out=xo[:, cs], in0=bank, in1=mods[:, j, 2 * D + nb * 512:2 * D + (nb + 1) * 512],
                                                                     op=ALU.mult), reads=[bkey, "mods"], writes=["xo"])
                p.op("dve", lambda e: e.scalar_tensor_tensor(out=xo, in0=xt[b], scalar=DN_ALPHA, in1=xo, op0=ALU.mult, op1=ALU.add),
                     reads=[f"mxt{b}", "xo"], writes=["xo"])
                ln_tile(xo, stats, mv, rstd, "xo", "mst")
                p.op("dve", lambda e: e.tensor_tensor(out=xo, in0=xo, in1=lnw, op=ALU.mult), reads=["xo", "lnw"], writes=["xo"])
                p.op("dve", lambda e: e.tensor_tensor(out=xo, in0=xo, in1=lnb, op=ALU.add), reads=["xo", "lnb"], writes=["xo"])
                p.dma("sp", lambda e: e.dma_start(out=XRES.ap()[tok:tok + 128, :], in_=xo), reads=["xo"], writes=["xres_w"])

            for t in range(ntiles):
                tile_fn(t, t % 2, 0 if t < 32 else 1)


        NBLK = 526
        NROW = 640
        NSLOT = NROW * 128
        H16 = scratch("H16", [NT + 1, D])
        SHO = scratch("SHO", [NT, D])
        TBL = scratch("TBL", [NSLOT, 1], F32)
        OUTS = scratch("OUTS", [NSLOT, D])
        ident_f = sb("ident_f", [128, 128], F32)
        p.op("pool", lambda e: e.memset(ident_f[:], 1.0), writes=["ident_f"])
        p.op("pool", lambda e: e.affine_select(out=ident_f[:], in_=ident_f[:], pattern=[[-1, 128]],
                                              compare_op=ALU.is_equal, fill=0.0, base=0, channel_multiplier=1),
             reads=["ident_f"], writes=["ident_f"])

        def moe(l, ntiles):
            areset()
            rw = alloc([8, 256], F32)
            rb = alloc([256], F32)
            shgu = alloc([8, 512], BF16)
            shd = alloc([2, 1024], BF16)
            triU = alloc([128], BF16)
            ones16 = alloc([128], BF16)
            onesr = alloc([256], F32)
            cum = alloc([256], F32)
            eidi = alloc([256], I32)
            eidx = alloc([256], F32)
            qci = alloc([1], I32)
            qcf = alloc([1], F32)
            toki = alloc([NTILE], I32)
            tokf = alloc([NTILE], F32)
            W8 = alloc([NTILE, 8], F32)
            E8 = alloc([NTILE, 8], F32)
            R8 = alloc([NTILE, 8], F32)
            D8 = alloc([NTILE, 8], I32)
            BE = alloc([NROW], F32)
            WIDX = alloc([NROW], I32)
            padded = alloc([256], F32)
            pad_end = alloc([256], F32)
            pad_start = alloc([256], F32)
            xt = [alloc([1024], F32) for _ in range(2)]
            h16 = alloc([1024], BF16)
            hT16 = alloc([8, 128], BF16)
            hT32 = alloc([8, 128], F32)
            hm16 = alloc([256], BF16)
            sgt = alloc([256], F32)
            hmT = alloc([2, 128], BF16)
            sho16 = alloc([1024], BF16)
            sc = alloc([256], F32)
            sel = alloc([256], F32)
            selm = alloc([256], F32)
            m8 = alloc([8, 8], F32)
            gs = alloc([8], F32)
            g8 = alloc([8], F32)
            pen = alloc([8], F32)
            v8 = alloc([8], F32)
            A = alloc([256], F32)
            A16 = alloc([256], BF16)
            rankd = alloc([256], F32)
            junk = alloc([256], F32)
            d8f = alloc([8], F32)
            w8 = alloc([8], F32)
            wsum = alloc([1], F32)
            stats = alloc([2, 6], F32)
            mv = alloc([2], F32)
            rstd = alloc([1], F32)
            tfill = alloc([NROW], F32)
            zrow = alloc([1024], BF16)

            p.dma("sp", lambda e: e.dma_start(out=rw, in_=router_w.ap()[l].rearrange("(c q) n -> q c n", q=128)), writes=["rw"])
            p.dma("sp", lambda e: e.dma_start(out=rb, in_=router_bias.ap()[l, :].partition_broadcast(128)), writes=["rb"])
            p.dma("pool", lambda e: e.dma_start(out=shgu[:, :, 0:256], in_=sh_w_gate.ap()[l].rearrange("(c q) n -> q c n", q=128)), writes=["shgu_a"])
            p.dma("pool", lambda e: e.dma_start(out=shgu[:, :, 256:512], in_=sh_w_up.ap()[l].rearrange("(c q) n -> q c n", q=128)), writes=["shgu_b"])
            p.dma("pool", lambda e: e.dma_start(out=shd, in_=sh_w_down.ap()[l].rearrange("(c q) n -> q c n", q=128)), writes=["shd"])
            p.op("pool", lambda e: e.memset(triU, 1.0), writes=["triU"])
            p.op("pool", lambda e: e.affine_select(out=triU, in_=triU, pattern=[[1, 128]], compare_op=ALU.is_gt, fill=0.0,
                                                  base=0, channel_multiplier=-1), reads=["triU"], writes=["triU"])
            p.op("pool", lambda e: e.memset(ones16, 1.0), writes=["ones16"])
            p.op("pool", lambda e: e.memset(onesr, 1.0), writes=["onesr"])
            p.op("pool", lambda e: e.memset(cum, 0.0), writes=["cum"])
            p.op("pool", lambda e: e.iota(eidi, pattern=[[1, 256]], base=0, channel_multiplier=0), writes=["eidi"])
            p.op("dve", lambda e: e.tensor_copy(out=eidx, in_=eidi), reads=["eidi"], writes=["eidx"])
            p.op("pool", lambda e: e.iota(qci, pattern=[[0, 1]], base=0, channel_multiplier=1), writes=["qci"])
            p.op("dve", lambda e: e.tensor_copy(out=qcf, in_=qci), reads=["qci"], writes=["qcf"])
            p.op("pool", lambda e: e.iota(toki, pattern=[[128, NTILE]], base=0, channel_multiplier=1), writes=["toki"])
            p.op("dve", lambda e: e.tensor_copy(out=tokf, in_=toki), reads=["toki"], writes=["tokf"])
            p.op("pool", lambda e: e.memset(tfill, float(NT)), writes=["tfill"])
            p.op("pool", lambda e: e.memset(zrow, 0.0), writes=["zrow"])
            p.dma("sp", lambda e: e.dma_start(out=TBL.ap().rearrange("(q f) o -> q (f o)", q=128), in_=tfill), reads=["tfill"], writes=["TBL"])
            p.dma("sp", lambda e: e.dma_start(out=H16.ap()[NT:NT + 1, :], in_=zrow[0:1, :]), reads=["zrow"], writes=["H16"])

            def m1_tile(t, b, j):
                tok = t * 128
                p.dma("sp", lambda e: e.dma_start(out=xt[b], in_=XRES.ap()[tok:tok + 128, :]), reads=["xres", "xres_w"], writes=[f"xt{b}"])
                ln_tile(xt[b], stats, mv, rstd, f"xt{b}", "st")
                p.op("dve", lambda e: e.tensor_tensor(out=xt[b], in0=xt[b], in1=mods[:, j, 4 * D:5 * D], op=ALU.mult),
                     reads=[f"xt{b}", "mods"], writes=[f"xt{b}"])
                p.op("dve", lambda e: e.tensor_tensor(out=xt[b], in0=xt[b], in1=mods[:, j, 3 * D:4 * D], op=ALU.add),
                     reads=[f"xt{b}", "mods"], writes=[f"xt{b}"])
                p.op("act", lambda e: e.copy(out=h16, in_=xt[b]), reads=[f"xt{b}"], writes=["h16"])
                p.dma("sp", lambda e: e.dma_start(out=H16.ap()[tok:tok + 128, :], in_=h16), reads=["h16"], writes=["H16"])
                for k in range(8):
                    p.op("pe", lambda e, k=k: e.transpose(ptr[0][:, k * 128:(k + 1) * 128], h16[:, k * 128:(k + 1) * 128], ident[:]),
                         reads=["h16", "ident"], writes=["ptr0"])
                p.op("act", lambda e: e.copy(out=hT16, in_=ptr[0][:, :].rearrange("q (k n) -> q k n", k=8)), reads=["ptr0"], writes=["hT16"])
                for k in range(8):
                    p.op("pe", lambda e, k=k: e.matmul(pb45[0][:, :], lhsT=hT16[:, k, :], rhs=shgu[:, k, :], start=(k == 0), stop=(k == 7)),
                         reads=["hT16", "shgu_a", "shgu_b"], writes=["pb4"])
                p.op("act", lambda e: e.activation(out=sgt, in_=pb45[0][:, 0:256], func=AF.Silu), reads=["pb4"], writes=["sgt"])
                p.op("dve", lambda e: e.tensor_tensor(out=hm16, in0=sgt, in1=pb45[0][:, 256:512], op=ALU.mult), reads=["sgt", "pb4"], writes=["hm16"])
                for k in range(2):
                    p.op("pe", lambda e, k=k: e.transpose(ptr[1][:, k * 128:(k + 1) * 128], hm16[:, k * 128:(k + 1) * 128], ident[:]),
                         reads=["hm16", "ident"], writes=["ptr1"])
                p.op("act", lambda e: e.copy(out=hmT, in_=ptr[1][:, 0:256].rearrange("q (k n) -> q k n", k=2)), reads=["ptr1"], writes=["hmT"])
                for nb in range(2):
                    bank = pbank[2 + nb]
                    for k in range(2):
                        p.op("pe", lambda e, k=k, nb=nb, bank=bank: e.matmul(bank, lhsT=hmT[:, k, :], rhs=shd[:, k, nb * 512:(nb + 1) * 512],
                                                                             start=(k == 0), stop=(k == 1)), reads=["hmT", "shd"], writes=[f"pb{2 + nb}"])
                    copy_any(sho16[:, nb * 512:(nb + 1) * 512], bank, [f"pb{2 + nb}"], ["sho16"])
                p.dma("sp", lambda e: e.dma_start(out=SHO.ap()[tok:tok + 128, :], in_=sho16), reads=["sho16"], writes=["SHO"])
                for k in range(8):
                    p.op("pe", lambda e, k=k: e.transpose(pbig[0][:, k * 128:(k + 1) * 128], xt[b][:, k * 128:(k + 1) * 128], ident_f[:]),
                         reads=[f"xt{b}", "ident_f"], writes=["pb0", "pb1"])
                p.op("dve", lambda e: e.tensor_copy(out=hT32, in_=pbig[0][:, :].rearrange("q (k n) -> q k n", k=8)), reads=["pb0", "pb1"], writes=["hT32"])
                for k in range(8):
                    p.op("pe", lambda e, k=k: e.matmul(pb45[1][:, 0:256], lhsT=hT32[:, k, :], rhs=rw[:, k, :], start=(k == 0), stop=(k == 7)),
                         reads=["hT32", "rw"], writes=["pb5"])
                p.op("act", lambda e: e.activation(out=sc, in_=pb45[1][:, 0:256], func=AF.Sigmoid), reads=["pb5"], writes=["sc"])
                p.op("dve", lambda e: e.tensor_tensor(out=sel, in0=sc, in1=rb, op=ALU.add), reads=["sc", "rb"], writes=["sel"])
                for g in range(8):
                    p.op("dve", lambda e, g=g: e.max(out=m8[:, g, :], in_=sel[:, g * 32:(g + 1) * 32]), reads=["sel"], writes=["m8"])
                p.op("dve", lambda e: e.tensor_tensor(out=gs, in0=m8[:, :, 0], in1=m8[:, :, 1], op=ALU.add), reads=["m8"], writes=["gs"])
                p.op("dve", lambda e: e.max(out=g8, in_=gs), reads=["gs"], writes=["g8"])
                p.op("dve", lambda e: e.tensor_scalar(out=pen, in0=gs, scalar1=g8[:, 3:4], scalar2=None, op0=ALU.is_ge), reads=["gs", "g8"], writes=["pen"])
                p.op("dve", lambda e: e.tensor_scalar(out=pen, in0=pen, scalar1=1.0, scalar2=1.0e4, op0=ALU.subtract, op1=ALU.mult),
                     reads=["pen"], writes=["pen"])
                p.op("dve", lambda e: e.tensor_tensor(out=selm.rearrange("q (g x) -> q g x", g=8), in0=sel.rearrange("q (g x) -> q g x", g=8),
                                                      in1=pen.unsqueeze(2).broadcast_to([128, 8, 32]), op=ALU.add), reads=["sel", "pen"], writes=["selm"])
                p.op("dve", lambda e: e.max(out=v8, in_=selm), reads=["selm"], writes=["v8"])
                p.op("dve", lambda e: e.tensor_scalar(out=A, in0=selm, scalar1=v8[:, 7:8], scalar2=None, op0=ALU.is_ge), reads=["selm", "v8"], writes=["A"])
                p.op("dve", lambda e: e.tensor_copy(out=A16, in_=A), reads=["A"], writes=["A16"])
                p.op("pe", lambda e: e.matmul(pbank[2][:, 0:256], lhsT=triU, rhs=A16, start=True, stop=True), reads=["triU", "A16"], writes=["pb2"])
                p.op("pe", lambda e: e.matmul(pbank[3][:, 0:256], lhsT=ones16, rhs=A16, start=True, stop=True), reads=["ones16", "A16"], writes=["pb3"])
                p.op("dve", lambda e: e.tensor_tensor(out=rankd, in0=pbank[2][:, 0:256], in1=cum, op=ALU.add), reads=["pb2", "cum"], writes=["rankd"])
                p.op("dve", lambda e: e.tensor_tensor(out=cum, in0=pbank[3][:, 0:256], in1=cum, op=ALU.add), reads=["pb3", "cum"], writes=["cum"])
                for k in range(8):
                    for (src, dstT, key) in ((rankd, R8, "R8"), (sc, w8.unsqueeze(1), "w8"), (eidx, E8, "E8")):
                        oap = dstT[:, t, k:k + 1] if key != "w8" else w8[:, k:k + 1]
                        p.op("dve", lambda e, k=k, src=src, oap=oap: e.scalar_tensor_tensor(
                            out=junk, in0=selm, scalar=v8[:, k:k + 1], in1=src, op0=ALU.is_equal, op1=ALU.mult, accum_out=oap),
                            reads=["selm", "v8", "rankd", "sc", "eidx"], writes=["junk", f"{key}_{t}"])
                p.op("dve", lambda e: e.reduce_sum(out=wsum, in_=w8, axis=AX.X), reads=[f"w8_{t}"], writes=["wsum"])
                p.op("dve", lambda e: e.reciprocal(out=wsum, in_=wsum), reads=["wsum"], writes=["wsum"])
                p.op("dve", lambda e: e.tensor_scalar(out=W8[:, t, :], in0=w8, scalar1=wsum[:, 0:1], scalar2=2.5, op0=ALU.mult, op1=ALU.mult),
                     reads=[f"w8_{t}", "wsum"], writes=[f"W8_{t}"])

            for t in range(ntiles):
                m1_tile(t, t % 2, 0 if t < 32 else 1)

            p.barrier()
            MAGIC_ = 12582912.0
            p.op("dve", lambda e: e.tensor_scalar(out=padded, in0=cum, scalar1=63.25, scalar2=1.0 / 128, op0=ALU.add, op1=ALU.mult),
                 reads=["cum"], writes=["padded"])
            p.op("dve", lambda e: e.tensor_scalar_add(out=padded, in0=padded, scalar1=MAGIC_), reads=["padded"], writes=["padded"])
            p.op("dve", lambda e: e.tensor_scalar(out=padded, in0=padded, scalar1=MAGIC_, scalar2=128.0, op0=ALU.subtract, op1=ALU.mult),
                 reads=["padded"], writes=["padded"])
            p.op("dve", lambda e: e.tensor_tensor_scan(out=pad_end, data0=onesr, data1=padded, initial=0.0, op0=ALU.mult, op1=ALU.add),
                 reads=["padded", "onesr"], writes=["pad_end"])
            p.op("dve", lambda e: e.tensor_tensor(out=pad_start, in0=pad_end, in1=padded, op=ALU.subtract), reads=["pad_end", "padded"], writes=["pad_start"])
            for bq in range(NROW):
                p.op("dve", lambda e, bq=bq: e.scalar_tensor_tensor(out=junk, in0=pad_end, scalar=float(128 * bq), in1=onesr, op0=ALU.is_le,
                                                                    op1=ALU.mult, accum_out=BE[:, bq:bq + 1]),
                     reads=["pad_end", "onesr"], writes=["junk", "BE"])
            p.op("dve", lambda e: e.tensor_scalar(out=BE, in0=BE, scalar1=255.0, scalar2=128.0, op0=ALU.min, op1=ALU.mult), reads=["BE"], writes=["BE"])
            p.op("dve", lambda e: e.tensor_scalar_add(out=BE, in0=BE, scalar1=qcf[:, 0:1]), reads=["BE", "qcf"], writes=["BE"])
            p.op("dve", lambda e: e.tensor_copy(out=WIDX, in_=BE), reads=["BE"], writes=["WIDX"])

            def m1b_tile(t):
                for k in range(8):
                    p.op("dve", lambda e, k=k: e.scalar_tensor_tensor(
                        out=junk, in0=eidx, scalar=E8[:, t, k:k + 1], in1=pad_start, op0=ALU.is_equal, op1=ALU.mult, accum_out=d8f[:, k:k + 1]),
                        reads=["eidx", f"E8_{t}", "pad_start"], writes=["junk", "d8f"])
                p.op("dve", lambda e: e.tensor_tensor(out=d8f, in0=d8f, in1=R8[:, t, :], op=ALU.add), reads=["d8f", f"R8_{t}"], writes=["d8f"])
                p.op("dve", lambda e: e.tensor_copy(out=D8[:, t, :], in_=d8f), reads=["d8f"], writes=[f"D8_{t}"])
                for k in range(8):
                    p.dma("pool", lambda e, k=k: e.indirect_dma_start(
                        out=TBL.ap()[:, :], out_offset=bass.IndirectOffsetOnAxis(ap=D8[:, t, k:k + 1], axis=0),
                        in_=tokf[:, t:t + 1], in_offset=None), reads=[f"D8_{t}", "tokf", "TBL"], writes=["TBLs"])

            for t in range(ntiles):
                m1b_tile(t)

            p.barrier()
            tbl_sb = alloc([5, 128], F32)
            idxc = alloc([NROW], I32)
            wg = [alloc([8, 256], BF16) for _ in range(3)]
            wu = [alloc([8, 256], BF16) for _ in range(3)]
            wd = [alloc([2, 1024], BF16) for _ in range(3)]
            xg = [alloc([1024], BF16) for _ in range(3)]
            XT = alloc([8, 128], BF16)
            ostg = [alloc([1024], BF16) for _ in range(2)]
            p.dma("sp", lambda e: e.dma_start(out=tbl_sb, in_=TBL.ap().rearrange("(c r q) o -> r c (q o)", c=5, r=128)),
                  reads=["TBL", "TBLs"], writes=["tbl_sb"])
            for c in range(5):
                p.op("pe", lambda e, c=c: e.transpose(pbig[0][:, c * 128:(c + 1) * 128], tbl_sb[:, c, :], ident_f[:]),
                     reads=["tbl_sb", "ident_f"], writes=["pb0", "pb1"])
            p.op("dve", lambda e: e.tensor_copy(out=idxc, in_=pbig[0][:, 0:NROW]), reads=["pb0", "pb1"], writes=["idxc"])

            def load_block_w(bq):
                b3 = bq % 3
                off = bass.IndirectOffsetOnAxis(ap=WIDX[:, bq:bq + 1], axis=0)
                p.dma("pool", lambda e: e.indirect_dma_start(out=wg[b3].rearrange("q c n -> q (c n)"), out_offset=None,
                                                             in_=exp_w_gate[l].ap(), in_offset=off), reads=["WIDX"], writes=[f"wg{b3}"])
                if cfg.get("skipw") and bq > 2:
                    return
                p.dma("pool", lambda e: e.indirect_dma_start(out=wu[b3].rearrange("q c n -> q (c n)"), out_offset=None,
                                                             in_=exp_w_up[l].ap(), in_offset=off), reads=["WIDX"], writes=[f"wu{b3}"])
                p.dma("pool", lambda e: e.indirect_dma_start(out=wd[b3].rearrange("q c n -> q (c n)"), out_offset=None,
                                                             in_=exp_w_down[l].ap(), in_offset=off), reads=["WIDX"], writes=[f"wd{b3}"])

            XTs = [alloc([8, 128], BF16) for _ in range(3)]
            sgts = [alloc([256], F32) for _ in range(2)]
            hm16s = [alloc([256], BF16) for _ in range(2)]
            hmTs = [alloc([2, 128], BF16) for _ in range(2)]

            def stage1(bq):
                b3 = bq % 3
                p.dma("pool", lambda e: e.indirect_dma_start(
                    out=xg[b3], out_offset=None, in_=H16.ap()[:, :],
                    in_offset=bass.IndirectOffsetOnAxis(ap=idxc[:, bq:bq + 1], axis=0)),
                    reads=["idxc", "H16"], writes=[f"xg{b3}"])
                for k in range(8):
                    p.op("pe", lambda e, k=k: e.transpose(ptr[0][:, k * 128:(k + 1) * 128], xg[b3][:, k * 128:(k + 1) * 128], ident[:]),
                         reads=[f"xg{b3}", "ident"], writes=["ptr0"])
                copy_any(XTs[b3], ptr[0][:, :].rearrange("q (k n) -> q k n", k=8), ["ptr0"], [f"XT{b3}"])

            def stage2(bq):
                b3 = bq % 3
                b2 = bq % 2
                for k in range(8):
                    p.op("pe", lambda e, k=k: e.matmul(pb45[0][:, 0:256], lhsT=XTs[b3][:, k, :], rhs=wg[b3][:, k, :], start=(k == 0), stop=(k == 7)),
                         reads=[f"XT{b3}", f"wg{b3}"], writes=["pb4"])
                for k in range(8):
                    p.op("pe", lambda e, k=k: e.matmul(pb45[1][:, 0:256], lhsT=XTs[b3][:, k, :], rhs=wu[b3][:, k, :], start=(k == 0), stop=(k == 7)),
                         reads=[f"XT{b3}", f"wu{b3}"], writes=["pb5"])
                p.op("act", lambda e: e.activation(out=sgts[b2], in_=pb45[0][:, 0:256], func=AF.Silu), reads=["pb4"], writes=[f"sgt{b2}"])
                p.op("dve", lambda e: e.tensor_tensor(out=hm16s[b2], in0=sgts[b2], in1=pb45[1][:, 0:256], op=ALU.mult),
                     reads=[f"sgt{b2}", "pb5"], writes=[f"hm16{b2}"])

            def stage3(bq):
                b3 = bq % 3
                b2 = bq % 2
                o2 = bq % 2
                for k in range(2):
                    p.op("pe", lambda e, k=k: e.transpose(ptr[1][:, k * 128:(k + 1) * 128], hm16s[b2][:, k * 128:(k + 1) * 128], ident[:]),
                         reads=[f"hm16{b2}", "ident"], writes=["ptr1"])
                p.op("act", lambda e: e.copy(out=hmTs[b2], in_=ptr[1][:, 0:256].rearrange("q (k n) -> q k n", k=2)), reads=["ptr1"], writes=[f"hmT{b2}"])
                for nb in range(2):
                    bank = pbank[2 * o2 + nb]
                    bkey = f"pb{2 * o2 + nb}"
                    for k in range(2):
                        p.op("pe", lambda e, k=k, nb=nb, bank=bank: e.matmul(bank, lhsT=hmTs[b2][:, k, :], rhs=wd[b3][:, k, nb * 512:(nb + 1) * 512],
                                                                             start=(k == 0), stop=(k == 1)), reads=[f"hmT{b2}", f"wd{b3}"], writes=[bkey])
                    copy_any(ostg[o2][:, nb * 512:(nb + 1) * 512], bank, [bkey], [f"ostg{o2}"])
                r0 = bq * 128
                p.dma("sp", lambda e: e.dma_start(out=OUTS.ap()[r0:r0 + 128, :], in_=ostg[o2]), reads=[f"ostg{o2}"], writes=["OUTS"])

            NB_ = cfg.get("nblk", NBLK)
            load_block_w(0)
            load_block_w(1)
            stage1(0)
            stage1(1)
            stage2(0)
            for bq in range(NB_):
                if bq + 2 < NB_:
                    load_block_w(bq + 2)
                    stage1(bq + 2)
                if bq + 1 < NB_:
                    stage2(bq + 1)
                stage3(bq)

            p.barrier()
            og = [alloc([1024], BF16) for _ in range(3)]
            acc = alloc([1024], F32)
            lnw = alloc([1024], F32)
            lnb = alloc([1024], F32)
            sh_in = alloc([1024], BF16)
            xo = alloc([1024], F32)
            p.dma("sp", lambda e: e.dma_start(out=lnw, in_=ln_ffn_w.ap()[l, :].partition_broadcast(128)), writes=["lnw"])
            p.dma("sp", lambda e: e.dma_start(out=lnb, in_=ln_ffn_b.ap()[l, :].partition_broadcast(128)), writes=["lnb"])
            gc = [0]

            def m3_tile(t, b, j):
                tok = t * 128
                p.dma("sp", lambda e: e.dma_start(out=sh_in, in_=SHO.ap()[tok:tok + 128, :]), reads=["SHO"], writes=["sh_in"])
                p.dma("sp", lambda e: e.dma_start(out=xt[b], in_=XRES.ap()[tok:tok + 128, :]), reads=["xres", "xres_w"], writes=[f"xt{b}"])
                p.op("dve", lambda e: e.tensor_copy(out=acc, in_=sh_in), reads=["sh_in"], writes=["acc"])
                for k in range(8):
                    g3 = gc[0] % 3
                    gc[0] += 1
                    p.dma("pool", lambda e, k=k, g3=g3: e.indirect_dma_start(
                        out=og[g3], out_offset=None, in_=OUTS.ap()[:, :],
                        in_offset=bass.IndirectOffsetOnAxis(ap=D8[:, t, k:k + 1], axis=0)),
                        reads=[f"D8_{t}", "OUTS"], writes=[f"og{g3}"])
                    p.op("dve", lambda e, k=k, g3=g3: e.scalar_tensor_tensor(out=acc, in0=og[g3], scalar=W8[:, t, k:k + 1], in1=acc,
                                                                             op0=ALU.mult, op1=ALU.add),
                         reads=[f"og{g3}", f"W8_{t}", "acc"], writes=["acc"])
                p.op("dve", lambda e: e.tensor_tensor(out=xo, in0=acc, in1=mods[:, j, 5 * D:6 * D], op=ALU.mult), reads=["acc", "mods"], writes=["xo"])
                p.op("dve", lambda e: e.scalar_tensor_tensor(out=xo, in0=xt[b], scalar=DN_ALPHA, in1=xo, op0=ALU.mult, op1=ALU.add),
                     reads=[f"xt{b}", "xo"], writes=["xo"])
                ln_tile(xo, stats, mv, rstd, "xo", "st")
                p.op("dve", lambda e: e.tensor_tensor(out=xo, in0=xo, in1=lnw, op=ALU.mult), reads=["xo", "lnw"], writes=["xo"])
                p.op("dve", lambda e: e.tensor_tensor(out=xo, in0=xo, in1=lnb, op=ALU.add), reads=["xo", "lnb"], writes=["xo"])
                p.dma("sp", lambda e: e.dma_start(out=XRES.ap()[tok:tok + 128, :], in_=xo), reads=["xo"], writes=["xres_w2"])

            for t in range(ntiles):
                m3_tile(t, t % 2, 0 if t < 32 else 1)

        p.dma("sp", lambda e: e.dma_start(out=XRES.ap()[0:SEQ, :], in_=x_in.ap()), writes=["xres"])
        p.dma("sp", lambda e: e.dma_start(out=XRES.ap()[SEQ:NT, :], in_=ctx_in.ap()), writes=["xres"])
        for l in range(cfg.get("layers", DEPTH)):
            phase0(l)
            if stages >= 1 and not cfg.get("moe_only"):
                phase1(l, XRES.ap())
            if stages >= 5 and not cfg.get("moe_only"):
                nattn(l, l < DEPTH - 1)
            if stages >= 4 and not cfg.get("noret") and not cfg.get("moe_only"):
                retention(l)
            if stages >= 3 and not cfg.get("nofourier") and not cfg.get("moe_only"):
                fourier(SEQ, 0)
                if l < DEPTH - 1:
                    fourier(CTX, SEQ)
            if stages >= 6 and not cfg.get("moe_only"):
                merge(l, NTILE if l < DEPTH - 1 else 32)
            if stages >= 7:
                moe(l, NTILE if l < DEPTH - 1 else 32)

        p.barrier()
        for i in range(4):
            p.dma("sp", lambda e, i=i: e.dma_start(out=out_t.ap()[i * 1024:(i + 1) * 1024, :],
                                                   in_=XRES.ap()[i * 1024:(i + 1) * 1024, :]),
                  reads=["xres"], writes=["out"])
        p.barrier()
        with nc.Block() as block:
            p.emit(block)
    return nc


_NC_CACHE = {}


def _rope_table():
    t = np.arange(SEQ)
    row, col = t // 64, t % 64
    inv = (10000.0 ** (-np.arange(32, dtype=np.float32) / 32)).astype(np.float32)
    ar = row.astype(np.float32)[:, None] * inv[None, :]
    ac = col.astype(np.float32)[:, None] * inv[None, :]
    C = np.concatenate([np.cos(ar), np.cos(ar), np.cos(ac), np.cos(ac)], axis=1)
    S = np.concatenate([np.sin(ar), np.sin(ar), np.sin(ac), np.sin(ac)], axis=1)
    return np.ascontiguousarray(np.concatenate([C, S], axis=1), dtype=np.float32)


def _na_tables():
    DR = np.zeros((5, 128, 5, 128), np.int64)
    DC = np.zeros((5, 128, 5, 128), np.int64)
    M = np.zeros((5, 128, 5, 128), np.float32)
    pp = np.arange(128)
    for cls, tq in enumerate((0, 1, 2, 30, 31)):
        k0 = min(max(tq - 2, 0), 27)
        for j in range(5):
            kt = k0 + j
            kr = (2 * kt + pp // 64)[:, None]
            kc = (pp % 64)[:, None]
            qr = (2 * tq + pp // 64)[None, :]
            qc = (pp % 64)[None, :]
            r0 = np.clip(qr - 4, 0, 56)
            c0 = np.clip(qc - 8, 0, 48)
            valid = (kr >= r0) & (kr < r0 + 8) & (kc >= c0) & (kc < c0 + 16)
            DR[cls, :, j, :] = np.clip(kr - qr + 7, 0, 14)
            DC[cls, :, j, :] = np.clip(kc - qc, -15, 15) + 15
            M[cls, :, j, :] = valid
    return DR, DC, M


def _na_bias_layout(rpb):
    DR, DC, M = _na_tables()
    g = rpb[:, :, DR, DC]
    g = np.transpose(g, (0, 1, 3, 2, 4, 5))
    L = rpb.shape[0]
    return (np.ascontiguousarray(g.reshape(L, 8, 128, 3200), dtype=np.float32),
            np.ascontiguousarray(np.transpose(M, (1, 0, 2, 3)).reshape(128, 3200), dtype=np.float32))


def _wlay(w, c):
    L, E, K, N = w.shape
    return np.ascontiguousarray(w.reshape(L, E, c, 128, N).transpose(0, 1, 3, 2, 4)).reshape(L, E * 128, c * N)


def _run(inputs, batches, cfg=None):
    f32 = lambda a: np.ascontiguousarray(np.asarray(a), dtype=np.float32)
    g = {k: f32(v) for k, v in inputs.items()}
    key = str(sorted((cfg or {}).items()))
    if key not in _NC_CACHE:
        _NC_CACHE[key] = build_program(dict(cfg or {}))
    nc = _NC_CACHE[key]
    nab, nam = _na_bias_layout(g["na_rpb"])
    shared = {
        "ada_w": g["ada_w"], "ada_b": g["ada_b"], "w_in": g["w_in"],
        "ret_decay": np.ascontiguousarray(np.concatenate([g["ret_decay_fwd"], g["ret_decay_bwd"]], axis=1)),
        "ret_gn_w": g["ret_gn_w"], "rope": _rope_table(), "na_bias": nab, "na_mask": nam,
        "w_o_na": g["w_o_na"], "w_fourier": g["w_fourier"], "w_o_ret": g["w_o_ret"], "w_out": g["w_out"],
        "ln_mix_w": g["ln_mix_w"], "ln_mix_b": g["ln_mix_b"], "router_w": g["router_w"], "router_bias": g["router_bias"],
        "sh_w_gate": g["sh_w_gate"], "sh_w_up": g["sh_w_up"], "sh_w_down": g["sh_w_down"],
        "ln_ffn_w": g["ln_ffn_w"], "ln_ffn_b": g["ln_ffn_b"],
    }
    for nm, c in (("exp_w_gate", 8), ("exp_w_up", 8), ("exp_w_down", 2)):
        wl = _wlay(g[nm], c)
        for i in range(wl.shape[0]):
            shared[f"{nm}{i}"] = wl[i]
    in_maps = []
    for b in batches:
        m = dict(shared)
        m["x"] = g["x"][b]
        m["ctx"] = g["ctx"][b]
        m["cvec"] = np.ascontiguousarray(np.stack([g["c"][b], g["c_ctx"]]))
        in_maps.append(m)
    res = run_bass_kernel_spmd(nc, in_maps, core_ids=list(range(len(batches))))
    return np.stack([np.asarray(res.results[i]["out"], dtype=np.float32) for i in range(len(batches))], axis=0)


def kernel(**inputs):
    return _run(inputs, list(range(8)))
```

```python
import numpy as np
import concourse.bass as bass
import concourse.mybir as mybir
from concourse.bass_utils import run_bass_kernel_spmd

F32 = mybir.dt.float32
BF16 = mybir.dt.bfloat16
I32 = mybir.dt.int32
AF = mybir.ActivationFunctionType
ALU = mybir.AluOpType
AX = mybir.AxisListType

D = 1024
SEQ = 4096
CTX = 256
NT = SEQ + CTX
NTILE = NT // 128
DEPTH = 2
INW = 8192
LN_EPS = 1e-6
DN_ALPHA = (2.0 * DEPTH) ** 0.25


class P:
    def __init__(self, nc):
        self.nc = nc
        self.ops = {k: [] for k in ("pe", "act", "dve", "pool", "sp")}
        self.cnt = {k: 0 for k in self.ops}
        self.sem = {}
        self.waited = {k: {} for k in self.ops}
        self.last_w = {}
        self.readers = {}
        self.dma_sems = {"sp": [], "pool": []}
        self.dma_rr = {"sp": 0, "pool": 0}
        self.dma_val = {}
        self.dma_last = {}
        self.final_tokens = []
        self.pending = {k: [] for k in self.ops}

    def setup_sems(self, stack):
        for k in self.ops:
            self.sem[k] = stack.enter_context(self.nc.semaphore("e_" + k))
        for q in ("sp", "pool"):
            for i in range(12):
                s = stack.enter_context(self.nc.semaphore(f"d_{q}{i}"))
                self.dma_sems[q].append(s)
                self.dma_val[id(s)] = 0
                self.dma_last[id(s)] = None

    def _deps(self, eng, reads, writes):
        toks = []
        for r in reads:
            toks.extend(self._lw(r))
        for w in writes:
            toks.extend(self._lw(w))
            toks.extend(self.readers.get(w, ()))
        need = {}
        for (s, v, own) in toks:
            if own == eng and eng == "pe":
                continue
            key = id(s)
            if self.waited[eng].get(key, 0) >= v:
                continue
            if key not in need or need[key][1] < v:
                need[key] = (s, v)
        for key, (s, v) in need.items():
            self.waited[eng][key] = v
        return list(need.values())

    def _lw(self, key):
        t = self.last_w.get(key)
        if t is None:
            return []
        return t if isinstance(t, list) else [t]

    def _commit(self, tok, reads, writes):
        is_dma = tok[2].startswith("dma_")
        for w in writes:
            prev = self._lw(w)
            if is_dma and prev and all(t[2].startswith("dma_") for t in prev) and not self.readers.get(w):
                self.last_w[w] = prev + [tok]
            else:
                self.last_w[w] = [tok]
            self.readers[w] = []
        for r in reads:
            self.readers.setdefault(r, []).append(tok)

    def barrier(self):
        for eng in self.ops:
            w = []
            for k in self.ops:
                if k != eng and self.cnt[k] > 0 and self.waited[eng].get(id(self.sem[k]), 0) < self.cnt[k]:
                    w.append((self.sem[k], self.cnt[k]))
                    self.waited[eng][id(self.sem[k])] = self.cnt[k]
            for q in ("sp", "pool"):
                for ds in self.dma_sems[q]:
                    v = self.dma_val[id(ds)]
                    if v > 0 and self.waited[eng].get(id(ds), 0) < v:
                        w.append((ds, v))
                        self.waited[eng][id(ds)] = v
            self.pending[eng].extend(w)
        self.last_w = {}
        self.readers = {}

    def op(self, eng, fn, reads=(), writes=()):
        waits = self.pending[eng] + self._deps(eng, reads, writes)
        self.pending[eng] = []
        self.cnt[eng] += 1
        v = self.cnt[eng]
        s = self.sem[eng]
        self.ops[eng].append((waits, fn, s, 1))
        tok = (s, v, eng)
        self._commit(tok, reads, writes)
        return tok

    def dma(self, q, fn, reads=(), writes=(), inc=16):
        waits = self.pending[q] + self._deps(q, reads, writes)
        self.pending[q] = []
        i = self.dma_rr[q]
        self.dma_rr[q] = (i + 1) % len(self.dma_sems[q])
        s = self.dma_sems[q][i]
        prev = self.dma_val[id(s)]
        if prev > 0 and self.waited[q].get(id(s), 0) < prev:
            waits.append((s, prev))
            self.waited[q][id(s)] = prev
        self.dma_val[id(s)] = prev + inc
        v = prev + inc
        self.ops[q].append((waits, fn, s, inc))
        tok = (s, v, "dma_" + q)
        self._commit(tok, reads, writes)
        return tok

    def emit(self, block):
        def run(eng_name):
            def body(e):
                for waits, fn, s, inc in self.ops[eng_name]:
                    for (ws, wv) in waits:
                        e.wait_ge(ws, wv)
                    fn(e).then_inc(s, inc)
                if eng_name == "sp":
                    for q in ("sp", "pool"):
                        for ds in self.dma_sems[q]:
                            v = self.dma_val[id(ds)]
                            if v > 0:
                                e.wait_ge(ds, v)
                    for k in self.ops:
                        if k != "sp" and self.cnt[k] > 0:
                            e.wait_ge(self.sem[k], self.cnt[k])
            return body
        block.tensor(run("pe"))
        block.scalar(run("act"))
        block.vector(run("dve"))
        block.gpsimd(run("pool"))
        block.sync(run("sp"))


def build_program(cfg):
    from contextlib import ExitStack
    nc = bass.Bass("TRN2", target_bir_lowering=False)
    p = P(nc)
    stages = cfg.get("stages", 99)
    dbg = cfg.get("debug", ())

    def din(name, shape, dt=F32):
        return nc.dram_tensor(name, list(shape), dt, kind="ExternalInput")

    x_in = din("x", [SEQ, D])
    ctx_in = din("ctx", [CTX, D])
    cvec = din("cvec", [2, D])
    WD = cfg.get("wdepth", DEPTH)
    ada_w = din("ada_w", [WD, D, 6 * D])
    ada_b = din("ada_b", [WD, 6 * D])
    w_in = din("w_in", [WD, D, INW])
    ret_decay = din("ret_decay", [WD, 8])
    ret_gn_w = din("ret_gn_w", [WD, 1024])
    rope_t = din("rope", [SEQ, 256])
    na_bias = din("na_bias", [WD, 8, 128, 3200])
    na_mask = din("na_mask", [128, 3200])
    w_o_na = din("w_o_na", [WD, 512, D])
    w_fourier = din("w_fourier", [WD, 512, D])
    w_o_ret = din("w_o_ret", [WD, 1024, D])
    w_out = din("w_out", [WD, D, D])
    ln_mix_w = din("ln_mix_w", [WD, D])
    ln_mix_b = din("ln_mix_b", [WD, D])
    router_w = din("router_w", [WD, D, 256])
    router_bias = din("router_bias", [WD, 256])
    exp_w_gate = [din(f"exp_w_gate{i}", [256 * 128, 2048]) for i in range(WD)]
    exp_w_up = [din(f"exp_w_up{i}", [256 * 128, 2048]) for i in range(WD)]
    exp_w_down = [din(f"exp_w_down{i}", [256 * 128, 2048]) for i in range(WD)]
    sh_w_gate = din("sh_w_gate", [WD, D, 256])
    sh_w_up = din("sh_w_up", [WD, D, 256])
    sh_w_down = din("sh_w_down", [WD, 256, D])
    ln_ffn_w = din("ln_ffn_w", [WD, D])
    ln_ffn_b = din("ln_ffn_b", [WD, D])
    out_t = nc.dram_tensor("out", [SEQ, D], F32, kind="ExternalOutput")

    def scratch(name, shape, dt=BF16):
        kind = "ExternalOutput" if name in dbg else "Internal"
        return nc.dram_tensor(name, list(shape), dt, kind=kind)

    QAT = scratch("QAT", [512, NT])
    KAT = scratch("KAT", [512, NT])
    UBT = scratch("UBT", [512, NT])
    VA = scratch("VA", [NT, 512])
    QR = scratch("QR", [NT, 512])
    KR = scratch("KR", [NT, 512])
    VR = scratch("VR", [NT, 1024])
    GR = scratch("GR", [NT, 1024])
    GL = scratch("GL", [NT, 3072])
    MODS = scratch("MODS", [2, 6 * D], F32)
    XRES = scratch("XRES", [NT, D], F32)

    with ExitStack() as st:
        p.setup_sems(st)

        def sb(name, shape, dt):
            return st.enter_context(nc.sbuf_tensor(name, list(shape), dt))

        def ps(name, shape, dt=F32):
            return st.enter_context(nc.psum_tensor(name, list(shape), dt))


        ident = sb("ident", [128, 128], BF16)
        ones_f = sb("ones_f", [128, 128], F32)
        mods = sb("mods", [128, 2, 6 * D], F32)
        ARENA_N = 79 * 1024
        arena = sb("arena", [128, ARENA_N], BF16)
        pbig = [ps(f"pbig{i}", [128, 1024], F32) for i in range(2)]
        pb45 = [ps(f"pb{i}", [128, 512], F32) for i in (4, 5)]
        pbank = [pbig[0][:, 0:512], pbig[0][:, 512:1024], pbig[1][:, 0:512], pbig[1][:, 512:1024], pb45[0][:, :], pb45[1][:, :]]
        ptr = [ps(f"ptr{i}", [128, 1024], BF16) for i in range(2)]
        aoff = [0]

        def areset():
            p.barrier()
            aoff[0] = 0

        def alloc(shape, dt, parts=128):
            n = 1
            for d_ in shape:
                n *= d_
            n16 = n * (2 if dt in (F32, I32) else 1)
            assert aoff[0] + n16 <= ARENA_N, (aoff[0], n16)
            v = arena[0:parts, aoff[0]:aoff[0] + n16]
            aoff[0] += (n16 + 31) // 32 * 32
            if dt != BF16:
                v = v.bitcast(dt)
            if len(shape) == 2:
                v = v.rearrange("q (a b) -> q a b", a=shape[0])
            elif len(shape) == 3:
                v = v.rearrange("q (a b c) -> q a b c", a=shape[0], b=shape[1])
            return v

        p.op("pool", lambda e: e.memset(ones_f[:], 1.0), writes=["ones_f"])
        p.op("pool", lambda e: e.memset(ident[:], 1.0), writes=["ident"])
        p.op("pool", lambda e: e.affine_select(out=ident[:], in_=ident[:], pattern=[[-1, 128]],
                                              compare_op=ALU.is_equal, fill=0.0, base=0,
                                              channel_multiplier=1),
             reads=["ident"], writes=["ident"])

        ev = [0]

        def copy_any(out_ap, in_ap, reads, writes):
            ev[0] += 1
            if ev[0] % 2 == 0:
                p.op("act", lambda e: e.copy(out=out_ap, in_=in_ap), reads=reads, writes=writes)
            else:
                p.op("dve", lambda e: e.tensor_copy(out=out_ap, in_=in_ap), reads=reads, writes=writes)

        bank_rr = [0]

        def next_bank():
            bank_rr[0] = (bank_rr[0] + 1) % 6
            i = bank_rr[0]
            return pbank[i], f"pb{i}"

        def phase0(l):
            areset()
            csb = alloc([2, 8], F32)
            crep = alloc([2, 8, 128], F32)
            adaw = [alloc([8, 512], F32) for _ in range(2)]
            bia = [alloc([512], F32, parts=1) for _ in range(2)]
            for j in range(2):
                p.dma("sp", lambda e, j=j: e.dma_start(out=csb[:, j, :], in_=cvec.ap()[j, :].rearrange("(c q) -> q c", q=128),
                                                       allow_slow_non_contiguous=True), writes=["csb"])
            p.op("act", lambda e: e.activation(out=csb, in_=csb, func=AF.Silu), reads=["csb"], writes=["csb"])
            for j in range(2):
                for k in range(8):
                    p.op("dve", lambda e, j=j, k=k: e.tensor_scalar_mul(
                        out=crep[:, j, k, :], in0=ones_f[:], scalar1=csb[:, j, k:k + 1]),
                        reads=["csb", "ones_f"], writes=["crep"])
            for nb in range(12):
                wb = adaw[nb % 2]
                bb = bia[nb % 2]
                p.dma("sp", lambda e, nb=nb, wb=wb: e.dma_start(
                    out=wb, in_=ada_w.ap()[l, :, nb * 512:(nb + 1) * 512].rearrange("(c q) n -> q c n", q=128)),
                    writes=[f"adaw{nb % 2}"])
                p.dma("sp", lambda e, nb=nb, bb=bb: e.dma_start(
                    out=bb, in_=ada_b.ap()[l:l + 1, nb * 512:(nb + 1) * 512]), writes=[f"bia{nb % 2}"])
                for j in range(2):
                    bank, bkey = next_bank()
                    for k in range(8):
                        p.op("pe", lambda e, j=j, k=k, wb=wb, bank=bank: e.matmul(
                            bank, lhsT=crep[:, j, k, :], rhs=wb[:, k, :], start=(k == 0), stop=False),
                            reads=["crep", f"adaw{nb % 2}"], writes=[bkey])
                    p.op("pe", lambda e, bb=bb, bank=bank: e.matmul(
                        bank, lhsT=ones_f[0:1, :], rhs=bb, start=False, stop=True),
                        reads=["ones_f", f"bia{nb % 2}"], writes=[bkey])
                    is_scale = (nb // 2) in (1, 4)
                    if is_scale:
                        p.op("dve", lambda e, j=j, nb=nb, bank=bank: e.tensor_scalar_add(
                            out=mods[:, j, nb * 512:(nb + 1) * 512], in0=bank, scalar1=1.0),
                            reads=[bkey], writes=["mods"])
                    else:
                        p.op("dve", lambda e, j=j, nb=nb, bank=bank: e.tensor_copy(
                            out=mods[:, j, nb * 512:(nb + 1) * 512], in_=bank),
                            reads=[bkey], writes=["mods"])
            if "MODS" in dbg:
                p.dma("sp", lambda e: e.dma_start(out=MODS.ap(), in_=mods[0:1, :, :]), reads=["mods"], writes=["MODS"])

        def ln_tile(xt_ap, stats_ap, mv_ap, rstd_ap, kx, ks):
            for c in range(2):
                p.op("dve", lambda e, c=c: e.bn_stats(out=stats_ap[:, c, :], in_=xt_ap[:, c * 512:(c + 1) * 512]),
                     reads=[kx], writes=[ks])
            p.op("dve", lambda e: e.bn_aggr(out=mv_ap, in_=stats_ap), reads=[ks], writes=[ks + "mv"])
            p.op("dve", lambda e: e.tensor_scalar_add(out=rstd_ap, in0=mv_ap[:, 1:2], scalar1=LN_EPS),
                 reads=[ks + "mv"], writes=[ks + "r"])
            p.op("act", lambda e: e.sqrt(out=rstd_ap, in_=rstd_ap), reads=[ks + "r"], writes=[ks + "r"])
            p.op("dve", lambda e: e.reciprocal(out=rstd_ap, in_=rstd_ap), reads=[ks + "r"], writes=[ks + "r"])
            p.op("dve", lambda e: e.tensor_scalar(out=xt_ap, in0=xt_ap, scalar1=mv_ap[:, 0:1],
                                                  scalar2=rstd_ap[:, 0:1], op0=ALU.subtract, op1=ALU.mult),
                 reads=[kx, ks + "mv", ks + "r"], writes=[kx])

        def phase1(l, xres):
            areset()
            hT = alloc([8, NT], BF16)
            wbuf = [alloc([8, 1024], BF16) for _ in range(2)]
            xt = [alloc([1024], F32) for _ in range(2)]
            xn = [alloc([1024], BF16) for _ in range(2)]
            stats = [alloc([2, 6], F32) for _ in range(2)]
            mv = [alloc([2], F32) for _ in range(2)]
            rstd = [alloc([1], F32) for _ in range(2)]
            stg = [alloc([1024], BF16) for _ in range(4)]

            def load_w(g):
                wb = wbuf[g % 2]
                p.dma("pool", lambda e: e.dma_start(
                    out=wb, in_=w_in.ap()[l, :, g * 1024:(g + 1) * 1024].rearrange("(c q) n -> q c n", q=128)),
                    writes=[f"wbuf{g % 2}"])

            if stages >= 2:
                load_w(0)
            for t in range(NTILE):
                b = t % 2
                j = 0 if t < 32 else 1
                src = xres[t * 128:(t + 1) * 128, :]
                p.dma("sp", lambda e, b=b, src=src: e.dma_start(out=xt[b], in_=src), reads=["xres"], writes=[f"xt{b}"])
                if not cfg.get("noln"):
                    ln_tile(xt[b], stats[b], mv[b], rstd[b], f"xt{b}", f"st{b}")
                p.op("dve", lambda e, b=b, j=j: e.tensor_tensor(out=xt[b], in0=xt[b], in1=mods[:, j, D:2 * D], op=ALU.mult),
                     reads=[f"xt{b}", "mods"], writes=[f"xt{b}"])
                p.op("dve", lambda e, b=b, j=j: e.tensor_tensor(out=xn[b], in0=xt[b], in1=mods[:, j, 0:D], op=ALU.add),
                     reads=[f"xt{b}", "mods"], writes=[f"xn{b}"])
                bv = ptr[b]
                if cfg.get("notr"):
                    continue
                for k in range(8):
                    p.op("pe", lambda e, k=k, b=b, bv=bv: e.transpose(
                        bv[:, k * 128:(k + 1) * 128], xn[b][:, k * 128:(k + 1) * 128], ident[:]),
                        reads=[f"xn{b}", "ident"], writes=[f"ptr{b}"])
                if cfg.get("nocp"):
                    continue
                copy_any(hT[:, :, t * 128:(t + 1) * 128], bv[:, :].rearrange("q (k n) -> q k n", k=8),
                         [f"ptr{b}"], [f"hT{t}a", f"hT{t}b"])

            stg_rr = [0]

            def next_stg():
                stg_rr[0] = (stg_rr[0] + 1) % 4
                return stg[stg_rr[0]], f"stg{stg_rr[0]}"

            def gemm_tm(g, col0, ncols, dst, dst_col0):
                wb = wbuf[g % 2]
                for t in range(NTILE):
                    sg, sk = next_stg()
                    for cb in range(ncols // 512):
                        bank, bkey = next_bank()
                        for k in range(8):
                            p.op("pe", lambda e, k=k, cb=cb, bank=bank, t=t: e.matmul(
                                bank, lhsT=hT[:, k, t * 128:(t + 1) * 128],
                                rhs=wb[:, k, col0 + cb * 512:col0 + (cb + 1) * 512], start=(k == 0), stop=(k == 7)),
                                reads=[f"hT{t}a", f"hT{t}b", f"wbuf{g % 2}"], writes=[bkey])
                        copy_any(sg[:, cb * 512:(cb + 1) * 512], bank, [bkey], [sk])
                    p.dma("sp", lambda e, sg=sg, t=t: e.dma_start(
                        out=dst.ap()[t * 128:(t + 1) * 128, dst_col0:dst_col0 + ncols], in_=sg[:, 0:ncols]),
                        reads=[sk], writes=[dst.name])

            def gemm_fm(g, col0, ncols, dst):
                wb = wbuf[g % 2]
                for fb in range(ncols // 128):
                    for tg in range(0, NT, 1024):
                        ntok = min(1024, NT - tg)
                        sg, sk = next_stg()
                        for tb in range(0, ntok, 512):
                            nn = min(512, ntok - tb)
                            bank, bkey = next_bank()
                            rk = []
                            for tt in range((tg + tb) // 128, (tg + tb + nn) // 128):
                                rk += [f"hT{tt}a", f"hT{tt}b"]
                            for k in range(8):
                                p.op("pe", lambda e, k=k, bank=bank, tb=tb, nn=nn, tg=tg, fb=fb: e.matmul(
                                    bank[:, 0:nn], lhsT=wb[:, k, col0 + fb * 128:col0 + (fb + 1) * 128],
                                    rhs=hT[:, k, tg + tb:tg + tb + nn], start=(k == 0), stop=(k == 7)),
                                    reads=rk + [f"wbuf{g % 2}"], writes=[bkey])
                            copy_any(sg[:, tb:tb + nn], bank[:, 0:nn], [bkey], [sk])
                        p.dma("sp", lambda e, sg=sg, tg=tg, ntok=ntok, fb=fb: e.dma_start(
                            out=dst.ap()[fb * 128:(fb + 1) * 128, tg:tg + ntok], in_=sg[:, 0:ntok]),
                            reads=[sk], writes=[dst.name])

            if stages < 2:
                return
            plan = [
                [("fm", 0, 512, QAT, 0), ("fm", 512, 512, KAT, 0)],
                [("tm", 0, 512, VA, 0), ("fm", 512, 512, UBT, 0)],
                [("tm", 0, 512, QR, 0), ("tm", 512, 512, KR, 0)],
                [("tm", 0, 1024, VR, 0)],
                [("tm", 0, 1024, GR, 0)],
                [("tm", 0, 1024, GL, 0)],
                [("tm", 0, 1024, GL, 1024)],
                [("tm", 0, 1024, GL, 2048)],
            ]
            for g in range(8):
                if g + 1 < min(8, cfg.get("ngroups", 8)):
                    load_w(g + 1)
                if g >= cfg.get("ngroups", 8):
                    break
                for (mode, c0, ncol, dst, dc0) in plan[g]:
                    if mode == "tm":
                        gemm_tm(g, c0, ncol, dst, dc0)
                    else:
                        gemm_fm(g, c0, ncol, dst)


        YFNT = scratch("YFNT", [512, NT])
        MAGIC = 12582912.0

        def gen_cs(dst_c, dst_s, cols, nval, nvs, nmod, tmps, tk, dkeys):
            y, r, t_ = tmps
            p.op("dve", lambda e: e.tensor_scalar(out=y, in0=cols, scalar1=nvs, scalar2=MAGIC, op0=ALU.mult, op1=ALU.add),
                 reads=["fconst"], writes=[tk + "y"])
            p.op("dve", lambda e: e.tensor_scalar(out=r, in0=y, scalar1=MAGIC, scalar2=float(nmod), op0=ALU.subtract, op1=ALU.mult),
                 reads=[tk + "y"], writes=[tk + "r"])
            p.op("dve", lambda e: e.scalar_tensor_tensor(out=t_, in0=cols, scalar=nval, in1=r, op0=ALU.mult, op1=ALU.subtract),
                 reads=[tk + "r", "fconst"], writes=[tk + "t"])
            p.op("dve", lambda e: e.scalar_tensor_tensor(out=y, in0=t_, scalar=-1.0, in1=t_, op0=ALU.mult, op1=ALU.max),
                 reads=[tk + "t"], writes=[tk + "y"])
            p.op("act", lambda e: e.activation(out=dst_s, in_=t_, func=AF.Sin, scale=float(2 * np.pi / nmod)),
                 reads=[tk + "t"], writes=[dkeys[1]])
            p.op("act", lambda e: e.activation(out=dst_c, in_=y, func=AF.Sin, scale=float(-2 * np.pi / nmod), bias=float(np.pi / 2)),
                 reads=[tk + "y"], writes=[dkeys[0]])

        def fourier(N, tok0):
            areset()
            nch = N // 128
            W = min(512, N)
            ubt = alloc([4, N], BF16)
            ucs = alloc([nch, 4, 2 * 128], BF16)
            cs128 = alloc([256], BF16)
            colf = alloc([N], F32)
            nval = alloc([nch], F32)
            nvs = alloc([nch], F32)
            nvs128 = alloc([1], F32)
            tmps = [[alloc([512], F32) for _ in range(3)] for _ in range(2)]
            cblk = [alloc([512], BF16) for _ in range(2)]
            sblk = [alloc([512], BF16) for _ in range(2)]
            fstg = [alloc([512], BF16) for _ in range(4)]
            coli = alloc([N], I32)
            nvi = alloc([nch], I32)
            p.op("pool", lambda e: e.iota(coli, pattern=[[1, N]], base=0, channel_multiplier=0), writes=["coli"])
            p.op("pool", lambda e: e.iota(nvi, pattern=[[128, nch]], base=0, channel_multiplier=1), writes=["nvi"])
            p.op("dve", lambda e: e.tensor_copy(out=colf, in_=coli), reads=["coli"], writes=["fconst"])
            p.op("dve", lambda e: e.tensor_copy(out=nval, in_=nvi), reads=["nvi"], writes=["fconst"])
            p.op("dve", lambda e: e.tensor_scalar_mul(out=nvs, in0=nval, scalar1=1.0 / N), reads=["fconst"], writes=["fconst"])
            p.op("dve", lambda e: e.tensor_scalar_mul(out=nvs128, in0=nval[:, 0:1], scalar1=1.0 / 128), reads=["fconst"], writes=["fconst"])
            for g in range(4):
                p.dma("sp", lambda e, g=g: e.dma_start(out=ubt[:, g, :], in_=UBT.ap()[g * 128:(g + 1) * 128, tok0:tok0 + N]),
                      reads=["UBT"], writes=[f"ubt{g}"])
            gen_cs(cs128[:, 0:128], cs128[:, 128:256], colf[:, 0:128], nval[:, 0:1], nvs128[:, 0:1], 128,
                   [tm[:, 0:128] for tm in tmps[0]], "gt0", ["cs128", "cs128"])
            for i in range(nch):
                for gp in range(2):
                    bank, bkey = next_bank()
                    for gg in range(2):
                        g = gp * 2 + gg
                        p.op("pe", lambda e, g=g, gg=gg, i=i, bank=bank: e.matmul(
                            bank[:, gg * 256:(gg + 1) * 256], lhsT=ubt[:, g, i * 128:(i + 1) * 128], rhs=cs128,
                            start=True, stop=True), reads=[f"ubt{g}", "cs128"], writes=[bkey])
                    bv = bank.rearrange("q (g s c) -> q g s c", g=2, s=2)
                    ov = ucs[:, i, gp * 2:gp * 2 + 2, :].rearrange("q g (s c) -> q g s c", s=2)
                    p.op("dve", lambda e, bv=bv, ov=ov: e.tensor_copy(out=ov[:, :, 0, :], in_=bv[:, :, 0, :]),
                         reads=[bkey], writes=[f"ucs{i}c{gp}"])
                    p.op("dve", lambda e, bv=bv, ov=ov: e.tensor_scalar_mul(out=ov[:, :, 1, :], in0=bv[:, :, 1, :], scalar1=-1.0),
                         reads=[bkey], writes=[f"ucs{i}s{gp}"])
            scale = float(1.0 / np.sqrt(N * 128.0))
            blk = 0
            for mg in range(N // W):
                for i in range(nch):
                    b = blk % 2
                    blk += 1
                    gen_cs(cblk[b][:, 0:W], sblk[b][:, 0:W], colf[:, mg * W:(mg + 1) * W], nval[:, i:i + 1], nvs[:, i:i + 1], N,
                           [tm[:, 0:W] for tm in tmps[b]], f"gt{b}", [f"cblk{b}", f"sblk{b}"])
                    for g in range(4):
                        rk = [f"ucs{i}c{g // 2}", f"ucs{i}s{g // 2}", f"cblk{b}", f"sblk{b}"]
                        p.op("pe", lambda e, g=g, i=i, b=b: e.matmul(
                            pbank[g][:, 0:W], lhsT=ucs[:, i, g, 0:128], rhs=cblk[b][:, 0:W], start=(i == 0), stop=False),
                            reads=rk, writes=[f"pb{g}"])
                        p.op("pe", lambda e, g=g, i=i, b=b: e.matmul(
                            pbank[g][:, 0:W], lhsT=ucs[:, i, g, 128:256], rhs=sblk[b][:, 0:W], start=False, stop=(i == nch - 1)),
                            reads=rk, writes=[f"pb{g}"])
                for g in range(4):
                    if g % 2 == 0:
                        p.op("act", lambda e, g=g: e.mul(out=fstg[g][:, 0:W], in_=pbank[g][:, 0:W], mul=scale),
                             reads=[f"pb{g}"], writes=[f"fstg{g}"])
                    else:
                        p.op("dve", lambda e, g=g: e.tensor_scalar_mul(out=fstg[g][:, 0:W], in0=pbank[g][:, 0:W], scalar1=scale),
                             reads=[f"pb{g}"], writes=[f"fstg{g}"])
                    p.dma("sp", lambda e, g=g, mg=mg: e.dma_start(
                        out=YFNT.ap()[g * 128:(g + 1) * 128, tok0 + mg * W:tok0 + (mg + 1) * W], in_=fstg[g][:, 0:W]),
                        reads=[f"fstg{g}"], writes=["YFNT"])


        YRETT = scratch("YRETT", [1024, NT])
        QS = 128.0 ** -0.5
        LNQS = float(np.log(QS))
        GN_EPS = 1e-5

        def retention(l):
            areset()
            dec = alloc([8], F32)
            lg = alloc([8], F32)
            gC = alloc([8], F32)
            diffi = alloc([128], I32)
            diff = alloc([128], F32)
            rp = alloc([128], F32)
            rn = alloc([128], F32)
            ef = alloc([128], F32)
            eb = alloc([128], F32)
            pci = alloc([2], I32)
            pc = alloc([2], F32)
            cri = alloc([2, 128], I32)
            cr = alloc([2, 128], F32)
            dmask = alloc([4, 128], F32)
            qdf = alloc([4, 128], F32)
            qdb = alloc([4, 128], F32)
            kdf = alloc([4], F32)
            kdb = alloc([4], F32)
            gnw = alloc([1024], F32)
            Sf = alloc([4, 256], F32)
            Sb = alloc([4, 256], F32)
            Sf16 = alloc([4, 256], BF16)
            Sbprev = alloc([32, 1024], BF16)
            qt = [alloc([512], BF16) for _ in range(2)]
            kt = [alloc([512], BF16) for _ in range(2)]
            vt = [alloc([1024], BF16) for _ in range(2)]
            gt = [alloc([1024], BF16) for _ in range(2)]
            rt = [alloc([256], F32) for _ in range(2)]
            t1 = alloc([512], F32)
            t2 = alloc([512], F32)
            q16 = alloc([512], BF16)
            k16 = alloc([512], BF16)
            ks16 = alloc([512], BF16)
            qkT = alloc([8, 128], BF16)
            PT = alloc([4, 128], BF16)
            qfT = alloc([4, 128], BF16)
            qbT = alloc([4, 128], BF16)
            rstat = alloc([4, 6], F32)
            rmv = alloc([4, 2], F32)
            rr = alloc([4], F32)
            yn = alloc([1024], F32)
            sg = alloc([1024], F32)
            y16 = alloc([1024], BF16)
            yT = alloc([8, 128], BF16)

            p.dma("sp", lambda e: e.dma_start(out=dec, in_=ret_decay.ap()[l, :].partition_broadcast(128)), writes=["dec"])
            p.dma("sp", lambda e: e.dma_start(out=gnw, in_=ret_gn_w.ap()[l, :].partition_broadcast(128)), writes=["gnw"])
            p.op("act", lambda e: e.activation(out=lg, in_=dec, func=AF.Exp, scale=-1.0), reads=["dec"], writes=["lg"])
            p.op("act", lambda e: e.activation(out=lg, in_=lg, func=AF.Ln, bias=1.0), reads=["lg"], writes=["lg"])
            p.op("dve", lambda e: e.tensor_scalar_mul(out=lg, in0=lg, scalar1=-1.0), reads=["lg"], writes=["lg"])
            p.op("act", lambda e: e.activation(out=gC, in_=lg, func=AF.Exp, scale=128.0), reads=["lg"], writes=["gC"])
            p.op("pool", lambda e: e.iota(diffi, pattern=[[1, 128]], base=0, channel_multiplier=-1), writes=["diffi"])
            p.op("pool", lambda e: e.iota(pci[:, 0:1], pattern=[[0, 1]], base=127, channel_multiplier=-1), writes=["pci"])
            p.op("pool", lambda e: e.iota(pci[:, 1:2], pattern=[[0, 1]], base=0, channel_multiplier=1), reads=["pci"], writes=["pci"])
            p.op("pool", lambda e: e.iota(cri[:, 0, :], pattern=[[1, 128]], base=1, channel_multiplier=0), writes=["cri"])
            p.op("pool", lambda e: e.iota(cri[:, 1, :], pattern=[[-1, 128]], base=128, channel_multiplier=0), reads=["cri"], writes=["cri"])
            p.op("dve", lambda e: e.tensor_copy(out=diff, in_=diffi), reads=["diffi"], writes=["diff"])
            p.op("dve", lambda e: e.tensor_copy(out=pc, in_=pci), reads=["pci"], writes=["pc"])
            p.op("dve", lambda e: e.tensor_copy(out=cr, in_=cri), reads=["cri"], writes=["cr"])
            p.op("dve", lambda e: e.tensor_scalar_max(out=rp, in0=diff, scalar1=0.0), reads=["diff"], writes=["rp"])
            p.op("dve", lambda e: e.tensor_tensor(out=rn, in0=rp, in1=diff, op=ALU.subtract), reads=["rp", "diff"], writes=["rn"])
            for h in range(4):
                p.op("act", lambda e, h=h: e.activation(out=kdf[:, h:h + 1], in_=pc[:, 0:1], func=AF.Exp, scale=lg[:, h:h + 1]),
                     reads=["pc", "lg"], writes=["kdf"])
                p.op("act", lambda e, h=h: e.activation(out=kdb[:, h:h + 1], in_=pc[:, 1:2], func=AF.Exp, scale=lg[:, 4 + h:5 + h]),
                     reads=["pc", "lg"], writes=["kdb"])
                p.op("act", lambda e, h=h: e.activation(out=qdf[:, h, :], in_=cr[:, 0, :], func=AF.Exp, scale=lg[:, h:h + 1], bias=LNQS),
                     reads=["cr", "lg"], writes=["qdf"])
                p.op("act", lambda e, h=h: e.activation(out=qdb[:, h, :], in_=cr[:, 1, :], func=AF.Exp, scale=lg[:, 4 + h:5 + h], bias=LNQS),
                     reads=["cr", "lg"], writes=["qdb"])
                p.op("act", lambda e, h=h: e.activation(out=ef, in_=rp, func=AF.Exp, scale=lg[:, h:h + 1], bias=LNQS),
                     reads=["rp", "lg"], writes=["ef"])
                p.op("act", lambda e, h=h: e.activation(out=eb, in_=rn, func=AF.Exp, scale=lg[:, 4 + h:5 + h], bias=LNQS),
                     reads=["rn", "lg"], writes=["eb"])
                p.op("pool", lambda e: e.affine_select(out=ef, in_=ef, pattern=[[1, 128]], compare_op=ALU.is_ge, fill=0.0,
                                                      base=0, channel_multiplier=-1), reads=["ef"], writes=["ef"])
                p.op("pool", lambda e: e.affine_select(out=eb, in_=eb, pattern=[[-1, 128]], compare_op=ALU.is_gt, fill=0.0,
                                                      base=0, channel_multiplier=1), reads=["eb"], writes=["eb"])
                p.op("dve", lambda e, h=h: e.tensor_tensor(out=dmask[:, h, :], in0=ef, in1=eb, op=ALU.add),
                     reads=["ef", "eb"], writes=["dmask"])
            p.op("pool", lambda e: e.memset(Sf, 0.0), writes=["Sf"])
            p.op("pool", lambda e: e.memset(Sb, 0.0), writes=["Sb"])
            p.op("pool", lambda e: e.memset(Sf16, 0.0), writes=["Sf16"])

            pbS = pbank[0]
            pbO = [pbank[1], pbank[2]]
            pbK = [pbank[3], pbank[4]]

            def rope(src, dst, rtile, dk):
                sv = src.rearrange("q (h r u d) -> q (h r) u d", h=4, r=2, u=2)
                Cb = rtile[:, 0:128].unsqueeze(1).broadcast_to([128, 4, 128])
                Sv = rtile[:, 128:256].rearrange("q (r u d) -> q r u d", r=2, u=2)
                t1v = t1.rearrange("q (h x) -> q h x", h=4)
                t2v = t2.rearrange("q (h r u d) -> q h r u d", h=4, r=2, u=2)
                s5 = src.rearrange("q (h r u d) -> q h r u d", h=4, r=2, u=2)
                p.op("dve", lambda e: e.tensor_tensor(out=t1v, in0=src.rearrange("q (h x) -> q h x", h=4), in1=Cb, op=ALU.mult),
                     reads=[dk + "src", dk + "rt"], writes=["t1"])
                for u in range(2):
                    for r in range(2):
                        p.op("dve", lambda e, u=u, r=r: e.tensor_tensor(
                            out=t2v[:, :, r, u, :], in0=s5[:, :, r, 1 - u, :],
                            in1=Sv[:, r, u, :].unsqueeze(1).broadcast_to([128, 4, 32]), op=ALU.mult),
                            reads=[dk + "src", dk + "rt"], writes=["t2"])
                t1w = t1.rearrange("q (h r u d) -> q (h r) u d", h=4, r=2, u=2)
                t2w = t2.rearrange("q (h r u d) -> q (h r) u d", h=4, r=2, u=2)
                dw = dst.rearrange("q (h r u d) -> q (h r) u d", h=4, r=2, u=2)
                p.op("dve", lambda e: e.tensor_tensor(out=dw[:, :, 0, :], in0=t1w[:, :, 0, :], in1=t2w[:, :, 0, :], op=ALU.subtract),
                     reads=["t1", "t2"], writes=[dk])
                p.op("dve", lambda e: e.tensor_tensor(out=dw[:, :, 1, :], in0=t1w[:, :, 1, :], in1=t2w[:, :, 1, :], op=ALU.add),
                     reads=["t1", "t2"], writes=[dk])

            def load_k_v(n, tok, b, use_rope, need_q):
                p.dma("sp", lambda e: e.dma_start(out=kt[b], in_=KR.ap()[tok:tok + 128, :]), reads=["KR"], writes=[f"kt{b}"])
                p.dma("sp", lambda e: e.dma_start(out=vt[b], in_=VR.ap()[tok:tok + 128, :]), reads=["VR"], writes=[f"vt{b}"])
                if use_rope:
                    p.dma("sp", lambda e: e.dma_start(out=rt[b], in_=rope_t.ap()[tok:tok + 128, :]), writes=[f"rt{b}"])
                if need_q:
                    p.dma("sp", lambda e: e.dma_start(out=qt[b], in_=QR.ap()[tok:tok + 128, :]), reads=["QR"], writes=[f"qt{b}"])
                    p.dma("sp", lambda e: e.dma_start(out=gt[b], in_=GR.ap()[tok:tok + 128, :]), reads=["GR"], writes=[f"gt{b}"])

            def prep_k(b, use_rope):
                if use_rope:
                    p.last_w["k16src"] = p.last_w.get(f"kt{b}")
                    p.last_w["k16rt"] = p.last_w.get(f"rt{b}")
                    rope(kt[b], k16, rt[b], "k16")
                    p.readers.setdefault(f"kt{b}", []).extend(p._lw("k16"))
                    p.readers.setdefault(f"rt{b}", []).extend(p._lw("k16"))
                else:
                    p.op("dve", lambda e: e.tensor_copy(out=k16, in_=kt[b]), reads=[f"kt{b}"], writes=["k16"])

            def prep_q(b, use_rope):
                if use_rope:
                    p.last_w["q16src"] = p.last_w.get(f"qt{b}")
                    p.last_w["q16rt"] = p.last_w.get(f"rt{b}")
                    rope(qt[b], q16, rt[b], "q16")
                    p.readers.setdefault(f"qt{b}", []).extend(p._lw("q16"))
                    p.readers.setdefault(f"rt{b}", []).extend(p._lw("q16"))
                else:
                    p.op("dve", lambda e: e.tensor_copy(out=q16, in_=qt[b]), reads=[f"qt{b}"], writes=["q16"])

            def kv_update(b, kd, S, gcol, skey):
                for h in range(4):
                    p.op("dve", lambda e, h=h: e.tensor_scalar_mul(out=ks16[:, h * 128:(h + 1) * 128], in0=k16[:, h * 128:(h + 1) * 128],
                                                                   scalar1=kd[:, h:h + 1]), reads=["k16", "kdf", "kdb"], writes=["ks16"])
                for h in range(4):
                    p.op("pe", lambda e, h=h: e.matmul(pbK[h // 2][:, (h % 2) * 256:(h % 2) * 256 + 256], lhsT=ks16[:, h * 128:(h + 1) * 128],
                                                       rhs=vt[b][:, h * 256:(h + 1) * 256], start=True, stop=True),
                         reads=["ks16", f"vt{b}"], writes=[f"pb{3 + h // 2}"])
                for h in range(4):
                    p.op("dve", lambda e, h=h: e.scalar_tensor_tensor(
                        out=S[:, h, :], in0=S[:, h, :], scalar=gC[:, gcol + h:gcol + h + 1],
                        in1=pbK[h // 2][:, (h % 2) * 256:(h % 2) * 256 + 256], op0=ALU.mult, op1=ALU.add),
                        reads=[skey, "gC", f"pb{3 + h // 2}"], writes=[skey])


            def pass1_chunk(n, b, tok, use_rope):
                if True:
                    load_k_v(n, tok, b, use_rope, False)
                    prep_k(b, use_rope)
                    p.op("act", lambda e, n=n: e.copy(out=Sbprev[:, n, :], in_=Sb.rearrange("q h d -> q (h d)")),
                         reads=["Sb"], writes=[f"Sbprev{n}"])
                    kv_update(b, kdb, Sb, 4, "Sb")
            def pass2_chunk(n, b, tok, use_rope):
                if True:
                    load_k_v(n, tok, b, use_rope, True)
                    prep_k(b, use_rope)
                    prep_q(b, use_rope)
                    for h in range(4):
                        p.op("pe", lambda e, h=h: e.transpose(ptr[0][:, h * 128:(h + 1) * 128], q16[:, h * 128:(h + 1) * 128], ident[:]),
                             reads=["q16", "ident"], writes=["ptr0"])
                        p.op("pe", lambda e, h=h: e.transpose(ptr[0][:, (4 + h) * 128:(5 + h) * 128], k16[:, h * 128:(h + 1) * 128], ident[:]),
                             reads=["k16", "ident"], writes=["ptr0"])
                    p.op("act", lambda e: e.copy(out=qkT, in_=ptr[0][:, :].rearrange("q (k n) -> q k n", k=8)),
                         reads=["ptr0"], writes=["qkT"])
                    for h in range(4):
                        p.op("pe", lambda e, h=h: e.matmul(pbS[:, h * 128:(h + 1) * 128], lhsT=qkT[:, 4 + h, :], rhs=qkT[:, h, :],
                                                           start=True, stop=True), reads=["qkT"], writes=["pb0"])
                    p.op("dve", lambda e: e.tensor_tensor(out=PT, in0=pbS[:, :].rearrange("q (h c) -> q h c", h=4), in1=dmask, op=ALU.mult),
                         reads=["pb0", "dmask"], writes=["PT"])
                    p.op("dve", lambda e: e.tensor_tensor(out=qfT, in0=qkT[:, 0:4, :], in1=qdf, op=ALU.mult),
                         reads=["qkT", "qdf"], writes=["qfT"])
                    p.op("dve", lambda e: e.tensor_tensor(out=qbT, in0=qkT[:, 0:4, :], in1=qdb, op=ALU.mult),
                         reads=["qkT", "qdb"], writes=["qbT"])
                    for h in range(4):
                        ob = pbO[h // 2][:, (h % 2) * 256:(h % 2) * 256 + 256]
                        ok = f"pb{1 + h // 2}"
                        p.op("pe", lambda e, h=h, ob=ob: e.matmul(ob, lhsT=PT[:, h, :], rhs=vt[b][:, h * 256:(h + 1) * 256], start=True, stop=False),
                             reads=["PT", f"vt{b}"], writes=[ok])
                        p.op("pe", lambda e, h=h, ob=ob: e.matmul(ob, lhsT=qfT[:, h, :], rhs=Sf16[:, h, :], start=False, stop=False),
                             reads=["qfT", "Sf16"], writes=[ok])
                        p.op("pe", lambda e, h=h, ob=ob, n=n: e.matmul(ob, lhsT=qbT[:, h, :], rhs=Sbprev[:, n, h * 256:(h + 1) * 256],
                                                                       start=False, stop=True),
                             reads=["qbT", f"Sbprev{n}"], writes=[ok])
                    kv_update(b, kdf, Sf, 0, "Sf")
                    p.op("act", lambda e: e.copy(out=Sf16, in_=Sf), reads=["Sf"], writes=["Sf16"])
                    for h in range(4):
                        ob = pbO[h // 2][:, (h % 2) * 256:(h % 2) * 256 + 256]
                        ok = f"pb{1 + h // 2}"
                        p.op("dve", lambda e, h=h, ob=ob: e.bn_stats(out=rstat[:, h, :], in_=ob), reads=[ok], writes=["rstat"])
                    for h in range(4):
                        p.op("dve", lambda e, h=h: e.bn_aggr(out=rmv[:, h, :], in_=rstat[:, h, :]), reads=["rstat"], writes=["rmv"])
                    p.op("dve", lambda e: e.tensor_scalar_add(out=rr, in0=rmv[:, :, 1], scalar1=GN_EPS), reads=["rmv"], writes=["rr"])
                    p.op("act", lambda e: e.sqrt(out=rr, in_=rr), reads=["rr"], writes=["rr"])
                    p.op("dve", lambda e: e.reciprocal(out=rr, in_=rr), reads=["rr"], writes=["rr"])
                    for h in range(4):
                        ob = pbO[h // 2][:, (h % 2) * 256:(h % 2) * 256 + 256]
                        ok = f"pb{1 + h // 2}"
                        p.op("dve", lambda e, h=h, ob=ob: e.tensor_scalar(out=yn[:, h * 256:(h + 1) * 256], in0=ob, scalar1=rmv[:, h, 0:1],
                                                                          scalar2=rr[:, h:h + 1], op0=ALU.subtract, op1=ALU.mult),
                             reads=[ok, "rmv", "rr"], writes=["yn"])
                    p.op("act", lambda e: e.activation(out=sg, in_=gt[b], func=AF.Silu), reads=[f"gt{b}"], writes=["sg"])
                    p.op("dve", lambda e: e.tensor_tensor(out=yn, in0=yn, in1=gnw, op=ALU.mult), reads=["yn", "gnw"], writes=["yn"])
                    p.op("dve", lambda e: e.tensor_tensor(out=y16, in0=yn, in1=sg, op=ALU.mult), reads=["yn", "sg"], writes=["y16"])
                    for k in range(8):
                        p.op("pe", lambda e, k=k: e.transpose(ptr[1][:, k * 128:(k + 1) * 128], y16[:, k * 128:(k + 1) * 128], ident[:]),
                             reads=["y16", "ident"], writes=["ptr1"])
                    p.op("act", lambda e: e.copy(out=yT, in_=ptr[1][:, :].rearrange("q (k n) -> q k n", k=8)), reads=["ptr1"], writes=["yT"])
                    p.dma("sp", lambda e, tok=tok: e.dma_start(out=YRETT.ap()[:, tok:tok + 128].rearrange("(k q) t -> q k t", q=128), in_=yT),
                          reads=["yT"], writes=["YRETT"])

            def segment(tok0, nchunks, use_rope):
                for n in range(nchunks - 1, -1, -1):
                    pass1_chunk(n, n % 2, tok0 + n * 128, use_rope)
                for n in range(nchunks):
                    pass2_chunk(n, n % 2, tok0 + n * 128, use_rope)

            segment(SEQ, 2, False)
            segment(0, 32, True)


        YNAT = scratch("YNAT", [512, NT])

        def nattn(l, with_ctx_q):
            areset()
            qT = alloc([NT], BF16)
            kT = alloc([NT], BF16)
            va_aug = alloc([NTILE, 8, 65], BF16)
            y_tm = alloc([NTILE, 512], BF16)
            tmpv = alloc([17, 512], BF16)
            nabt = alloc([3200], F32)
            maskt = alloc([3200], F32)
            emb = alloc([5, 5, 128], BF16)
            PTs = [alloc([7, 128], BF16) for _ in range(2)]
            rec = alloc([4], F32)
            nstg = alloc([4, 128], BF16)
            p.dma("sp", lambda e: e.dma_start(out=maskt, in_=na_mask.ap()), writes=["maskt"])
            p.op("pool", lambda e: e.memset(va_aug[:, :, :, 64:65], 1.0), writes=["va_ones"])
            for half in range(2):
                p.dma("sp", lambda e, half=half: e.dma_start(
                    out=tmpv, in_=VA.ap()[half * 17 * 128:(half + 1) * 17 * 128, :].rearrange("(t q) d -> q t d", q=128)),
                    reads=["VA"], writes=["tmpv"])
                p.op("dve", lambda e, half=half: e.tensor_copy(
                    out=va_aug[:, half * 17:(half + 1) * 17, :, 0:64], in_=tmpv.rearrange("q t (h d) -> q t h d", h=8)),
                    reads=["tmpv"], writes=[f"va{half}"])
            slot_rr = [0]

            def one(h, tq, keytiles, cls, off):
                bi = tq % 2
                big = pbig[bi][:, :].rearrange("q (j n) -> q j n", j=8)
                PT = PTs[bi]
                nk = len(keytiles)
                for j, ktile in enumerate(keytiles):
                    p.op("pe", lambda e, j=j, ktile=ktile: e.matmul(
                        big[:, j, :], lhsT=kT[off:off + 64, ktile * 128:(ktile + 1) * 128],
                        rhs=qT[off:off + 64, tq * 128:(tq + 1) * 128], start=True, stop=True),
                        reads=["qT", "kT"], writes=[f"pbig{bi}"])
                n0 = min(nk, 4)
                p.op("act", lambda e: e.activation(out=PT[:, 0:n0, :], in_=big[:, 0:n0, :], func=AF.Exp, scale=0.125),
                     reads=[f"pbig{bi}"], writes=[f"PT{bi}"])
                if nk > 4:
                    p.op("act", lambda e: e.activation(out=PT[:, 4:nk, :], in_=big[:, 4:nk, :], func=AF.Exp, scale=0.125),
                         reads=[f"pbig{bi}"], writes=[f"PT{bi}"])
                if cls is not None:
                    p.op("dve", lambda e: e.tensor_tensor(out=PT[:, 0:5, :], in0=PT[:, 0:5, :], in1=emb[:, cls, :, :], op=ALU.mult),
                         reads=[f"PT{bi}", "emb"], writes=[f"PT{bi}"])
                slot_rr[0] = (slot_rr[0] + 1) % 8
                sl = slot_rr[0]
                po = pb45[sl // 4][:, (sl % 4) * 128:(sl % 4) * 128 + 65]
                pk = f"po{sl}"
                for j, ktile in enumerate(keytiles):
                    p.op("pe", lambda e, j=j, ktile=ktile: e.matmul(
                        po, lhsT=PT[:, j, :], rhs=va_aug[:, ktile, h, :], start=(j == 0), stop=(j == nk - 1)),
                        reads=[f"PT{bi}", "va0", "va1", "va_ones"], writes=[pk])
                rc = rec[:, sl % 4:sl % 4 + 1]
                p.op("dve", lambda e: e.reciprocal(out=rc, in_=po[:, 64:65]), reads=[pk], writes=[f"rec{sl % 4}"])
                p.op("dve", lambda e: e.tensor_scalar_mul(out=y_tm[:, tq, h * 64:(h + 1) * 64], in0=po[:, 0:64], scalar1=rc),
                     reads=[pk, f"rec{sl % 4}"], writes=[f"ytm{tq}"])

            for h in range(8):
                pair, off = h // 2, (h % 2) * 64
                if h % 2 == 0:
                    p.dma("sp", lambda e, pair=pair: e.dma_start(out=qT, in_=QAT.ap()[pair * 128:(pair + 1) * 128, :]),
                          reads=["QAT"], writes=["qT"])
                    p.dma("sp", lambda e, pair=pair: e.dma_start(out=kT, in_=KAT.ap()[pair * 128:(pair + 1) * 128, :]),
                          reads=["KAT"], writes=["kT"])
                p.dma("sp", lambda e, h=h: e.dma_start(out=nabt, in_=na_bias.ap()[l, h, :, :]), writes=["nabt"])
                p.op("act", lambda e: e.activation(out=nabt, in_=nabt, func=AF.Exp), reads=["nabt"], writes=["nabt"])
                p.op("dve", lambda e: e.tensor_tensor(out=emb.rearrange("q c j n -> q (c j n)"), in0=nabt, in1=maskt, op=ALU.mult),
                     reads=["nabt", "maskt"], writes=["emb"])
                for tq in range(32):
                    cls = {0: 0, 1: 1, 30: 3, 31: 4}.get(tq, 2)
                    k0 = min(max(tq - 2, 0), 27)
                    one(h, tq, [k0 + j for j in range(5)] + [32, 33], cls, off)
                if with_ctx_q:
                    for tq in (32, 33):
                        one(h, tq, [32, 33], None, off)
            for t in range(NTILE if with_ctx_q else 32):
                for k in range(4):
                    p.op("pe", lambda e, k=k, t=t: e.transpose(ptr[t % 2][:, k * 128:(k + 1) * 128], y_tm[:, t, k * 128:(k + 1) * 128], ident[:]),
                         reads=[f"ytm{t}", "ident"], writes=[f"ptr{t % 2}"])
                copy_any(nstg, ptr[t % 2][:, 0:512].rearrange("q (k n) -> q k n", k=4), [f"ptr{t % 2}"], ["nstg"])
                p.dma("sp", lambda e, t=t: e.dma_start(out=YNAT.ap()[:, t * 128:(t + 1) * 128].rearrange("(k q) n -> q k n", q=128), in_=nstg),
                      reads=["nstg"], writes=["YNAT"])


        def merge(l, ntiles):
            areset()
            wna = alloc([4, 1024], BF16)
            wfn = alloc([4, 1024], BF16)
            wret = alloc([8, 1024], BF16)
            wout = alloc([8, 1024], BF16)
            lnw = alloc([1024], F32)
            lnb = alloc([1024], F32)
            glt = [alloc([3072], BF16) for _ in range(2)]
            gates = alloc([3072], F32)
            ynT = [alloc([4, 128], BF16) for _ in range(2)]
            yfT = [alloc([4, 128], BF16) for _ in range(2)]
            yrT = [alloc([8, 128], BF16) for _ in range(2)]
            xt = [alloc([1024], F32) for _ in range(2)]
            ysum = alloc([1024], F32)
            ytmp = alloc([1024], F32)
            y16 = alloc([1024], BF16)
            ysT = alloc([8, 128], BF16)
            xo = alloc([1024], F32)
            stats = alloc([2, 6], F32)
            mv = alloc([2], F32)
            rstd = alloc([1], F32)
            p.dma("pool", lambda e: e.dma_start(out=wna, in_=w_o_na.ap()[l].rearrange("(c q) n -> q c n", q=128)), writes=["wna"])
            p.dma("pool", lambda e: e.dma_start(out=wfn, in_=w_fourier.ap()[l].rearrange("(c q) n -> q c n", q=128)), writes=["wfn"])
            p.dma("pool", lambda e: e.dma_start(out=wret, in_=w_o_ret.ap()[l].rearrange("(c q) n -> q c n", q=128)), writes=["wret"])
            p.dma("pool", lambda e: e.dma_start(out=wout, in_=w_out.ap()[l].rearrange("(c q) n -> q c n", q=128)), writes=["wout"])
            p.dma("sp", lambda e: e.dma_start(out=lnw, in_=ln_mix_w.ap()[l, :].partition_broadcast(128)), writes=["lnw"])
            p.dma("sp", lambda e: e.dma_start(out=lnb, in_=ln_mix_b.ap()[l, :].partition_broadcast(128)), writes=["lnb"])

            def tile_fn(t, b, j):
                tok = t * 128
                p.dma("sp", lambda e: e.dma_start(out=glt[b], in_=GL.ap()[tok:tok + 128, :]), reads=["GL"], writes=[f"glt{b}"])
                p.dma("sp", lambda e: e.dma_start(out=ynT[b], in_=YNAT.ap()[:, tok:tok + 128].rearrange("(k q) n -> q k n", q=128)),
                      reads=["YNAT"], writes=[f"ynT{b}"])
                p.dma("sp", lambda e: e.dma_start(out=yfT[b], in_=YFNT.ap()[:, tok:tok + 128].rearrange("(k q) n -> q k n", q=128)),
                      reads=["YFNT"], writes=[f"yfT{b}"])
                p.dma("sp", lambda e: e.dma_start(out=yrT[b], in_=YRETT.ap()[:, tok:tok + 128].rearrange("(k q) n -> q k n", q=128)),
                      reads=["YRETT"], writes=[f"yrT{b}"])
                p.dma("sp", lambda e: e.dma_start(out=xt[b], in_=XRES.ap()[tok:tok + 128, :]), reads=["xres"], writes=[f"mxt{b}"])
                p.op("act", lambda e: e.activation(out=gates, in_=glt[b], func=AF.Sigmoid), reads=[f"glt{b}"], writes=["gates"])
                branches = [(ynT[b], wna, 4, f"ynT{b}", "wna"), (yfT[b], wfn, 4, f"yfT{b}", "wfn"), (yrT[b], wret, 8, f"yrT{b}", "wret")]
                for nb in range(2):
                    cs = slice(nb * 512, (nb + 1) * 512)
                    for bi, (yT, w, nk, yk, wk) in enumerate(branches):
                        bank, bkey = next_bank()
                        for k in range(nk):
                            p.op("pe", lambda e, k=k, yT=yT, w=w, bank=bank, nk=nk, cs=cs: e.matmul(
                                bank, lhsT=yT[:, k, :], rhs=w[:, k, cs], start=(k == 0), stop=(k == nk - 1)),
                                reads=[yk, wk], writes=[bkey])
                        gsl = gates[:, bi * 1024 + nb * 512: bi * 1024 + (nb + 1) * 512]
                        if bi == 0:
                            p.op("dve", lambda e, bank=bank, gsl=gsl, cs=cs: e.tensor_tensor(out=ysum[:, cs], in0=bank, in1=gsl, op=ALU.mult),
                                 reads=[bkey, "gates"], writes=[f"ysum{nb}"])
                        else:
                            p.op("dve", lambda e, bank=bank, gsl=gsl, cs=cs: e.tensor_tensor(out=ytmp[:, cs], in0=bank, in1=gsl, op=ALU.mult),
                                 reads=[bkey, "gates"], writes=[f"ytmp{nb}"])
                            dst = y16 if bi == 2 else ysum
                            p.op("dve", lambda e, dst=dst, cs=cs: e.tensor_tensor(out=dst[:, cs], in0=ysum[:, cs], in1=ytmp[:, cs], op=ALU.add),
                                 reads=[f"ysum{nb}", f"ytmp{nb}"], writes=[f"ysum{nb}", f"y16{nb}"])
                for k in range(8):
                    p.op("pe", lambda e, k=k: e.transpose(ptr[b][:, k * 128:(k + 1) * 128], y16[:, k * 128:(k + 1) * 128], ident[:]),
                         reads=["y160", "y161", "ident"], writes=[f"ptr{b}"])
                copy_any(ysT, ptr[b][:, :].rearrange("q (k n) -> q k n", k=8), [f"ptr{b}"], ["ysT"])
                for nb in range(2):
                    cs = slice(nb * 512, (nb + 1) * 512)
                    bank, bkey = next_bank()
                    for k in range(8):
                        p.op("pe", lambda e, k=k, bank=bank, cs=cs: e.matmul(bank, lhsT=ysT[:, k, :], rhs=wout[:, k, cs], start=(k == 0), stop=(k == 7)),
                             reads=["ysT", "wout"], writes=[bkey])
                    p.op("dve", lambda e, bank=bank, cs=cs, nb=nb: e.tensor_tensor(out=xo[:, cs], in0=bank, in1=mods[:, j, 2 * D + nb * 512:2 * D + (nb + 1) * 512],
                                                                     op=ALU.mult), reads=[bkey, "mods"], writes=["xo"])
                p.op("dve", lambda e: e.scalar_tensor_tensor(out=xo, in0=xt[b], scalar=DN_ALPHA, in1=xo, op0=ALU.mult, op1=ALU.add),
                     reads=[f"mxt{b}", "xo"], writes=["xo"])
                ln_tile(xo, stats, mv, rstd, "xo", "mst")
                p.op("dve", lambda e: e.tensor_tensor(out=xo, in0=xo, in1=lnw, op=ALU.mult), reads=["xo", "lnw"], writes=["xo"])
                p.op("dve", lambda e: e.tensor_tensor(out=xo, in0=xo, in1=lnb, op=ALU.add), reads=["xo", "lnb"], writes=["xo"])
                p.dma("sp", lambda e: e.dma_start(out=XRES.ap()[tok:tok + 128, :], in_=xo), reads=["xo"], writes=["xres_w"])

            for t in range(ntiles):
                tile_fn(t, t % 2, 0 if t < 32 else 1)


        NPAIR = 392
        NBLK = 2 * NPAIR
        NROW = 896
        NSLOT = NROW * 128
        H16 = scratch("H16", [NT + 1, D])
        SHO = scratch("SHO", [NT, D])
        TBL = scratch("TBL", [NSLOT, 1], F32)
        OUTS = scratch("OUTS", [NSLOT, D])
        ident_f = sb("ident_f", [128, 128], F32)
        p.op("pool", lambda e: e.memset(ident_f[:], 1.0), writes=["ident_f"])
        p.op("pool", lambda e: e.affine_select(out=ident_f[:], in_=ident_f[:], pattern=[[-1, 128]],
                                              compare_op=ALU.is_equal, fill=0.0, base=0, channel_multiplier=1),
             reads=["ident_f"], writes=["ident_f"])

        def moe(l, ntiles):
            areset()
            rw = alloc([8, 256], F32)
            rb = alloc([256], F32)
            shgu = alloc([8, 512], BF16)
            shd = alloc([2, 1024], BF16)
            triU = alloc([128], BF16)
            ones16 = alloc([128], BF16)
            onesr = alloc([256], F32)
            cum = alloc([256], F32)
            eidi = alloc([256], I32)
            eidx = alloc([256], F32)
            qci = alloc([1], I32)
            qcf = alloc([1], F32)
            toki = alloc([NTILE], I32)
            tokf = alloc([NTILE], F32)
            W8 = alloc([NTILE, 8], F32)
            E8 = alloc([NTILE, 8], F32)
            R8 = alloc([NTILE, 8], F32)
            D8 = alloc([NTILE, 8], I32)
            BE = alloc([NPAIR], F32)
            WIDX = alloc([NPAIR], I32)
            padded = alloc([256], F32)
            pad_end = alloc([256], F32)
            pad_start = alloc([256], F32)
            xt = [alloc([1024], F32) for _ in range(2)]
            h16 = alloc([1024], BF16)
            hT16 = alloc([8, 128], BF16)
            hT32 = alloc([8, 128], F32)
            hm16 = alloc([256], BF16)
            sgt = alloc([256], F32)
            hmT = alloc([2, 128], BF16)
            sho16 = alloc([1024], BF16)
            sc = alloc([256], F32)
            sel = alloc([256], F32)
            selm = alloc([256], F32)
            m8 = alloc([8, 8], F32)
            gs = alloc([8], F32)
            g8 = alloc([8], F32)
            pen = alloc([8], F32)
            v8 = alloc([8], F32)
            A = alloc([256], F32)
            A16 = alloc([256], BF16)
            rankd = alloc([256], F32)
            junk = alloc([256], F32)
            d8f = alloc([8], F32)
            w8 = alloc([8], F32)
            wsum = alloc([1], F32)
            stats = alloc([2, 6], F32)
            mv = alloc([2], F32)
            rstd = alloc([1], F32)

            p.dma("sp", lambda e: e.dma_start(out=rw, in_=router_w.ap()[l].rearrange("(c q) n -> q c n", q=128)), writes=["rw"])
            p.dma("sp", lambda e: e.dma_start(out=rb, in_=router_bias.ap()[l, :].partition_broadcast(128)), writes=["rb"])
            p.dma("pool", lambda e: e.dma_start(out=shgu[:, :, 0:256], in_=sh_w_gate.ap()[l].rearrange("(c q) n -> q c n", q=128)), writes=["shgu_a"])
            p.dma("pool", lambda e: e.dma_start(out=shgu[:, :, 256:512], in_=sh_w_up.ap()[l].rearrange("(c q) n -> q c n", q=128)), writes=["shgu_b"])
            p.dma("pool", lambda e: e.dma_start(out=shd, in_=sh_w_down.ap()[l].rearrange("(c q) n -> q c n", q=128)), writes=["shd"])
            p.op("pool", lambda e: e.memset(triU, 1.0), writes=["triU"])
            p.op("pool", lambda e: e.affine_select(out=triU, in_=triU, pattern=[[1, 128]], compare_op=ALU.is_gt, fill=0.0,
                                                  base=0, channel_multiplier=-1), reads=["triU"], writes=["triU"])
            p.op("pool", lambda e: e.memset(ones16, 1.0), writes=["ones16"])
            p.op("pool", lambda e: e.memset(onesr, 1.0), writes=["onesr"])
            p.op("pool", lambda e: e.memset(cum, 0.0), writes=["cum"])
            p.op("pool", lambda e: e.iota(eidi, pattern=[[1, 256]], base=0, channel_multiplier=0), writes=["eidi"])
            p.op("dve", lambda e: e.tensor_copy(out=eidx, in_=eidi), reads=["eidi"], writes=["eidx"])
            p.op("pool", lambda e: e.iota(qci, pattern=[[0, 1]], base=0, channel_multiplier=1), writes=["qci"])
            p.op("dve", lambda e: e.tensor_copy(out=qcf, in_=qci), reads=["qci"], writes=["qcf"])
            p.op("pool", lambda e: e.iota(toki, pattern=[[128, NTILE]], base=0, channel_multiplier=1), writes=["toki"])
            p.op("dve", lambda e: e.tensor_copy(out=tokf, in_=toki), reads=["toki"], writes=["tokf"])
            tfill = xt[1][:, 0:NROW]
            zrow = sho16
            p.op("pool", lambda e: e.memset(tfill, float(NT)), writes=["xt1"])
            p.op("pool", lambda e: e.memset(zrow, 0.0), writes=["sho16"])
            p.dma("sp", lambda e: e.dma_start(out=TBL.ap().rearrange("(q f) o -> q (f o)", q=128), in_=tfill), reads=["xt1"], writes=["TBL"])
            p.dma("sp", lambda e: e.dma_start(out=H16.ap()[NT:NT + 1, :], in_=zrow[0:1, :]), reads=["sho16"], writes=["H16"])

            def m1_tile(t, b, j):
                tok = t * 128
                p.dma("sp", lambda e: e.dma_start(out=xt[b], in_=XRES.ap()[tok:tok + 128, :]), reads=["xres", "xres_w"], writes=[f"xt{b}"])
                ln_tile(xt[b], stats, mv, rstd, f"xt{b}", "st")
                p.op("dve", lambda e: e.tensor_tensor(out=xt[b], in0=xt[b], in1=mods[:, j, 4 * D:5 * D], op=ALU.mult),
                     reads=[f"xt{b}", "mods"], writes=[f"xt{b}"])
                p.op("dve", lambda e: e.tensor_tensor(out=xt[b], in0=xt[b], in1=mods[:, j, 3 * D:4 * D], op=ALU.add),
                     reads=[f"xt{b}", "mods"], writes=[f"xt{b}"])
                p.op("act", lambda e: e.copy(out=h16, in_=xt[b]), reads=[f"xt{b}"], writes=["h16"])
                p.dma("sp", lambda e: e.dma_start(out=H16.ap()[tok:tok + 128, :], in_=h16), reads=["h16"], writes=["H16"])
                for k in range(8):
                    p.op("pe", lambda e, k=k: e.transpose(ptr[0][:, k * 128:(k + 1) * 128], h16[:, k * 128:(k + 1) * 128], ident[:]),
                         reads=["h16", "ident"], writes=["ptr0"])
                p.op("act", lambda e: e.copy(out=hT16, in_=ptr[0][:, :].rearrange("q (k n) -> q k n", k=8)), reads=["ptr0"], writes=["hT16"])
                for k in range(8):
                    p.op("pe", lambda e, k=k: e.matmul(pb45[0][:, :], lhsT=hT16[:, k, :], rhs=shgu[:, k, :], start=(k == 0), stop=(k == 7)),
                         reads=["hT16", "shgu_a", "shgu_b"], writes=["pb4"])
                p.op("act", lambda e: e.activation(out=sgt, in_=pb45[0][:, 0:256], func=AF.Silu), reads=["pb4"], writes=["sgt"])
                p.op("dve", lambda e: e.tensor_tensor(out=hm16, in0=sgt, in1=pb45[0][:, 256:512], op=ALU.mult), reads=["sgt", "pb4"], writes=["hm16"])
                for k in range(2):
                    p.op("pe", lambda e, k=k: e.transpose(ptr[1][:, k * 128:(k + 1) * 128], hm16[:, k * 128:(k + 1) * 128], ident[:]),
                         reads=["hm16", "ident"], writes=["ptr1"])
                p.op("act", lambda e: e.copy(out=hmT, in_=ptr[1][:, 0:256].rearrange("q (k n) -> q k n", k=2)), reads=["ptr1"], writes=["hmT"])
                for nb in range(2):
                    bank = pbank[2 + nb]
                    for k in range(2):
                        p.op("pe", lambda e, k=k, nb=nb, bank=bank: e.matmul(bank, lhsT=hmT[:, k, :], rhs=shd[:, k, nb * 512:(nb + 1) * 512],
                                                                             start=(k == 0), stop=(k == 1)), reads=["hmT", "shd"], writes=[f"pb{2 + nb}"])
                    copy_any(sho16[:, nb * 512:(nb + 1) * 512], bank, [f"pb{2 + nb}"], ["sho16"])
                p.dma("sp", lambda e: e.dma_start(out=SHO.ap()[tok:tok + 128, :], in_=sho16), reads=["sho16"], writes=["SHO"])
                for k in range(8):
                    p.op("pe", lambda e, k=k: e.transpose(pbig[0][:, k * 128:(k + 1) * 128], xt[b][:, k * 128:(k + 1) * 128], ident_f[:]),
                         reads=[f"xt{b}", "ident_f"], writes=["pb0", "pb1"])
                p.op("dve", lambda e: e.tensor_copy(out=hT32, in_=pbig[0][:, :].rearrange("q (k n) -> q k n", k=8)), reads=["pb0", "pb1"], writes=["hT32"])
                for k in range(8):
                    p.op("pe", lambda e, k=k: e.matmul(pb45[1][:, 0:256], lhsT=hT32[:, k, :], rhs=rw[:, k, :], start=(k == 0), stop=(k == 7)),
                         reads=["hT32", "rw"], writes=["pb5"])
                p.op("act", lambda e: e.activation(out=sc, in_=pb45[1][:, 0:256], func=AF.Sigmoid), reads=["pb5"], writes=["sc"])
                p.op("dve", lambda e: e.tensor_tensor(out=sel, in0=sc, in1=rb, op=ALU.add), reads=["sc", "rb"], writes=["sel"])
                for g in range(8):
                    p.op("dve", lambda e, g=g: e.max(out=m8[:, g, :], in_=sel[:, g * 32:(g + 1) * 32]), reads=["sel"], writes=["m8"])
                p.op("dve", lambda e: e.tensor_tensor(out=gs, in0=m8[:, :, 0], in1=m8[:, :, 1], op=ALU.add), reads=["m8"], writes=["gs"])
                p.op("dve", lambda e: e.max(out=g8, in_=gs), reads=["gs"], writes=["g8"])
                p.op("dve", lambda e: e.tensor_scalar(out=pen, in0=gs, scalar1=g8[:, 3:4], scalar2=None, op0=ALU.is_ge), reads=["gs", "g8"], writes=["pen"])
                p.op("dve", lambda e: e.tensor_scalar(out=pen, in0=pen, scalar1=1.0, scalar2=1.0e4, op0=ALU.subtract, op1=ALU.mult),
                     reads=["pen"], writes=["pen"])
                p.op("dve", lambda e: e.tensor_tensor(out=selm.rearrange("q (g x) -> q g x", g=8), in0=sel.rearrange("q (g x) -> q g x", g=8),
                                                      in1=pen.unsqueeze(2).broadcast_to([128, 8, 32]), op=ALU.add), reads=["sel", "pen"], writes=["selm"])
                p.op("dve", lambda e: e.max(out=v8, in_=selm), reads=["selm"], writes=["v8"])
                p.op("dve", lambda e: e.tensor_scalar(out=A, in0=selm, scalar1=v8[:, 7:8], scalar2=None, op0=ALU.is_ge), reads=["selm", "v8"], writes=["A"])
                p.op("dve", lambda e: e.tensor_copy(out=A16, in_=A), reads=["A"], writes=["A16"])
                p.op("pe", lambda e: e.matmul(pbank[2][:, 0:256], lhsT=triU, rhs=A16, start=True, stop=True), reads=["triU", "A16"], writes=["pb2"])
                p.op("pe", lambda e: e.matmul(pbank[3][:, 0:256], lhsT=ones16, rhs=A16, start=True, stop=True), reads=["ones16", "A16"], writes=["pb3"])
                p.op("dve", lambda e: e.tensor_tensor(out=rankd, in0=pbank[2][:, 0:256], in1=cum, op=ALU.add), reads=["pb2", "cum"], writes=["rankd"])
                p.op("dve", lambda e: e.tensor_tensor(out=cum, in0=pbank[3][:, 0:256], in1=cum, op=ALU.add), reads=["pb3", "cum"], writes=["cum"])
                for k in range(8):
                    for (src, dstT, key) in ((rankd, R8, "R8"), (sc, w8.unsqueeze(1), "w8"), (eidx, E8, "E8")):
                        oap = dstT[:, t, k:k + 1] if key != "w8" else w8[:, k:k + 1]
                        p.op("dve", lambda e, k=k, src=src, oap=oap: e.scalar_tensor_tensor(
                            out=junk, in0=selm, scalar=v8[:, k:k + 1], in1=src, op0=ALU.is_equal, op1=ALU.mult, accum_out=oap),
                            reads=["selm", "v8", "rankd", "sc", "eidx"], writes=["junk", f"{key}_{t}"])
                p.op("dve", lambda e: e.reduce_sum(out=wsum, in_=w8, axis=AX.X), reads=[f"w8_{t}"], writes=["wsum"])
                p.op("dve", lambda e: e.reciprocal(out=wsum, in_=wsum), reads=["wsum"], writes=["wsum"])
                p.op("dve", lambda e: e.tensor_scalar(out=W8[:, t, :], in0=w8, scalar1=wsum[:, 0:1], scalar2=2.5, op0=ALU.mult, op1=ALU.mult),
                     reads=[f"w8_{t}", "wsum"], writes=[f"W8_{t}"])

            for t in range(ntiles):
                m1_tile(t, t % 2, 0 if t < 32 else 1)

            p.barrier()
            MAGIC_ = 12582912.0
            p.op("dve", lambda e: e.tensor_scalar(out=padded, in0=cum, scalar1=127.25, scalar2=1.0 / 256, op0=ALU.add, op1=ALU.mult),
                 reads=["cum"], writes=["padded"])
            p.op("dve", lambda e: e.tensor_scalar_add(out=padded, in0=padded, scalar1=MAGIC_), reads=["padded"], writes=["padded"])
            p.op("dve", lambda e: e.tensor_scalar(out=padded, in0=padded, scalar1=MAGIC_, scalar2=256.0, op0=ALU.subtract, op1=ALU.mult),
                 reads=["padded"], writes=["padded"])
            p.op("dve", lambda e: e.tensor_tensor_scan(out=pad_end, data0=onesr, data1=padded, initial=0.0, op0=ALU.mult, op1=ALU.add),
                 reads=["padded", "onesr"], writes=["pad_end"])
            p.op("dve", lambda e: e.tensor_tensor(out=pad_start, in0=pad_end, in1=padded, op=ALU.subtract), reads=["pad_end", "padded"], writes=["pad_start"])
            for bq in range(NPAIR):
                p.op("dve", lambda e, bq=bq: e.scalar_tensor_tensor(out=junk, in0=pad_end, scalar=float(256 * bq), in1=onesr, op0=ALU.is_le,
                                                                    op1=ALU.mult, accum_out=BE[:, bq:bq + 1]),
                     reads=["pad_end", "onesr"], writes=["junk", "BE"])
            p.op("dve", lambda e: e.tensor_scalar(out=BE, in0=BE, scalar1=255.0, scalar2=128.0, op0=ALU.min, op1=ALU.mult), reads=["BE"], writes=["BE"])
            p.op("dve", lambda e: e.tensor_scalar_add(out=BE, in0=BE, scalar1=qcf[:, 0:1]), reads=["BE", "qcf"], writes=["BE"])
            p.op("dve", lambda e: e.tensor_copy(out=WIDX, in_=BE), reads=["BE"], writes=["WIDX"])

            def m1b_tile(t):
                for k in range(8):
                    p.op("dve", lambda e, k=k: e.scalar_tensor_tensor(
                        out=junk, in0=eidx, scalar=E8[:, t, k:k + 1], in1=pad_start, op0=ALU.is_equal, op1=ALU.mult, accum_out=d8f[:, k:k + 1]),
                        reads=["eidx", f"E8_{t}", "pad_start"], writes=["junk", "d8f"])
                p.op("dve", lambda e: e.tensor_tensor(out=d8f, in0=d8f, in1=R8[:, t, :], op=ALU.add), reads=["d8f", f"R8_{t}"], writes=["d8f"])
                p.op("dve", lambda e: e.tensor_copy(out=D8[:, t, :], in_=d8f), reads=["d8f"], writes=[f"D8_{t}"])
                for k in range(8):
                    p.dma("pool", lambda e, k=k: e.indirect_dma_start(
                        out=TBL.ap()[:, :], out_offset=bass.IndirectOffsetOnAxis(ap=D8[:, t, k:k + 1], axis=0),
                        in_=tokf[:, t:t + 1], in_offset=None), reads=[f"D8_{t}", "tokf", "TBL"], writes=["TBLs"])

            for t in range(ntiles):
                m1b_tile(t)

            p.barrier()
            tbl_sb = alloc([7, 128], F32)
            idxc = alloc([NROW], I32)
            wg = [alloc([8, 256], BF16) for _ in range(3)]
            wu = [alloc([8, 256], BF16) for _ in range(3)]
            wd = [alloc([2, 1024], BF16) for _ in range(3)]
            xg = [alloc([1024], BF16) for _ in range(3)]
            XT = alloc([8, 128], BF16)
            ostg = [alloc([1024], BF16) for _ in range(2)]
            p.dma("sp", lambda e: e.dma_start(out=tbl_sb, in_=TBL.ap().rearrange("(c r q) o -> r c (q o)", c=7, r=128)),
                  reads=["TBL", "TBLs"], writes=["tbl_sb"])
            for c in range(7):
                p.op("pe", lambda e, c=c: e.transpose(pbig[0][:, c * 128:(c + 1) * 128], tbl_sb[:, c, :], ident_f[:]),
                     reads=["tbl_sb", "ident_f"], writes=["pb0", "pb1"])
            p.op("dve", lambda e: e.tensor_copy(out=idxc, in_=pbig[0][:, 0:NROW]), reads=["pb0", "pb1"], writes=["idxc"])

            def load_block_w(bq):
                b3 = bq % 3
                off = bass.IndirectOffsetOnAxis(ap=WIDX[:, bq:bq + 1], axis=0)
                p.dma("pool", lambda e: e.indirect_dma_start(out=wg[b3].rearrange("q c n -> q (c n)"), out_offset=None,
                                                             in_=exp_w_gate[l].ap(), in_offset=off), reads=["WIDX"], writes=[f"wg{b3}"])
                if cfg.get("skipw") and bq > 2:
                    return
                p.dma("pool", lambda e: e.indirect_dma_start(out=wu[b3].rearrange("q c n -> q (c n)"), out_offset=None,
                                                             in_=exp_w_up[l].ap(), in_offset=off), reads=["WIDX"], writes=[f"wu{b3}"])
                p.dma("pool", lambda e: e.indirect_dma_start(out=wd[b3].rearrange("q c n -> q (c n)"), out_offset=None,
                                                             in_=exp_w_down[l].ap(), in_offset=off), reads=["WIDX"], writes=[f"wd{b3}"])

            XTs = [alloc([8, 128], BF16) for _ in range(3)]
            sgts = [alloc([256], F32) for _ in range(2)]
            hm16s = [alloc([256], BF16) for _ in range(2)]
            hmTs = [alloc([2, 128], BF16) for _ in range(2)]

            def stage1(bq):
                b3 = bq % 3
                p.dma("pool", lambda e: e.indirect_dma_start(
                    out=xg[b3], out_offset=None, in_=H16.ap()[:, :],
                    in_offset=bass.IndirectOffsetOnAxis(ap=idxc[:, bq:bq + 1], axis=0)),
                    reads=["idxc", "H16"], writes=[f"xg{b3}"])
                for k in range(8):
                    p.op("pe", lambda e, k=k: e.transpose(ptr[0][:, k * 128:(k + 1) * 128], xg[b3][:, k * 128:(k + 1) * 128], ident[:]),
                         reads=[f"xg{b3}", "ident"], writes=["ptr0"])
                copy_any(XTs[b3], ptr[0][:, :].rearrange("q (k n) -> q k n", k=8), ["ptr0"], [f"XT{b3}"])

            def stage2(bq):
                b3 = bq % 3
                w3 = (bq // 2) % 3
                b2 = bq % 2
                for k in range(8):
                    p.op("pe", lambda e, k=k: e.matmul(pb45[0][:, 0:256], lhsT=XTs[b3][:, k, :], rhs=wg[w3][:, k, :], start=(k == 0), stop=(k == 7)),
                         reads=[f"XT{b3}", f"wg{w3}"], writes=["pb4"])
                for k in range(8):
                    p.op("pe", lambda e, k=k: e.matmul(pb45[1][:, 0:256], lhsT=XTs[b3][:, k, :], rhs=wu[w3][:, k, :], start=(k == 0), stop=(k == 7)),
                         reads=[f"XT{b3}", f"wu{w3}"], writes=["pb5"])
                p.op("act", lambda e: e.activation(out=sgts[b2], in_=pb45[0][:, 0:256], func=AF.Silu), reads=["pb4"], writes=[f"sgt{b2}"])
                p.op("dve", lambda e: e.tensor_tensor(out=hm16s[b2], in0=sgts[b2], in1=pb45[1][:, 0:256], op=ALU.mult),
                     reads=[f"sgt{b2}", "pb5"], writes=[f"hm16{b2}"])

            def stage3(bq):
                b3 = (bq // 2) % 3
                b2 = bq % 2
                o2 = bq % 2
                for k in range(2):
                    p.op("pe", lambda e, k=k: e.transpose(ptr[1][:, k * 128:(k + 1) * 128], hm16s[b2][:, k * 128:(k + 1) * 128], ident[:]),
                         reads=[f"hm16{b2}", "ident"], writes=["ptr1"])
                p.op("act", lambda e: e.copy(out=hmTs[b2], in_=ptr[1][:, 0:256].rearrange("q (k n) -> q k n", k=2)), reads=["ptr1"], writes=[f"hmT{b2}"])
                for nb in range(2):
                    bank = pbank[2 * o2 + nb]
                    bkey = f"pb{2 * o2 + nb}"
                    for k in range(2):
                        p.op("pe", lambda e, k=k, nb=nb, bank=bank: e.matmul(bank, lhsT=hmTs[b2][:, k, :], rhs=wd[b3][:, k, nb * 512:(nb + 1) * 512],
                                                                             start=(k == 0), stop=(k == 1)), reads=[f"hmT{b2}", f"wd{b3}"], writes=[bkey])
                    copy_any(ostg[o2][:, nb * 512:(nb + 1) * 512], bank, [bkey], [f"ostg{o2}"])
                r0 = bq * 128
                p.dma("sp", lambda e: e.dma_start(out=OUTS.ap()[r0:r0 + 128, :], in_=ostg[o2]), reads=[f"ostg{o2}"], writes=["OUTS"])

            NB_ = cfg.get("nblk", NBLK)
            load_block_w(0)
            load_block_w(1)
            stage1(0)
            stage1(1)
            stage2(0)
            for bq in range(NB_):
                if bq % 2 == 0 and bq // 2 + 2 < (NB_ + 1) // 2:
                    load_block_w(bq // 2 + 2)
                if bq + 2 < NB_:
                    stage1(bq + 2)
                if bq + 1 < NB_:
                    stage2(bq + 1)
                stage3(bq)

            p.barrier()
            og = [alloc([1024], BF16) for _ in range(3)]
            acc = alloc([1024], F32)
            lnw = alloc([1024], F32)
            lnb = alloc([1024], F32)
            sh_in = alloc([1024], BF16)
            xo = alloc([1024], F32)
            p.dma("sp", lambda e: e.dma_start(out=lnw, in_=ln_ffn_w.ap()[l, :].partition_broadcast(128)), writes=["lnw"])
            p.dma("sp", lambda e: e.dma_start(out=lnb, in_=ln_ffn_b.ap()[l, :].partition_broadcast(128)), writes=["lnb"])
            gc = [0]

            def m3_tile(t, b, j):
                tok = t * 128
                p.dma("sp", lambda e: e.dma_start(out=sh_in, in_=SHO.ap()[tok:tok + 128, :]), reads=["SHO"], writes=["sh_in"])
                p.dma("sp", lambda e: e.dma_start(out=xt[b], in_=XRES.ap()[tok:tok + 128, :]), reads=["xres", "xres_w"], writes=[f"xt{b}"])
                p.op("dve", lambda e: e.tensor_copy(out=acc, in_=sh_in), reads=["sh_in"], writes=["acc"])
                for k in range(8):
                    g3 = gc[0] % 3
                    gc[0] += 1
                    p.dma("pool", lambda e, k=k, g3=g3: e.indirect_dma_start(
                        out=og[g3], out_offset=None, in_=OUTS.ap()[:, :],
                        in_offset=bass.IndirectOffsetOnAxis(ap=D8[:, t, k:k + 1], axis=0)),
                        reads=[f"D8_{t}", "OUTS"], writes=[f"og{g3}"])
                    p.op("dve", lambda e, k=k, g3=g3: e.scalar_tensor_tensor(out=acc, in0=og[g3], scalar=W8[:, t, k:k + 1], in1=acc,
                                                                             op0=ALU.mult, op1=ALU.add),
                         reads=[f"og{g3}", f"W8_{t}", "acc"], writes=["acc"])
                p.op("dve", lambda e: e.tensor_tensor(out=xo, in0=acc, in1=mods[:, j, 5 * D:6 * D], op=ALU.mult), reads=["acc", "mods"], writes=["xo"])
                p.op("dve", lambda e: e.scalar_tensor_tensor(out=xo, in0=xt[b], scalar=DN_ALPHA, in1=xo, op0=ALU.mult, op1=ALU.add),
                     reads=[f"xt{b}", "xo"], writes=["xo"])
                ln_tile(xo, stats, mv, rstd, "xo", "st")
                p.op("dve", lambda e: e.tensor_tensor(out=xo, in0=xo, in1=lnw, op=ALU.mult), reads=["xo", "lnw"], writes=["xo"])
                p.op("dve", lambda e: e.tensor_tensor(out=xo, in0=xo, in1=lnb, op=ALU.add), reads=["xo", "lnb"], writes=["xo"])
                p.dma("sp", lambda e: e.dma_start(out=XRES.ap()[tok:tok + 128, :], in_=xo), reads=["xo"], writes=["xres_w2"])

            for t in range(ntiles):
                m3_tile(t, t % 2, 0 if t < 32 else 1)

        p.dma("sp", lambda e: e.dma_start(out=XRES.ap()[0:SEQ, :], in_=x_in.ap()), writes=["xres"])
        p.dma("sp", lambda e: e.dma_start(out=XRES.ap()[SEQ:NT, :], in_=ctx_in.ap()), writes=["xres"])
        for l in range(cfg.get("layers", DEPTH)):
            phase0(l)
            if stages >= 1 and not cfg.get("moe_only"):
                phase1(l, XRES.ap())
            if stages >= 5 and not cfg.get("moe_only"):
                nattn(l, l < DEPTH - 1)
            if stages >= 4 and not cfg.get("noret") and not cfg.get("moe_only"):
                retention(l)
            if stages >= 3 and not cfg.get("nofourier") and not cfg.get("moe_only"):
                fourier(SEQ, 0)
                if l < DEPTH - 1:
                    fourier(CTX, SEQ)
            if stages >= 6 and not cfg.get("moe_only"):
                merge(l, NTILE if l < DEPTH - 1 else 32)
            if stages >= 7:
                moe(l, NTILE if l < DEPTH - 1 else 32)

        p.barrier()
        for i in range(4):
            p.dma("sp", lambda e, i=i: e.dma_start(out=out_t.ap()[i * 1024:(i + 1) * 1024, :],
                                                   in_=XRES.ap()[i * 1024:(i + 1) * 1024, :]),
                  reads=["xres"], writes=["out"])
        p.barrier()
        with nc.Block() as block:
            p.emit(block)
    return nc


_NC_CACHE = {}


def _rope_table():
    t = np.arange(SEQ)
    row, col = t // 64, t % 64
    inv = (10000.0 ** (-np.arange(32, dtype=np.float32) / 32)).astype(np.float32)
    ar = row.astype(np.float32)[:, None] * inv[None, :]
    ac = col.astype(np.float32)[:, None] * inv[None, :]
    C = np.concatenate([np.cos(ar), np.cos(ar), np.cos(ac), np.cos(ac)], axis=1)
    S = np.concatenate([np.sin(ar), np.sin(ar), np.sin(ac), np.sin(ac)], axis=1)
    return np.ascontiguousarray(np.concatenate([C, S], axis=1), dtype=np.float32)


def _na_tables():
    DR = np.zeros((5, 128, 5, 128), np.int64)
    DC = np.zeros((5, 128, 5, 128), np.int64)
    M = np.zeros((5, 128, 5, 128), np.float32)
    pp = np.arange(128)
    for cls, tq in enumerate((0, 1, 2, 30, 31)):
        k0 = min(max(tq - 2, 0), 27)
        for j in range(5):
            kt = k0 + j
            kr = (2 * kt + pp // 64)[:, None]
            kc = (pp % 64)[:, None]
            qr = (2 * tq + pp // 64)[None, :]
            qc = (pp % 64)[None, :]
            r0 = np.clip(qr - 4, 0, 56)
            c0 = np.clip(qc - 8, 0, 48)
            valid = (kr >= r0) & (kr < r0 + 8) & (kc >= c0) & (kc < c0 + 16)
            DR[cls, :, j, :] = np.clip(kr - qr + 7, 0, 14)
            DC[cls, :, j, :] = np.clip(kc - qc, -15, 15) + 15
            M[cls, :, j, :] = valid
    return DR, DC, M


def _na_bias_layout(rpb):
    DR, DC, M = _na_tables()
    g = rpb[:, :, DR, DC]
    g = np.transpose(g, (0, 1, 3, 2, 4, 5))
    L = rpb.shape[0]
    return (np.ascontiguousarray(g.reshape(L, 8, 128, 3200), dtype=np.float32),
            np.ascontiguousarray(np.transpose(M, (1, 0, 2, 3)).reshape(128, 3200), dtype=np.float32))


def _wlay(w, c):
    L, E, K, N = w.shape
    return np.ascontiguousarray(w.reshape(L, E, c, 128, N).transpose(0, 1, 3, 2, 4)).reshape(L, E * 128, c * N)


def _run(inputs, batches, cfg=None):
    f32 = lambda a: np.ascontiguousarray(np.asarray(a), dtype=np.float32)
    g = {k: f32(v) for k, v in inputs.items()}
    key = str(sorted((cfg or {}).items()))
    if key not in _NC_CACHE:
        _NC_CACHE[key] = build_program(dict(cfg or {}))
    nc = _NC_CACHE[key]
    nab, nam = _na_bias_layout(g["na_rpb"])
    shared = {
        "ada_w": g["ada_w"], "ada_b": g["ada_b"], "w_in": g["w_in"],
        "ret_decay": np.ascontiguousarray(np.concatenate([g["ret_decay_fwd"], g["ret_decay_bwd"]], axis=1)),
        "ret_gn_w": g["ret_gn_w"], "rope": _rope_table(), "na_bias": nab, "na_mask": nam,
        "w_o_na": g["w_o_na"], "w_fourier": g["w_fourier"], "w_o_ret": g["w_o_ret"], "w_out": g["w_out"],
        "ln_mix_w": g["ln_mix_w"], "ln_mix_b": g["ln_mix_b"], "router_w": g["router_w"], "router_bias": g["router_bias"],
        "sh_w_gate": g["sh_w_gate"], "sh_w_up": g["sh_w_up"], "sh_w_down": g["sh_w_down"],
        "ln_ffn_w": g["ln_ffn_w"], "ln_ffn_b": g["ln_ffn_b"],
    }
    for nm, c in (("exp_w_gate", 8), ("exp_w_up", 8), ("exp_w_down", 2)):
        wl = _wlay(g[nm], c)
        for i in range(wl.shape[0]):
            shared[f"{nm}{i}"] = wl[i]
    in_maps = []
    for b in batches:
        m = dict(shared)
        m["x"] = g["x"][b]
        m["ctx"] = g["ctx"][b]
        m["cvec"] = np.ascontiguousarray(np.stack([g["c"][b], g["c_ctx"]]))
        in_maps.append(m)
    res = run_bass_kernel_spmd(nc, in_maps, core_ids=list(range(len(batches))))
    return np.stack([np.asarray(res.results[i]["out"], dtype=np.float32) for i in range(len(batches))], axis=0)


def kernel(**inputs):
    return _run(inputs, list(range(8)))
```

```python
import numpy as np
import concourse.bass as bass
import concourse.mybir as mybir
from concourse.bass_utils import run_bass_kernel_spmd

F32 = mybir.dt.float32
BF16 = mybir.dt.bfloat16
I32 = mybir.dt.int32
AF = mybir.ActivationFunctionType
ALU = mybir.AluOpType
AX = mybir.AxisListType

D = 1024
SEQ = 4096
CTX = 256
NT = SEQ + CTX
NTILE = NT // 128
DEPTH = 2
INW = 8192
LN_EPS = 1e-6
DN_ALPHA = (2.0 * DEPTH) ** 0.25


class P:
    def __init__(self, nc):
        self.nc = nc
        self.ops = {k: [] for k in ("pe", "act", "dve", "pool", "sp")}
        self.cnt = {k: 0 for k in self.ops}
        self.sem = {}
        self.waited = {k: {} for k in self.ops}
        self.last_w = {}
        self.readers = {}
        self.dma_sems = {"sp": [], "pool": []}
        self.dma_rr = {"sp": 0, "pool": 0}
        self.dma_val = {}
        self.dma_last = {}
        self.final_tokens = []
        self.pending = {k: [] for k in self.ops}

    def setup_sems(self, stack):
        for k in self.ops:
            self.sem[k] = stack.enter_context(self.nc.semaphore("e_" + k))
        for q in ("sp", "pool"):
            for i in range(12):
                s = stack.enter_context(self.nc.semaphore(f"d_{q}{i}"))
                self.dma_sems[q].append(s)
                self.dma_val[id(s)] = 0
                self.dma_last[id(s)] = None

    def _deps(self, eng, reads, writes):
        toks = []
        for r in reads:
            toks.extend(self._lw(r))
        for w in writes:
            toks.extend(self._lw(w))
            toks.extend(self.readers.get(w, ()))
        need = {}
        for (s, v, own) in toks:
            if own == eng and eng == "pe":
                continue
            key = id(s)
            if self.waited[eng].get(key, 0) >= v:
                continue
            if key not in need or need[key][1] < v:
                need[key] = (s, v)
        for key, (s, v) in need.items():
            self.waited[eng][key] = v
        return list(need.values())

    def _lw(self, key):
        t = self.last_w.get(key)
        if t is None:
            return []
        return t if isinstance(t, list) else [t]

    def _commit(self, tok, reads, writes):
        is_dma = tok[2].startswith("dma_")
        for w in writes:
            prev = self._lw(w)
            if is_dma and prev and all(t[2].startswith("dma_") for t in prev) and not self.readers.get(w):
                self.last_w[w] = prev + [tok]
            else:
                self.last_w[w] = [tok]
            self.readers[w] = []
        for r in reads:
            self.readers.setdefault(r, []).append(tok)

    def barrier(self):
        for eng in self.ops:
            w = []
            for k in self.ops:
                if k != eng and self.cnt[k] > 0 and self.waited[eng].get(id(self.sem[k]), 0) < self.cnt[k]:
                    w.append((self.sem[k], self.cnt[k]))
                    self.waited[eng][id(self.sem[k])] = self.cnt[k]
            for q in ("sp", "pool"):
                for ds in self.dma_sems[q]:
                    v = self.dma_val[id(ds)]
                    if v > 0 and self.waited[eng].get(id(ds), 0) < v:
                        w.append((ds, v))
                        self.waited[eng][id(ds)] = v
            self.pending[eng].extend(w)
        self.last_w = {}
        self.readers = {}

    def op(self, eng, fn, reads=(), writes=()):
        waits = self.pending[eng] + self._deps(eng, reads, writes)
        self.pending[eng] = []
        self.cnt[eng] += 1
        v = self.cnt[eng]
        s = self.sem[eng]
        self.ops[eng].append((waits, fn, s, 1))
        tok = (s, v, eng)
        self._commit(tok, reads, writes)
        return tok

    def dma(self, q, fn, reads=(), writes=(), inc=16):
        waits = self.pending[q] + self._deps(q, reads, writes)
        self.pending[q] = []
        i = self.dma_rr[q]
        self.dma_rr[q] = (i + 1) % len(self.dma_sems[q])
        s = self.dma_sems[q][i]
        prev = self.dma_val[id(s)]
        if prev > 0 and self.waited[q].get(id(s), 0) < prev:
            waits.append((s, prev))
            self.waited[q][id(s)] = prev
        self.dma_val[id(s)] = prev + inc
        v = prev + inc
        self.ops[q].append((waits, fn, s, inc))
        tok = (s, v, "dma_" + q)
        self._commit(tok, reads, writes)
        return tok

    def emit(self, block):
        def run(eng_name):
            def body(e):
                for waits, fn, s, inc in self.ops[eng_name]:
                    for (ws, wv) in waits:
                        e.wait_ge(ws, wv)
                    fn(e).then_inc(s, inc)
                if eng_name == "sp":
                    for q in ("sp", "pool"):
                        for ds in self.dma_sems[q]:
                            v = self.dma_val[id(ds)]
                            if v > 0:
                                e.wait_ge(ds, v)
                    for k in self.ops:
                        if k != "sp" and self.cnt[k] > 0:
                            e.wait_ge(self.sem[k], self.cnt[k])
            return body
        block.tensor(run("pe"))
        block.scalar(run("act"))
        block.vector(run("dve"))
        block.gpsimd(run("pool"))
        block.sync(run("sp"))


def build_program(cfg):
    from contextlib import ExitStack
    nc = bass.Bass("TRN2", target_bir_lowering=False)
    p = P(nc)
    stages = cfg.get("stages", 99)
    dbg = cfg.get("debug", ())

    def din(name, shape, dt=F32):
        return nc.dram_tensor(name, list(shape), dt, kind="ExternalInput")

    x_in = din("x", [SEQ, D])
    ctx_in = din("ctx", [CTX, D])
    cvec = din("cvec", [2, D])
    WD = cfg.get("wdepth", DEPTH)
    ada_w = din("ada_w", [WD, D, 6 * D])
    ada_b = din("ada_b", [WD, 6 * D])
    w_in = din("w_in", [WD, D, INW])
    ret_decay = din("ret_decay", [WD, 8])
    ret_gn_w = din("ret_gn_w", [WD, 1024])
    rope_t = din("rope", [SEQ, 256])
    na_bias = din("na_bias", [WD, 8, 128, 3200])
    na_mask = din("na_mask", [128, 3200])
    w_o_na = din("w_o_na", [WD, 512, D])
    w_fourier = din("w_fourier", [WD, 512, D])
    w_o_ret = din("w_o_ret", [WD, 1024, D])
    w_out = din("w_out", [WD, D, D])
    ln_mix_w = din("ln_mix_w", [WD, D])
    ln_mix_b = din("ln_mix_b", [WD, D])
    router_w = din("router_w", [WD, D, 256])
    router_bias = din("router_bias", [WD, 256])
    exp_w_all = [din(f"exp_w_all{i}", [256 * 128, 6144]) for i in range(WD)]
    sh_w_gate = din("sh_w_gate", [WD, D, 256])
    sh_w_up = din("sh_w_up", [WD, D, 256])
    sh_w_down = din("sh_w_down", [WD, 256, D])
    ln_ffn_w = din("ln_ffn_w", [WD, D])
    ln_ffn_b = din("ln_ffn_b", [WD, D])
    out_t = nc.dram_tensor("out", [SEQ, D], F32, kind="ExternalOutput")

    def scratch(name, shape, dt=BF16):
        kind = "ExternalOutput" if name in dbg else "Internal"
        return nc.dram_tensor(name, list(shape), dt, kind=kind)

    QAT = scratch("QAT", [512, NT])
    KAT = scratch("KAT", [512, NT])
    UBT = scratch("UBT", [512, NT])
    VA = scratch("VA", [NT, 512])
    QR = scratch("QR", [NT, 512])
    KR = scratch("KR", [NT, 512])
    VR = scratch("VR", [NT, 1024])
    GR = scratch("GR", [NT, 1024])
    GL = scratch("GL", [NT, 3072])
    MODS = scratch("MODS", [2, 6 * D], F32)
    XRES = scratch("XRES", [NT, D], F32)

    with ExitStack() as st:
        p.setup_sems(st)

        def sb(name, shape, dt):
            return st.enter_context(nc.sbuf_tensor(name, list(shape), dt))

        def ps(name, shape, dt=F32):
            return st.enter_context(nc.psum_tensor(name, list(shape), dt))


        ident = sb("ident", [128, 128], BF16)
        ones_f = sb("ones_f", [128, 128], F32)
        mods = sb("mods", [128, 2, 6 * D], F32)
        ARENA_N = 79 * 1024
        arena = sb("arena", [128, ARENA_N], BF16)
        pbig = [ps(f"pbig{i}", [128, 1024], F32) for i in range(2)]
        pb45 = [ps(f"pb{i}", [128, 512], F32) for i in (4, 5)]
        pbank = [pbig[0][:, 0:512], pbig[0][:, 512:1024], pbig[1][:, 0:512], pbig[1][:, 512:1024], pb45[0][:, :], pb45[1][:, :]]
        ptr = [ps(f"ptr{i}", [128, 1024], BF16) for i in range(2)]
        aoff = [0]

        def areset():
            p.barrier()
            aoff[0] = 0

        def alloc(shape, dt, parts=128):
            n = 1
            for d_ in shape:
                n *= d_
            n16 = n * (2 if dt in (F32, I32) else 1)
            assert aoff[0] + n16 <= ARENA_N, (aoff[0], n16)
            v = arena[0:parts, aoff[0]:aoff[0] + n16]
            aoff[0] += (n16 + 31) // 32 * 32
            if dt != BF16:
                v = v.bitcast(dt)
            if len(shape) == 2:
                v = v.rearrange("q (a b) -> q a b", a=shape[0])
            elif len(shape) == 3:
                v = v.rearrange("q (a b c) -> q a b c", a=shape[0], b=shape[1])
            return v

        p.op("pool", lambda e: e.memset(ones_f[:], 1.0), writes=["ones_f"])
        p.op("pool", lambda e: e.memset(ident[:], 1.0), writes=["ident"])
        p.op("pool", lambda e: e.affine_select(out=ident[:], in_=ident[:], pattern=[[-1, 128]],
                                              compare_op=ALU.is_equal, fill=0.0, base=0,
                                              channel_multiplier=1),
             reads=["ident"], writes=["ident"])

        ev = [0]

        def copy_any(out_ap, in_ap, reads, writes):
            ev[0] += 1
            if ev[0] % 2 == 0:
                p.op("act", lambda e: e.copy(out=out_ap, in_=in_ap), reads=reads, writes=writes)
            else:
                p.op("dve", lambda e: e.tensor_copy(out=out_ap, in_=in_ap), reads=reads, writes=writes)

        bank_rr = [0]

        def next_bank():
            bank_rr[0] = (bank_rr[0] + 1) % 6
            i = bank_rr[0]
            return pbank[i], f"pb{i}"

        def phase0(l):
            areset()
            csb = alloc([2, 8], F32)
            crep = alloc([2, 8, 128], F32)
            adaw = [alloc([8, 512], F32) for _ in range(2)]
            bia = [alloc([512], F32, parts=1) for _ in range(2)]
            for j in range(2):
                p.dma("sp", lambda e, j=j: e.dma_start(out=csb[:, j, :], in_=cvec.ap()[j, :].rearrange("(c q) -> q c", q=128),
                                                       allow_slow_non_contiguous=True), writes=["csb"])
            p.op("act", lambda e: e.activation(out=csb, in_=csb, func=AF.Silu), reads=["csb"], writes=["csb"])
            for j in range(2):
                for k in range(8):
                    p.op("dve", lambda e, j=j, k=k: e.tensor_scalar_mul(
                        out=crep[:, j, k, :], in0=ones_f[:], scalar1=csb[:, j, k:k + 1]),
                        reads=["csb", "ones_f"], writes=["crep"])
            for nb in range(12):
                wb = adaw[nb % 2]
                bb = bia[nb % 2]
                p.dma("sp", lambda e, nb=nb, wb=wb: e.dma_start(
                    out=wb, in_=ada_w.ap()[l, :, nb * 512:(nb + 1) * 512].rearrange("(c q) n -> q c n", q=128)),
                    writes=[f"adaw{nb % 2}"])
                p.dma("sp", lambda e, nb=nb, bb=bb: e.dma_start(
                    out=bb, in_=ada_b.ap()[l:l + 1, nb * 512:(nb + 1) * 512]), writes=[f"bia{nb % 2}"])
                for j in range(2):
                    bank, bkey = next_bank()
                    for k in range(8):
                        p.op("pe", lambda e, j=j, k=k, wb=wb, bank=bank: e.matmul(
                            bank, lhsT=crep[:, j, k, :], rhs=wb[:, k, :], start=(k == 0), stop=False),
                            reads=["crep", f"adaw{nb % 2}"], writes=[bkey])
                    p.op("pe", lambda e, bb=bb, bank=bank: e.matmul(
                        bank, lhsT=ones_f[0:1, :], rhs=bb, start=False, stop=True),
                        reads=["ones_f", f"bia{nb % 2}"], writes=[bkey])
                    is_scale = (nb // 2) in (1, 4)
                    if is_scale:
                        p.op("dve", lambda e, j=j, nb=nb, bank=bank: e.tensor_scalar_add(
                            out=mods[:, j, nb * 512:(nb + 1) * 512], in0=bank, scalar1=1.0),
                            reads=[bkey], writes=["mods"])
                    else:
                        p.op("dve", lambda e, j=j, nb=nb, bank=bank: e.tensor_copy(
                            out=mods[:, j, nb * 512:(nb + 1) * 512], in_=bank),
                            reads=[bkey], writes=["mods"])
            if "MODS" in dbg:
                p.dma("sp", lambda e: e.dma_start(out=MODS.ap(), in_=mods[0:1, :, :]), reads=["mods"], writes=["MODS"])

        def ln_tile(xt_ap, stats_ap, mv_ap, rstd_ap, kx, ks):
            for c in range(2):
                p.op("dve", lambda e, c=c: e.bn_stats(out=stats_ap[:, c, :], in_=xt_ap[:, c * 512:(c + 1) * 512]),
                     reads=[kx], writes=[ks])
            p.op("dve", lambda e: e.bn_aggr(out=mv_ap, in_=stats_ap), reads=[ks], writes=[ks + "mv"])
            p.op("dve", lambda e: e.tensor_scalar_add(out=rstd_ap, in0=mv_ap[:, 1:2], scalar1=LN_EPS),
                 reads=[ks + "mv"], writes=[ks + "r"])
            p.op("act", lambda e: e.sqrt(out=rstd_ap, in_=rstd_ap), reads=[ks + "r"], writes=[ks + "r"])
            p.op("dve", lambda e: e.reciprocal(out=rstd_ap, in_=rstd_ap), reads=[ks + "r"], writes=[ks + "r"])
            p.op("dve", lambda e: e.tensor_scalar(out=xt_ap, in0=xt_ap, scalar1=mv_ap[:, 0:1],
                                                  scalar2=rstd_ap[:, 0:1], op0=ALU.subtract, op1=ALU.mult),
                 reads=[kx, ks + "mv", ks + "r"], writes=[kx])

        def phase1(l, xres):
            areset()
            hT = alloc([8, NT], BF16)
            wbuf = [alloc([8, 1024], BF16) for _ in range(2)]
            xt = [alloc([1024], F32) for _ in range(2)]
            xn = [alloc([1024], BF16) for _ in range(2)]
            stats = [alloc([2, 6], F32) for _ in range(2)]
            mv = [alloc([2], F32) for _ in range(2)]
            rstd = [alloc([1], F32) for _ in range(2)]
            stg = [alloc([1024], BF16) for _ in range(4)]

            def load_w(g):
                wb = wbuf[g % 2]
                p.dma("pool", lambda e: e.dma_start(
                    out=wb, in_=w_in.ap()[l, :, g * 1024:(g + 1) * 1024].rearrange("(c q) n -> q c n", q=128)),
                    writes=[f"wbuf{g % 2}"])

            if stages >= 2:
                load_w(0)
            for t in range(NTILE):
                b = t % 2
                j = 0 if t < 32 else 1
                src = xres[t * 128:(t + 1) * 128, :]
                p.dma("sp", lambda e, b=b, src=src: e.dma_start(out=xt[b], in_=src), reads=["xres"], writes=[f"xt{b}"])
                if not cfg.get("noln"):
                    ln_tile(xt[b], stats[b], mv[b], rstd[b], f"xt{b}", f"st{b}")
                p.op("dve", lambda e, b=b, j=j: e.tensor_tensor(out=xt[b], in0=xt[b], in1=mods[:, j, D:2 * D], op=ALU.mult),
                     reads=[f"xt{b}", "mods"], writes=[f"xt{b}"])
                p.op("dve", lambda e, b=b, j=j: e.tensor_tensor(out=xn[b], in0=xt[b], in1=mods[:, j, 0:D], op=ALU.add),
                     reads=[f"xt{b}", "mods"], writes=[f"xn{b}"])
                bv = ptr[b]
                if cfg.get("notr"):
                    continue
                for k in range(8):
                    p.op("pe", lambda e, k=k, b=b, bv=bv: e.transpose(
                        bv[:, k * 128:(k + 1) * 128], xn[b][:, k * 128:(k + 1) * 128], ident[:]),
                        reads=[f"xn{b}", "ident"], writes=[f"ptr{b}"])
                if cfg.get("nocp"):
                    continue
                copy_any(hT[:, :, t * 128:(t + 1) * 128], bv[:, :].rearrange("q (k n) -> q k n", k=8),
                         [f"ptr{b}"], [f"hT{t}a", f"hT{t}b"])

            stg_rr = [0]

            def next_stg():
                stg_rr[0] = (stg_rr[0] + 1) % 4
                return stg[stg_rr[0]], f"stg{stg_rr[0]}"

            def gemm_tm(g, col0, ncols, dst, dst_col0):
                wb = wbuf[g % 2]
                for t in range(NTILE):
                    sg, sk = next_stg()
                    for cb in range(ncols // 512):
                        bank, bkey = next_bank()
                        for k in range(8):
                            p.op("pe", lambda e, k=k, cb=cb, bank=bank, t=t: e.matmul(
                                bank, lhsT=hT[:, k, t * 128:(t + 1) * 128],
                                rhs=wb[:, k, col0 + cb * 512:col0 + (cb + 1) * 512], start=(k == 0), stop=(k == 7)),
                                reads=[f"hT{t}a", f"hT{t}b", f"wbuf{g % 2}"], writes=[bkey])
                        copy_any(sg[:, cb * 512:(cb + 1) * 512], bank, [bkey], [sk])
                    p.dma("sp", lambda e, sg=sg, t=t: e.dma_start(
                        out=dst.ap()[t * 128:(t + 1) * 128, dst_col0:dst_col0 + ncols], in_=sg[:, 0:ncols]),
                        reads=[sk], writes=[dst.name])

            def gemm_fm(g, col0, ncols, dst):
                wb = wbuf[g % 2]
                for fb in range(ncols // 128):
                    for tg in range(0, NT, 1024):
                        ntok = min(1024, NT - tg)
                        sg, sk = next_stg()
                        for tb in range(0, ntok, 512):
                            nn = min(512, ntok - tb)
                            bank, bkey = next_bank()
                            rk = []
                            for tt in range((tg + tb) // 128, (tg + tb + nn) // 128):
                                rk += [f"hT{tt}a", f"hT{tt}b"]
                            for k in range(8):
                                p.op("pe", lambda e, k=k, bank=bank, tb=tb, nn=nn, tg=tg, fb=fb: e.matmul(
                                    bank[:, 0:nn], lhsT=wb[:, k, col0 + fb * 128:col0 + (fb + 1) * 128],
                                    rhs=hT[:, k, tg + tb:tg + tb + nn], start=(k == 0), stop=(k == 7)),
                                    reads=rk + [f"wbuf{g % 2}"], writes=[bkey])
                            copy_any(sg[:, tb:tb + nn], bank[:, 0:nn], [bkey], [sk])
                        p.dma("sp", lambda e, sg=sg, tg=tg, ntok=ntok, fb=fb: e.dma_start(
                            out=dst.ap()[fb * 128:(fb + 1) * 128, tg:tg + ntok], in_=sg[:, 0:ntok]),
                            reads=[sk], writes=[dst.name])

            if stages < 2:
                return
            plan = [
                [("fm", 0, 512, QAT, 0), ("fm", 512, 512, KAT, 0)],
                [("tm", 0, 512, VA, 0), ("fm", 512, 512, UBT, 0)],
                [("tm", 0, 512, QR, 0), ("tm", 512, 512, KR, 0)],
                [("tm", 0, 1024, VR, 0)],
                [("tm", 0, 1024, GR, 0)],
                [("tm", 0, 1024, GL, 0)],
                [("tm", 0, 1024, GL, 1024)],
                [("tm", 0, 1024, GL, 2048)],
            ]
            for g in range(8):
                if g + 1 < min(8, cfg.get("ngroups", 8)):
                    load_w(g + 1)
                if g >= cfg.get("ngroups", 8):
                    break
                for (mode, c0, ncol, dst, dc0) in plan[g]:
                    if mode == "tm":
                        gemm_tm(g, c0, ncol, dst, dc0)
                    else:
                        gemm_fm(g, c0, ncol, dst)


        YFNT = scratch("YFNT", [512, NT])
        MAGIC = 12582912.0

        def gen_cs(dst_c, dst_s, cols, nval, nvs, nmod, tmps, tk, dkeys):
            y, r, t_ = tmps
            p.op("dve", lambda e: e.tensor_scalar(out=y, in0=cols, scalar1=nvs, scalar2=MAGIC, op0=ALU.mult, op1=ALU.add),
                 reads=["fconst"], writes=[tk + "y"])
            p.op("dve", lambda e: e.tensor_scalar(out=r, in0=y, scalar1=MAGIC, scalar2=float(nmod), op0=ALU.subtract, op1=ALU.mult),
                 reads=[tk + "y"], writes=[tk + "r"])
            p.op("dve", lambda e: e.scalar_tensor_tensor(out=t_, in0=cols, scalar=nval, in1=r, op0=ALU.mult, op1=ALU.subtract),
                 reads=[tk + "r", "fconst"], writes=[tk + "t"])
            p.op("dve", lambda e: e.scalar_tensor_tensor(out=y, in0=t_, scalar=-1.0, in1=t_, op0=ALU.mult, op1=ALU.max),
                 reads=[tk + "t"], writes=[tk + "y"])
            p.op("act", lambda e: e.activation(out=dst_s, in_=t_, func=AF.Sin, scale=float(2 * np.pi / nmod)),
                 reads=[tk + "t"], writes=[dkeys[1]])
            p.op("act", lambda e: e.activation(out=dst_c, in_=y, func=AF.Sin, scale=float(-2 * np.pi / nmod), bias=float(np.pi / 2)),
                 reads=[tk + "y"], writes=[dkeys[0]])

        def fourier(N, tok0):
            areset()
            nch = N // 128
            W = min(512, N)
            ubt = alloc([4, N], BF16)
            ucs = alloc([nch, 4, 2 * 128], BF16)
            cs128 = alloc([256], BF16)
            colf = alloc([N], F32)
            nval = alloc([nch], F32)
            nvs = alloc([nch], F32)
            nvs128 = alloc([1], F32)
            tmps = [[alloc([512], F32) for _ in range(3)] for _ in range(2)]
            cblk = [alloc([512], BF16) for _ in range(2)]
            sblk = [alloc([512], BF16) for _ in range(2)]
            fstg = [alloc([512], BF16) for _ in range(4)]
            coli = alloc([N], I32)
            nvi = alloc([nch], I32)
            p.op("pool", lambda e: e.iota(coli, pattern=[[1, N]], base=0, channel_multiplier=0), writes=["coli"])
            p.op("pool", lambda e: e.iota(nvi, pattern=[[128, nch]], base=0, channel_multiplier=1), writes=["nvi"])
            p.op("dve", lambda e: e.tensor_copy(out=colf, in_=coli), reads=["coli"], writes=["fconst"])
            p.op("dve", lambda e: e.tensor_copy(out=nval, in_=nvi), reads=["nvi"], writes=["fconst"])
            p.op("dve", lambda e: e.tensor_scalar_mul(out=nvs, in0=nval, scalar1=1.0 / N), reads=["fconst"], writes=["fconst"])
            p.op("dve", lambda e: e.tensor_scalar_mul(out=nvs128, in0=nval[:, 0:1], scalar1=1.0 / 128), reads=["fconst"], writes=["fconst"])
            for g in range(4):
                p.dma("sp", lambda e, g=g: e.dma_start(out=ubt[:, g, :], in_=UBT.ap()[g * 128:(g + 1) * 128, tok0:tok0 + N]),
                      reads=["UBT"], writes=[f"ubt{g}"])
            gen_cs(cs128[:, 0:128], cs128[:, 128:256], colf[:, 0:128], nval[:, 0:1], nvs128[:, 0:1], 128,
                   [tm[:, 0:128] for tm in tmps[0]], "gt0", ["cs128", "cs128"])
            for i in range(nch):
                for gp in range(2):
                    bank, bkey = next_bank()
                    for gg in range(2):
                        g = gp * 2 + gg
                        p.op("pe", lambda e, g=g, gg=gg, i=i, bank=bank: e.matmul(
                            bank[:, gg * 256:(gg + 1) * 256], lhsT=ubt[:, g, i * 128:(i + 1) * 128], rhs=cs128,
                            start=True, stop=True), reads=[f"ubt{g}", "cs128"], writes=[bkey])
                    bv = bank.rearrange("q (g s c) -> q g s c", g=2, s=2)
                    ov = ucs[:, i, gp * 2:gp * 2 + 2, :].rearrange("q g (s c) -> q g s c", s=2)
                    p.op("dve", lambda e, bv=bv, ov=ov: e.tensor_copy(out=ov[:, :, 0, :], in_=bv[:, :, 0, :]),
                         reads=[bkey], writes=[f"ucs{i}c{gp}"])
                    p.op("dve", lambda e, bv=bv, ov=ov: e.tensor_scalar_mul(out=ov[:, :, 1, :], in0=bv[:, :, 1, :], scalar1=-1.0),
                         reads=[bkey], writes=[f"ucs{i}s{gp}"])
            scale = float(1.0 / np.sqrt(N * 128.0))
            blk = 0
            for mg in range(N // W):
                for i in range(nch):
                    b = blk % 2
                    blk += 1
                    gen_cs(cblk[b][:, 0:W], sblk[b][:, 0:W], colf[:, mg * W:(mg + 1) * W], nval[:, i:i + 1], nvs[:, i:i + 1], N,
                           [tm[:, 0:W] for tm in tmps[b]], f"gt{b}", [f"cblk{b}", f"sblk{b}"])
                    for g in range(4):
                        rk = [f"ucs{i}c{g // 2}", f"ucs{i}s{g // 2}", f"cblk{b}", f"sblk{b}"]
                        p.op("pe", lambda e, g=g, i=i, b=b: e.matmul(
                            pbank[g][:, 0:W], lhsT=ucs[:, i, g, 0:128], rhs=cblk[b][:, 0:W], start=(i == 0), stop=False),
                            reads=rk, writes=[f"pb{g}"])
                        p.op("pe", lambda e, g=g, i=i, b=b: e.matmul(
                            pbank[g][:, 0:W], lhsT=ucs[:, i, g, 128:256], rhs=sblk[b][:, 0:W], start=False, stop=(i == nch - 1)),
                            reads=rk, writes=[f"pb{g}"])
                for g in range(4):
                    if g % 2 == 0:
                        p.op("act", lambda e, g=g: e.mul(out=fstg[g][:, 0:W], in_=pbank[g][:, 0:W], mul=scale),
                             reads=[f"pb{g}"], writes=[f"fstg{g}"])
                    else:
                        p.op("dve", lambda e, g=g: e.tensor_scalar_mul(out=fstg[g][:, 0:W], in0=pbank[g][:, 0:W], scalar1=scale),
                             reads=[f"pb{g}"], writes=[f"fstg{g}"])
                    p.dma("sp", lambda e, g=g, mg=mg: e.dma_start(
                        out=YFNT.ap()[g * 128:(g + 1) * 128, tok0 + mg * W:tok0 + (mg + 1) * W], in_=fstg[g][:, 0:W]),
                        reads=[f"fstg{g}"], writes=["YFNT"])


        YRETT = scratch("YRETT", [1024, NT])
        QS = 128.0 ** -0.5
        LNQS = float(np.log(QS))
        GN_EPS = 1e-5

        def retention(l):
            areset()
            dec = alloc([8], F32)
            lg = alloc([8], F32)
            gC = alloc([8], F32)
            diffi = alloc([128], I32)
            diff = alloc([128], F32)
            rp = alloc([128], F32)
            rn = alloc([128], F32)
            ef = alloc([128], F32)
            eb = alloc([128], F32)
            pci = alloc([2], I32)
            pc = alloc([2], F32)
            cri = alloc([2, 128], I32)
            cr = alloc([2, 128], F32)
            dmask = alloc([4, 128], F32)
            qdf = alloc([4, 128], F32)
            qdb = alloc([4, 128], F32)
            kdf = alloc([4], F32)
            kdb = alloc([4], F32)
            gnw = alloc([1024], F32)
            Sf = alloc([4, 256], F32)
            Sb = alloc([4, 256], F32)
            Sf16 = alloc([4, 256], BF16)
            Sbprev = alloc([32, 1024], BF16)
            qt = [alloc([512], BF16) for _ in range(2)]
            kt = [alloc([512], BF16) for _ in range(2)]
            vt = [alloc([1024], BF16) for _ in range(2)]
            gt = [alloc([1024], BF16) for _ in range(2)]
            rt = [alloc([256], F32) for _ in range(2)]
            t1 = alloc([512], F32)
            t2 = alloc([512], F32)
            q16 = alloc([512], BF16)
            k16 = alloc([512], BF16)
            ks16 = alloc([512], BF16)
            qkT = alloc([8, 128], BF16)
            PT = alloc([4, 128], BF16)
            qfT = alloc([4, 128], BF16)
            qbT = alloc([4, 128], BF16)
            rstat = alloc([4, 6], F32)
            rmv = alloc([4, 2], F32)
            rr = alloc([4], F32)
            yn = alloc([1024], F32)
            sg = alloc([1024], F32)
            y16 = alloc([1024], BF16)
            yT = alloc([8, 128], BF16)

            p.dma("sp", lambda e: e.dma_start(out=dec, in_=ret_decay.ap()[l, :].partition_broadcast(128)), writes=["dec"])
            p.dma("sp", lambda e: e.dma_start(out=gnw, in_=ret_gn_w.ap()[l, :].partition_broadcast(128)), writes=["gnw"])
            p.op("act", lambda e: e.activation(out=lg, in_=dec, func=AF.Exp, scale=-1.0), reads=["dec"], writes=["lg"])
            p.op("act", lambda e: e.activation(out=lg, in_=lg, func=AF.Ln, bias=1.0), reads=["lg"], writes=["lg"])
            p.op("dve", lambda e: e.tensor_scalar_mul(out=lg, in0=lg, scalar1=-1.0), reads=["lg"], writes=["lg"])
            p.op("act", lambda e: e.activation(out=gC, in_=lg, func=AF.Exp, scale=128.0), reads=["lg"], writes=["gC"])
            p.op("pool", lambda e: e.iota(diffi, pattern=[[1, 128]], base=0, channel_multiplier=-1), writes=["diffi"])
            p.op("pool", lambda e: e.iota(pci[:, 0:1], pattern=[[0, 1]], base=127, channel_multiplier=-1), writes=["pci"])
            p.op("pool", lambda e: e.iota(pci[:, 1:2], pattern=[[0, 1]], base=0, channel_multiplier=1), reads=["pci"], writes=["pci"])
            p.op("pool", lambda e: e.iota(cri[:, 0, :], pattern=[[1, 128]], base=1, channel_multiplier=0), writes=["cri"])
            p.op("pool", lambda e: e.iota(cri[:, 1, :], pattern=[[-1, 128]], base=128, channel_multiplier=0), reads=["cri"], writes=["cri"])
            p.op("dve", lambda e: e.tensor_copy(out=diff, in_=diffi), reads=["diffi"], writes=["diff"])
            p.op("dve", lambda e: e.tensor_copy(out=pc, in_=pci), reads=["pci"], writes=["pc"])
            p.op("dve", lambda e: e.tensor_copy(out=cr, in_=cri), reads=["cri"], writes=["cr"])
            p.op("dve", lambda e: e.tensor_scalar_max(out=rp, in0=diff, scalar1=0.0), reads=["diff"], writes=["rp"])
            p.op("dve", lambda e: e.tensor_tensor(out=rn, in0=rp, in1=diff, op=ALU.subtract), reads=["rp", "diff"], writes=["rn"])
            for h in range(4):
                p.op("act", lambda e, h=h: e.activation(out=kdf[:, h:h + 1], in_=pc[:, 0:1], func=AF.Exp, scale=lg[:, h:h + 1]),
                     reads=["pc", "lg"], writes=["kdf"])
                p.op("act", lambda e, h=h: e.activation(out=kdb[:, h:h + 1], in_=pc[:, 1:2], func=AF.Exp, scale=lg[:, 4 + h:5 + h]),
                     reads=["pc", "lg"], writes=["kdb"])
                p.op("act", lambda e, h=h: e.activation(out=qdf[:, h, :], in_=cr[:, 0, :], func=AF.Exp, scale=lg[:, h:h + 1], bias=LNQS),
                     reads=["cr", "lg"], writes=["qdf"])
                p.op("act", lambda e, h=h: e.activation(out=qdb[:, h, :], in_=cr[:, 1, :], func=AF.Exp, scale=lg[:, 4 + h:5 + h], bias=LNQS),
                     reads=["cr", "lg"], writes=["qdb"])
                p.op("act", lambda e, h=h: e.activation(out=ef, in_=rp, func=AF.Exp, scale=lg[:, h:h + 1], bias=LNQS),
                     reads=["rp", "lg"], writes=["ef"])
                p.op("act", lambda e, h=h: e.activation(out=eb, in_=rn, func=AF.Exp, scale=lg[:, 4 + h:5 + h], bias=LNQS),
                     reads=["rn", "lg"], writes=["eb"])
                p.op("pool", lambda e: e.affine_select(out=ef, in_=ef, pattern=[[1, 128]], compare_op=ALU.is_ge, fill=0.0,
                                                      base=0, channel_multiplier=-1), reads=["ef"], writes=["ef"])
                p.op("pool", lambda e: e.affine_select(out=eb, in_=eb, pattern=[[-1, 128]], compare_op=ALU.is_gt, fill=0.0,
                                                      base=0, channel_multiplier=1), reads=["eb"], writes=["eb"])
                p.op("dve", lambda e, h=h: e.tensor_tensor(out=dmask[:, h, :], in0=ef, in1=eb, op=ALU.add),
                     reads=["ef", "eb"], writes=["dmask"])
            p.op("pool", lambda e: e.memset(Sf, 0.0), writes=["Sf"])
            p.op("pool", lambda e: e.memset(Sb, 0.0), writes=["Sb"])
            p.op("pool", lambda e: e.memset(Sf16, 0.0), writes=["Sf16"])

            pbS = pbank[0]
            pbO = [pbank[1], pbank[2]]
            pbK = [pbank[3], pbank[4]]

            def rope(src, dst, rtile, dk):
                sv = src.rearrange("q (h r u d) -> q (h r) u d", h=4, r=2, u=2)
                Cb = rtile[:, 0:128].unsqueeze(1).broadcast_to([128, 4, 128])
                Sv = rtile[:, 128:256].rearrange("q (r u d) -> q r u d", r=2, u=2)
                t1v = t1.rearrange("q (h x) -> q h x", h=4)
                t2v = t2.rearrange("q (h r u d) -> q h r u d", h=4, r=2, u=2)
                s5 = src.rearrange("q (h r u d) -> q h r u d", h=4, r=2, u=2)
                p.op("dve", lambda e: e.tensor_tensor(out=t1v, in0=src.rearrange("q (h x) -> q h x", h=4), in1=Cb, op=ALU.mult),
                     reads=[dk + "src", dk + "rt"], writes=["t1"])
                for u in range(2):
                    for r in range(2):
                        p.op("dve", lambda e, u=u, r=r: e.tensor_tensor(
                            out=t2v[:, :, r, u, :], in0=s5[:, :, r, 1 - u, :],
                            in1=Sv[:, r, u, :].unsqueeze(1).broadcast_to([128, 4, 32]), op=ALU.mult),
                            reads=[dk + "src", dk + "rt"], writes=["t2"])
                t1w = t1.rearrange("q (h r u d) -> q (h r) u d", h=4, r=2, u=2)
                t2w = t2.rearrange("q (h r u d) -> q (h r) u d", h=4, r=2, u=2)
                dw = dst.rearrange("q (h r u d) -> q (h r) u d", h=4, r=2, u=2)
                p.op("dve", lambda e: e.tensor_tensor(out=dw[:, :, 0, :], in0=t1w[:, :, 0, :], in1=t2w[:, :, 0, :], op=ALU.subtract),
                     reads=["t1", "t2"], writes=[dk])
                p.op("dve", lambda e: e.tensor_tensor(out=dw[:, :, 1, :], in0=t1w[:, :, 1, :], in1=t2w[:, :, 1, :], op=ALU.add),
                     reads=["t1", "t2"], writes=[dk])

            def load_k_v(n, tok, b, use_rope, need_q):
                p.dma("sp", lambda e: e.dma_start(out=kt[b], in_=KR.ap()[tok:tok + 128, :]), reads=["KR"], writes=[f"kt{b}"])
                p.dma("sp", lambda e: e.dma_start(out=vt[b], in_=VR.ap()[tok:tok + 128, :]), reads=["VR"], writes=[f"vt{b}"])
                if use_rope:
                    p.dma("sp", lambda e: e.dma_start(out=rt[b], in_=rope_t.ap()[tok:tok + 128, :]), writes=[f"rt{b}"])
                if need_q:
                    p.dma("sp", lambda e: e.dma_start(out=qt[b], in_=QR.ap()[tok:tok + 128, :]), reads=["QR"], writes=[f"qt{b}"])
                    p.dma("sp", lambda e: e.dma_start(out=gt[b], in_=GR.ap()[tok:tok + 128, :]), reads=["GR"], writes=[f"gt{b}"])

            def prep_k(b, use_rope):
                if use_rope:
                    p.last_w["k16src"] = p.last_w.get(f"kt{b}")
                    p.last_w["k16rt"] = p.last_w.get(f"rt{b}")
                    rope(kt[b], k16, rt[b], "k16")
                    p.readers.setdefault(f"kt{b}", []).extend(p._lw("k16"))
                    p.readers.setdefault(f"rt{b}", []).extend(p._lw("k16"))
                else:
                    p.op("dve", lambda e: e.tensor_copy(out=k16, in_=kt[b]), reads=[f"kt{b}"], writes=["k16"])

            def prep_q(b, use_rope):
                if use_rope:
                    p.last_w["q16src"] = p.last_w.get(f"qt{b}")
                    p.last_w["q16rt"] = p.last_w.get(f"rt{b}")
                    rope(qt[b], q16, rt[b], "q16")
                    p.readers.setdefault(f"qt{b}", []).extend(p._lw("q16"))
                    p.readers.setdefault(f"rt{b}", []).extend(p._lw("q16"))
                else:
                    p.op("dve", lambda e: e.tensor_copy(out=q16, in_=qt[b]), reads=[f"qt{b}"], writes=["q16"])

            def kv_update(b, kd, S, gcol, skey):
                for h in range(4):
                    p.op("dve", lambda e, h=h: e.tensor_scalar_mul(out=ks16[:, h * 128:(h + 1) * 128], in0=k16[:, h * 128:(h + 1) * 128],
                                                                   scalar1=kd[:, h:h + 1]), reads=["k16", "kdf", "kdb"], writes=["ks16"])
                for h in range(4):
                    p.op("pe", lambda e, h=h: e.matmul(pbK[h // 2][:, (h % 2) * 256:(h % 2) * 256 + 256], lhsT=ks16[:, h * 128:(h + 1) * 128],
                                                       rhs=vt[b][:, h * 256:(h + 1) * 256], start=True, stop=True),
                         reads=["ks16", f"vt{b}"], writes=[f"pb{3 + h // 2}"])
                for h in range(4):
                    p.op("dve", lambda e, h=h: e.scalar_tensor_tensor(
                        out=S[:, h, :], in0=S[:, h, :], scalar=gC[:, gcol + h:gcol + h + 1],
                        in1=pbK[h // 2][:, (h % 2) * 256:(h % 2) * 256 + 256], op0=ALU.mult, op1=ALU.add),
                        reads=[skey, "gC", f"pb{3 + h // 2}"], writes=[skey])


            def pass1_chunk(n, b, tok, use_rope):
                if True:
                    load_k_v(n, tok, b, use_rope, False)
                    prep_k(b, use_rope)
                    p.op("act", lambda e, n=n: e.copy(out=Sbprev[:, n, :], in_=Sb.rearrange("q h d -> q (h d)")),
                         reads=["Sb"], writes=[f"Sbprev{n}"])
                    kv_update(b, kdb, Sb, 4, "Sb")
            def pass2_chunk(n, b, tok, use_rope):
                if True:
                    load_k_v(n, tok, b, use_rope, True)
                    prep_k(b, use_rope)
                    prep_q(b, use_rope)
                    for h in range(4):
                        p.op("pe", lambda e, h=h: e.transpose(ptr[0][:, h * 128:(h + 1) * 128], q16[:, h * 128:(h + 1) * 128], ident[:]),
                             reads=["q16", "ident"], writes=["ptr0"])
                        p.op("pe", lambda e, h=h: e.transpose(ptr[0][:, (4 + h) * 128:(5 + h) * 128], k16[:, h * 128:(h + 1) * 128], ident[:]),
                             reads=["k16", "ident"], writes=["ptr0"])
                    p.op("act", lambda e: e.copy(out=qkT, in_=ptr[0][:, :].rearrange("q (k n) -> q k n", k=8)),
                         reads=["ptr0"], writes=["qkT"])
                    for h in range(4):
                        p.op("pe", lambda e, h=h: e.matmul(pbS[:, h * 128:(h + 1) * 128], lhsT=qkT[:, 4 + h, :], rhs=qkT[:, h, :],
                                                           start=True, stop=True), reads=["qkT"], writes=["pb0"])
                    p.op("dve", lambda e: e.tensor_tensor(out=PT, in0=pbS[:, :].rearrange("q (h c) -> q h c", h=4), in1=dmask, op=ALU.mult),
                         reads=["pb0", "dmask"], writes=["PT"])
                    p.op("dve", lambda e: e.tensor_tensor(out=qfT, in0=qkT[:, 0:4, :], in1=qdf, op=ALU.mult),
                         reads=["qkT", "qdf"], writes=["qfT"])
                    p.op("dve", lambda e: e.tensor_tensor(out=qbT, in0=qkT[:, 0:4, :], in1=qdb, op=ALU.mult),
                         reads=["qkT", "qdb"], writes=["qbT"])
                    for h in range(4):
                        ob = pbO[h // 2][:, (h % 2) * 256:(h % 2) * 256 + 256]
                        ok = f"pb{1 + h // 2}"
                        p.op("pe", lambda e, h=h, ob=ob: e.matmul(ob, lhsT=PT[:, h, :], rhs=vt[b][:, h * 256:(h + 1) * 256], start=True, stop=False),
                             reads=["PT", f"vt{b}"], writes=[ok])
                        p.op("pe", lambda e, h=h, ob=ob: e.matmul(ob, lhsT=qfT[:, h, :], rhs=Sf16[:, h, :], start=False, stop=False),
                             reads=["qfT", "Sf16"], writes=[ok])
                        p.op("pe", lambda e, h=h, ob=ob, n=n: e.matmul(ob, lhsT=qbT[:, h, :], rhs=Sbprev[:, n, h * 256:(h + 1) * 256],
                                                                       start=False, stop=True),
                             reads=["qbT", f"Sbprev{n}"], writes=[ok])
                    kv_update(b, kdf, Sf, 0, "Sf")
                    p.op("act", lambda e: e.copy(out=Sf16, in_=Sf), reads=["Sf"], writes=["Sf16"])
                    for h in range(4):
                        ob = pbO[h // 2][:, (h % 2) * 256:(h % 2) * 256 + 256]
                        ok = f"pb{1 + h // 2}"
                        p.op("dve", lambda e, h=h, ob=ob: e.bn_stats(out=rstat[:, h, :], in_=ob), reads=[ok], writes=["rstat"])
                    for h in range(4):
                        p.op("dve", lambda e, h=h: e.bn_aggr(out=rmv[:, h, :], in_=rstat[:, h, :]), reads=["rstat"], writes=["rmv"])
                    p.op("dve", lambda e: e.tensor_scalar_add(out=rr, in0=rmv[:, :, 1], scalar1=GN_EPS), reads=["rmv"], writes=["rr"])
                    p.op("act", lambda e: e.sqrt(out=rr, in_=rr), reads=["rr"], writes=["rr"])
                    p.op("dve", lambda e: e.reciprocal(out=rr, in_=rr), reads=["rr"], writes=["rr"])
                    for h in range(4):
                        ob = pbO[h // 2][:, (h % 2) * 256:(h % 2) * 256 + 256]
                        ok = f"pb{1 + h // 2}"
                        p.op("dve", lambda e, h=h, ob=ob: e.tensor_scalar(out=yn[:, h * 256:(h + 1) * 256], in0=ob, scalar1=rmv[:, h, 0:1],
                                                                          scalar2=rr[:, h:h + 1], op0=ALU.subtract, op1=ALU.mult),
                             reads=[ok, "rmv", "rr"], writes=["yn"])
                    p.op("act", lambda e: e.activation(out=sg, in_=gt[b], func=AF.Silu), reads=[f"gt{b}"], writes=["sg"])
                    p.op("dve", lambda e: e.tensor_tensor(out=yn, in0=yn, in1=gnw, op=ALU.mult), reads=["yn", "gnw"], writes=["yn"])
                    p.op("dve", lambda e: e.tensor_tensor(out=y16, in0=yn, in1=sg, op=ALU.mult), reads=["yn", "sg"], writes=["y16"])
                    for k in range(8):
                        p.op("pe", lambda e, k=k: e.transpose(ptr[1][:, k * 128:(k + 1) * 128], y16[:, k * 128:(k + 1) * 128], ident[:]),
                             reads=["y16", "ident"], writes=["ptr1"])
                    p.op("act", lambda e: e.copy(out=yT, in_=ptr[1][:, :].rearrange("q (k n) -> q k n", k=8)), reads=["ptr1"], writes=["yT"])
                    p.dma("sp", lambda e, tok=tok: e.dma_start(out=YRETT.ap()[:, tok:tok + 128].rearrange("(k q) t -> q k t", q=128), in_=yT),
                          reads=["yT"], writes=["YRETT"])

            def segment(tok0, nchunks, use_rope):
                for n in range(nchunks - 1, -1, -1):
                    pass1_chunk(n, n % 2, tok0 + n * 128, use_rope)
                for n in range(nchunks):
                    pass2_chunk(n, n % 2, tok0 + n * 128, use_rope)

            segment(SEQ, 2, False)
            segment(0, 32, True)


        YNAT = scratch("YNAT", [512, NT])

        def nattn(l, with_ctx_q):
            areset()
            qT = alloc([NT], BF16)
            kT = alloc([NT], BF16)
            va_aug = alloc([NTILE, 8, 65], BF16)
            y_tm = alloc([NTILE, 512], BF16)
            tmpv = alloc([17, 512], BF16)
            nabt = alloc([3200], F32)
            maskt = alloc([3200], F32)
            emb = alloc([5, 5, 128], BF16)
            PTs = [alloc([7, 128], BF16) for _ in range(2)]
            rec = alloc([4], F32)
            nstg = alloc([4, 128], BF16)
            p.dma("sp", lambda e: e.dma_start(out=maskt, in_=na_mask.ap()), writes=["maskt"])
            p.op("pool", lambda e: e.memset(va_aug[:, :, :, 64:65], 1.0), writes=["va_ones"])
            for half in range(2):
                p.dma("sp", lambda e, half=half: e.dma_start(
                    out=tmpv, in_=VA.ap()[half * 17 * 128:(half + 1) * 17 * 128, :].rearrange("(t q) d -> q t d", q=128)),
                    reads=["VA"], writes=["tmpv"])
                p.op("dve", lambda e, half=half: e.tensor_copy(
                    out=va_aug[:, half * 17:(half + 1) * 17, :, 0:64], in_=tmpv.rearrange("q t (h d) -> q t h d", h=8)),
                    reads=["tmpv"], writes=[f"va{half}"])
            slot_rr = [0]

            def one(h, tq, keytiles, cls, off):
                bi = tq % 2
                big = pbig[bi][:, :].rearrange("q (j n) -> q j n", j=8)
                PT = PTs[bi]
                nk = len(keytiles)
                for j, ktile in enumerate(keytiles):
                    p.op("pe", lambda e, j=j, ktile=ktile: e.matmul(
                        big[:, j, :], lhsT=kT[off:off + 64, ktile * 128:(ktile + 1) * 128],
                        rhs=qT[off:off + 64, tq * 128:(tq + 1) * 128], start=True, stop=True),
                        reads=["qT", "kT"], writes=[f"pbig{bi}"])
                n0 = min(nk, 4)
                p.op("act", lambda e: e.activation(out=PT[:, 0:n0, :], in_=big[:, 0:n0, :], func=AF.Exp, scale=0.125),
                     reads=[f"pbig{bi}"], writes=[f"PT{bi}"])
                if nk > 4:
                    p.op("act", lambda e: e.activation(out=PT[:, 4:nk, :], in_=big[:, 4:nk, :], func=AF.Exp, scale=0.125),
                         reads=[f"pbig{bi}"], writes=[f"PT{bi}"])
                if cls is not None:
                    p.op("dve", lambda e: e.tensor_tensor(out=PT[:, 0:5, :], in0=PT[:, 0:5, :], in1=emb[:, cls, :, :], op=ALU.mult),
                         reads=[f"PT{bi}", "emb"], writes=[f"PT{bi}"])
                slot_rr[0] = (slot_rr[0] + 1) % 8
                sl = slot_rr[0]
                po = pb45[sl // 4][:, (sl % 4) * 128:(sl % 4) * 128 + 65]
                pk = f"po{sl}"
                for j, ktile in enumerate(keytiles):
                    p.op("pe", lambda e, j=j, ktile=ktile: e.matmul(
                        po, lhsT=PT[:, j, :], rhs=va_aug[:, ktile, h, :], start=(j == 0), stop=(j == nk - 1)),
                        reads=[f"PT{bi}", "va0", "va1", "va_ones"], writes=[pk])
                rc = rec[:, sl % 4:sl % 4 + 1]
                p.op("dve", lambda e: e.reciprocal(out=rc, in_=po[:, 64:65]), reads=[pk], writes=[f"rec{sl % 4}"])
                p.op("dve", lambda e: e.tensor_scalar_mul(out=y_tm[:, tq, h * 64:(h + 1) * 64], in0=po[:, 0:64], scalar1=rc),
                     reads=[pk, f"rec{sl % 4}"], writes=[f"ytm{tq}"])

            for h in range(8):
                pair, off = h // 2, (h % 2) * 64
                if h % 2 == 0:
                    p.dma("sp", lambda e, pair=pair: e.dma_start(out=qT, in_=QAT.ap()[pair * 128:(pair + 1) * 128, :]),
                          reads=["QAT"], writes=["qT"])
                    p.dma("sp", lambda e, pair=pair: e.dma_start(out=kT, in_=KAT.ap()[pair * 128:(pair + 1) * 128, :]),
                          reads=["KAT"], writes=["kT"])
                p.dma("sp", lambda e, h=h: e.dma_start(out=nabt, in_=na_bias.ap()[l, h, :, :]), writes=["nabt"])
                p.op("act", lambda e: e.activation(out=nabt, in_=nabt, func=AF.Exp), reads=["nabt"], writes=["nabt"])
                p.op("dve", lambda e: e.tensor_tensor(out=emb.rearrange("q c j n -> q (c j n)"), in0=nabt, in1=maskt, op=ALU.mult),
                     reads=["nabt", "maskt"], writes=["emb"])
                for tq in range(32):
                    cls = {0: 0, 1: 1, 30: 3, 31: 4}.get(tq, 2)
                    k0 = min(max(tq - 2, 0), 27)
                    one(h, tq, [k0 + j for j in range(5)] + [32, 33], cls, off)
                if with_ctx_q:
                    for tq in (32, 33):
                        one(h, tq, [32, 33], None, off)
            for t in range(NTILE if with_ctx_q else 32):
                for k in range(4):
                    p.op("pe", lambda e, k=k, t=t: e.transpose(ptr[t % 2][:, k * 128:(k + 1) * 128], y_tm[:, t, k * 128:(k + 1) * 128], ident[:]),
                         reads=[f"ytm{t}", "ident"], writes=[f"ptr{t % 2}"])
                copy_any(nstg, ptr[t % 2][:, 0:512].rearrange("q (k n) -> q k n", k=4), [f"ptr{t % 2}"], ["nstg"])
                p.dma("sp", lambda e, t=t: e.dma_start(out=YNAT.ap()[:, t * 128:(t + 1) * 128].rearrange("(k q) n -> q k n", q=128), in_=nstg),
                      reads=["nstg"], writes=["YNAT"])


        def merge(l, ntiles):
            areset()
            wna = alloc([4, 1024], BF16)
            wfn = alloc([4, 1024], BF16)
            wret = alloc([8, 1024], BF16)
            wout = alloc([8, 1024], BF16)
            lnw = alloc([1024], F32)
            lnb = alloc([1024], F32)
            glt = [alloc([3072], BF16) for _ in range(2)]
            gates = alloc([3072], F32)
            ynT = [alloc([4, 128], BF16) for _ in range(2)]
            yfT = [alloc([4, 128], BF16) for _ in range(2)]
            yrT = [alloc([8, 128], BF16) for _ in range(2)]
            xt = [alloc([1024], F32) for _ in range(2)]
            ysum = alloc([1024], F32)
            ytmp = alloc([1024], F32)
            y16 = alloc([1024], BF16)
            ysT = alloc([8, 128], BF16)
            xo = alloc([1024], F32)
            stats = alloc([2, 6], F32)
            mv = alloc([2], F32)
            rstd = alloc([1], F32)
            p.dma("pool", lambda e: e.dma_start(out=wna, in_=w_o_na.ap()[l].rearrange("(c q) n -> q c n", q=128)), writes=["wna"])
            p.dma("pool", lambda e: e.dma_start(out=wfn, in_=w_fourier.ap()[l].rearrange("(c q) n -> q c n", q=128)), writes=["wfn"])
            p.dma("pool", lambda e: e.dma_start(out=wret, in_=w_o_ret.ap()[l].rearrange("(c q) n -> q c n", q=128)), writes=["wret"])
            p.dma("pool", lambda e: e.dma_start(out=wout, in_=w_out.ap()[l].rearrange("(c q) n -> q c n", q=128)), writes=["wout"])
            p.dma("sp", lambda e: e.dma_start(out=lnw, in_=ln_mix_w.ap()[l, :].partition_broadcast(128)), writes=["lnw"])
            p.dma("sp", lambda e: e.dma_start(out=lnb, in_=ln_mix_b.ap()[l, :].partition_broadcast(128)), writes=["lnb"])

            def tile_fn(t, b, j):
                tok = t * 128
                p.dma("sp", lambda e: e.dma_start(out=glt[b], in_=GL.ap()[tok:tok + 128, :]), reads=["GL"], writes=[f"glt{b}"])
                p.dma("sp", lambda e: e.dma_start(out=ynT[b], in_=YNAT.ap()[:, tok:tok + 128].rearrange("(k q) n -> q k n", q=128)),
                      reads=["YNAT"], writes=[f"ynT{b}"])
                p.dma("sp", lambda e: e.dma_start(out=yfT[b], in_=YFNT.ap()[:, tok:tok + 128].rearrange("(k q) n -> q k n", q=128)),
                      reads=["YFNT"], writes=[f"yfT{b}"])
                p.dma("sp", lambda e: e.dma_start(out=yrT[b], in_=YRETT.ap()[:, tok:tok + 128].rearrange("(k q) n -> q k n", q=128)),
                      reads=["YRETT"], writes=[f"yrT{b}"])
                p.dma("sp", lambda e: e.dma_start(out=xt[b], in_=XRES.ap()[tok:tok + 128, :]), reads=["xres"], writes=[f"mxt{b}"])
                p.op("act", lambda e: e.activation(out=gates, in_=glt[b], func=AF.Sigmoid), reads=[f"glt{b}"], writes=["gates"])
                branches = [(ynT[b], wna, 4, f"ynT{b}", "wna"), (yfT[b], wfn, 4, f"yfT{b}", "wfn"), (yrT[b], wret, 8, f"yrT{b}", "wret")]
                for nb in range(2):
                    cs = slice(nb * 512, (nb + 1) * 512)
                    for bi, (yT, w, nk, yk, wk) in enumerate(branches):
                        bank, bkey = next_bank()
                        for k in range(nk):
                            p.op("pe", lambda e, k=k, yT=yT, w=w, bank=bank, nk=nk, cs=cs: e.matmul(
                                bank, lhsT=yT[:, k, :], rhs=w[:, k, cs], start=(k == 0), stop=(k == nk - 1)),
                                reads=[yk, wk], writes=[bkey])
                        gsl = gates[:, bi * 1024 + nb * 512: bi * 1024 + (nb + 1) * 512]
                        if bi == 0:
                            p.op("dve", lambda e, bank=bank, gsl=gsl, cs=cs: e.tensor_tensor(out=ysum[:, cs], in0=bank, in1=gsl, op=ALU.mult),
                                 reads=[bkey, "gates"], writes=[f"ysum{nb}"])
                        else:
                            p.op("dve", lambda e, bank=bank, gsl=gsl, cs=cs: e.tensor_tensor(out=ytmp[:, cs], in0=bank, in1=gsl, op=ALU.mult),
                                 reads=[bkey, "gates"], writes=[f"ytmp{nb}"])
                            dst = y16 if bi == 2 else ysum
                            p.op("dve", lambda e, dst=dst, cs=cs: e.tensor_tensor(out=dst[:, cs], in0=ysum[:, cs], in1=ytmp[:, cs], op=ALU.add),
                                 reads=[f"ysum{nb}", f"ytmp{nb}"], writes=[f"ysum{nb}", f"y16{nb}"])
                for k in range(8):
                    p.op("pe", lambda e, k=k: e.transpose(ptr[b][:, k * 128:(k + 1) * 128], y16[:, k * 128:(k + 1) * 128], ident[:]),
                         reads=["y160", "y161", "ident"], writes=[f"ptr{b}"])
                copy_any(ysT, ptr[b][:, :].rearrange("q (k n) -> q k n", k=8), [f"ptr{b}"], ["ysT"])
                for nb in range(2):
                    cs = slice(nb * 512, (nb + 1) * 512)
                    bank, bkey = next_bank()
                    for k in range(8):
                        p.op("pe", lambda e, k=k, bank=bank, cs=cs: e.matmul(bank, lhsT=ysT[:, k, :], rhs=wout[:, k, cs], start=(k == 0), stop=(k == 7)),
                             reads=["ysT", "wout"], writes=[bkey])
                    p.op("dve", lambda e, bank=bank, cs=cs, nb=nb: e.tensor_tensor(out=xo[:, cs], in0=bank, in1=mods[:, j, 2 * D + nb * 512:2 * D + (nb + 1) * 512],
                                                                     op=ALU.mult), reads=[bkey, "mods"], writes=["xo"])
                p.op("dve", lambda e: e.scalar_tensor_tensor(out=xo, in0=xt[b], scalar=DN_ALPHA, in1=xo, op0=ALU.mult, op1=ALU.add),
                     reads=[f"mxt{b}", "xo"], writes=["xo"])
                ln_tile(xo, stats, mv, rstd, "xo", "mst")
                p.op("dve", lambda e: e.tensor_tensor(out=xo, in0=xo, in1=lnw, op=ALU.mult), reads=["xo", "lnw"], writes=["xo"])
                p.op("dve", lambda e: e.tensor_tensor(out=xo, in0=xo, in1=lnb, op=ALU.add), reads=["xo", "lnb"], writes=["xo"])
                p.dma("sp", lambda e: e.dma_start(out=XRES.ap()[tok:tok + 128, :], in_=xo), reads=["xo"], writes=["xres_w"])

            for t in range(ntiles):
                tile_fn(t, t % 2, 0 if t < 32 else 1)


        NPAIR = 392
        NBLK = 2 * NPAIR
        NROW = 896
        NSLOT = NROW * 128
        H16 = scratch("H16", [NT + 1, D])
        SHO = scratch("SHO", [NT, D])
        TBL = scratch("TBL", [NSLOT, 1], F32)
        OUTS = scratch("OUTS", [NSLOT, D])
        ident_f = sb("ident_f", [128, 128], F32)
        p.op("pool", lambda e: e.memset(ident_f[:], 1.0), writes=["ident_f"])
        p.op("pool", lambda e: e.affine_select(out=ident_f[:], in_=ident_f[:], pattern=[[-1, 128]],
                                              compare_op=ALU.is_equal, fill=0.0, base=0, channel_multiplier=1),
             reads=["ident_f"], writes=["ident_f"])

        def moe(l, ntiles):
            areset()
            rw = alloc([8, 256], F32)
            rb = alloc([256], F32)
            shgu = alloc([8, 512], BF16)
            shd = alloc([2, 1024], BF16)
            triU = alloc([128], BF16)
            ones16 = alloc([128], BF16)
            onesr = alloc([256], F32)
            cum = alloc([256], F32)
            eidi = alloc([256], I32)
            eidx = alloc([256], F32)
            qci = alloc([1], I32)
            qcf = alloc([1], F32)
            toki = alloc([NTILE], I32)
            tokf = alloc([NTILE], F32)
            W8 = alloc([NTILE, 8], F32)
            E8 = alloc([NTILE, 8], F32)
            R8 = alloc([NTILE, 8], F32)
            D8 = alloc([NTILE, 8], I32)
            BE = alloc([NPAIR], F32)
            WIDX = alloc([NPAIR], I32)
            persist_mark = None
            padded = alloc([256], F32)
            pad_end = alloc([256], F32)
            pad_start = alloc([256], F32)
            m1_mark = aoff[0]
            xt = [alloc([1024], F32) for _ in range(2)]
            h16 = alloc([1024], BF16)
            hT16 = alloc([8, 128], BF16)
            hT32 = alloc([8, 128], F32)
            hm16 = alloc([256], BF16)
            sgt = alloc([256], F32)
            hmT = alloc([2, 128], BF16)
            sho16 = alloc([1024], BF16)
            sc = alloc([256], F32)
            sel = alloc([256], F32)
            selm = alloc([256], F32)
            m8 = alloc([8, 8], F32)
            gs = alloc([8], F32)
            g8 = alloc([8], F32)
            pen = alloc([8], F32)
            v8 = alloc([8], F32)
            A = alloc([256], F32)
            A16 = alloc([256], BF16)
            rankd = alloc([256], F32)
            junk = alloc([256], F32)
            d8f = alloc([8], F32)
            w8 = alloc([8], F32)
            wsum = alloc([1], F32)
            stats = alloc([2, 6], F32)
            mv = alloc([2], F32)
            rstd = alloc([1], F32)

            p.dma("sp", lambda e: e.dma_start(out=rw, in_=router_w.ap()[l].rearrange("(c q) n -> q c n", q=128)), writes=["rw"])
            p.dma("sp", lambda e: e.dma_start(out=rb, in_=router_bias.ap()[l, :].partition_broadcast(128)), writes=["rb"])
            p.dma("pool", lambda e: e.dma_start(out=shgu[:, :, 0:256], in_=sh_w_gate.ap()[l].rearrange("(c q) n -> q c n", q=128)), writes=["shgu_a"])
            p.dma("pool", lambda e: e.dma_start(out=shgu[:, :, 256:512], in_=sh_w_up.ap()[l].rearrange("(c q) n -> q c n", q=128)), writes=["shgu_b"])
            p.dma("pool", lambda e: e.dma_start(out=shd, in_=sh_w_down.ap()[l].rearrange("(c q) n -> q c n", q=128)), writes=["shd"])
            p.op("pool", lambda e: e.memset(triU, 1.0), writes=["triU"])
            p.op("pool", lambda e: e.affine_select(out=triU, in_=triU, pattern=[[1, 128]], compare_op=ALU.is_gt, fill=0.0,
                                                  base=0, channel_multiplier=-1), reads=["triU"], writes=["triU"])
            p.op("pool", lambda e: e.memset(ones16, 1.0), writes=["ones16"])
            p.op("pool", lambda e: e.memset(onesr, 1.0), writes=["onesr"])
            p.op("pool", lambda e: e.memset(cum, 0.0), writes=["cum"])
            p.op("pool", lambda e: e.iota(eidi, pattern=[[1, 256]], base=0, channel_multiplier=0), writes=["eidi"])
            p.op("dve", lambda e: e.tensor_copy(out=eidx, in_=eidi), reads=["eidi"], writes=["eidx"])
            p.op("pool", lambda e: e.iota(qci, pattern=[[0, 1]], base=0, channel_multiplier=1), writes=["qci"])
            p.op("dve", lambda e: e.tensor_copy(out=qcf, in_=qci), reads=["qci"], writes=["qcf"])
            p.op("pool", lambda e: e.iota(toki, pattern=[[128, NTILE]], base=0, channel_multiplier=1), writes=["toki"])
            p.op("dve", lambda e: e.tensor_copy(out=tokf, in_=toki), reads=["toki"], writes=["tokf"])
            tfill = xt[1][:, 0:NROW]
            zrow = sho16
            p.op("pool", lambda e: e.memset(tfill, float(NT)), writes=["xt1"])
            p.op("pool", lambda e: e.memset(zrow, 0.0), writes=["sho16"])
            p.dma("sp", lambda e: e.dma_start(out=TBL.ap().rearrange("(q f) o -> q (f o)", q=128), in_=tfill), reads=["xt1"], writes=["TBL"])
            p.dma("sp", lambda e: e.dma_start(out=H16.ap()[NT:NT + 1, :], in_=zrow[0:1, :]), reads=["sho16"], writes=["H16"])

            def m1_tile(t, b, j):
                tok = t * 128
                p.dma("sp", lambda e: e.dma_start(out=xt[b], in_=XRES.ap()[tok:tok + 128, :]), reads=["xres", "xres_w"], writes=[f"xt{b}"])
                ln_tile(xt[b], stats, mv, rstd, f"xt{b}", "st")
                p.op("dve", lambda e: e.tensor_tensor(out=xt[b], in0=xt[b], in1=mods[:, j, 4 * D:5 * D], op=ALU.mult),
                     reads=[f"xt{b}", "mods"], writes=[f"xt{b}"])
                p.op("dve", lambda e: e.tensor_tensor(out=xt[b], in0=xt[b], in1=mods[:, j, 3 * D:4 * D], op=ALU.add),
                     reads=[f"xt{b}", "mods"], writes=[f"xt{b}"])
                p.op("act", lambda e: e.copy(out=h16, in_=xt[b]), reads=[f"xt{b}"], writes=["h16"])
                p.dma("sp", lambda e: e.dma_start(out=H16.ap()[tok:tok + 128, :], in_=h16), reads=["h16"], writes=["H16"])
                for k in range(8):
                    p.op("pe", lambda e, k=k: e.transpose(ptr[0][:, k * 128:(k + 1) * 128], h16[:, k * 128:(k + 1) * 128], ident[:]),
                         reads=["h16", "ident"], writes=["ptr0"])
                p.op("act", lambda e: e.copy(out=hT16, in_=ptr[0][:, :].rearrange("q (k n) -> q k n", k=8)), reads=["ptr0"], writes=["hT16"])
                for k in range(8):
                    p.op("pe", lambda e, k=k: e.matmul(pb45[0][:, :], lhsT=hT16[:, k, :], rhs=shgu[:, k, :], start=(k == 0), stop=(k == 7)),
                         reads=["hT16", "shgu_a", "shgu_b"], writes=["pb4"])
                p.op("act", lambda e: e.activation(out=sgt, in_=pb45[0][:, 0:256], func=AF.Silu), reads=["pb4"], writes=["sgt"])
                p.op("dve", lambda e: e.tensor_tensor(out=hm16, in0=sgt, in1=pb45[0][:, 256:512], op=ALU.mult), reads=["sgt", "pb4"], writes=["hm16"])
                for k in range(2):
                    p.op("pe", lambda e, k=k: e.transpose(ptr[1][:, k * 128:(k + 1) * 128], hm16[:, k * 128:(k + 1) * 128], ident[:]),
                         reads=["hm16", "ident"], writes=["ptr1"])
                p.op("act", lambda e: e.copy(out=hmT, in_=ptr[1][:, 0:256].rearrange("q (k n) -> q k n", k=2)), reads=["ptr1"], writes=["hmT"])
                for nb in range(2):
                    bank = pbank[2 + nb]
                    for k in range(2):
                        p.op("pe", lambda e, k=k, nb=nb, bank=bank: e.matmul(bank, lhsT=hmT[:, k, :], rhs=shd[:, k, nb * 512:(nb + 1) * 512],
                                                                             start=(k == 0), stop=(k == 1)), reads=["hmT", "shd"], writes=[f"pb{2 + nb}"])
                    copy_any(sho16[:, nb * 512:(nb + 1) * 512], bank, [f"pb{2 + nb}"], ["sho16"])
                p.dma("sp", lambda e: e.dma_start(out=SHO.ap()[tok:tok + 128, :], in_=sho16), reads=["sho16"], writes=["SHO"])
                for k in range(8):
                    p.op("pe", lambda e, k=k: e.transpose(pbig[0][:, k * 128:(k + 1) * 128], xt[b][:, k * 128:(k + 1) * 128], ident_f[:]),
                         reads=[f"xt{b}", "ident_f"], writes=["pb0", "pb1"])
                p.op("dve", lambda e: e.tensor_copy(out=hT32, in_=pbig[0][:, :].rearrange("q (k n) -> q k n", k=8)), reads=["pb0", "pb1"], writes=["hT32"])
                for k in range(8):
                    p.op("pe", lambda e, k=k: e.matmul(pb45[1][:, 0:256], lhsT=hT32[:, k, :], rhs=rw[:, k, :], start=(k == 0), stop=(k == 7)),
                         reads=["hT32", "rw"], writes=["pb5"])
                p.op("act", lambda e: e.activation(out=sc, in_=pb45[1][:, 0:256], func=AF.Sigmoid), reads=["pb5"], writes=["sc"])
                p.op("dve", lambda e: e.tensor_tensor(out=sel, in0=sc, in1=rb, op=ALU.add), reads=["sc", "rb"], writes=["sel"])
                for g in range(8):
                    p.op("dve", lambda e, g=g: e.max(out=m8[:, g, :], in_=sel[:, g * 32:(g + 1) * 32]), reads=["sel"], writes=["m8"])
                p.op("dve", lambda e: e.tensor_tensor(out=gs, in0=m8[:, :, 0], in1=m8[:, :, 1], op=ALU.add), reads=["m8"], writes=["gs"])
                p.op("dve", lambda e: e.max(out=g8, in_=gs), reads=["gs"], writes=["g8"])
                p.op("dve", lambda e: e.tensor_scalar(out=pen, in0=gs, scalar1=g8[:, 3:4], scalar2=None, op0=ALU.is_ge), reads=["gs", "g8"], writes=["pen"])
                p.op("dve", lambda e: e.tensor_scalar(out=pen, in0=pen, scalar1=1.0, scalar2=1.0e4, op0=ALU.subtract, op1=ALU.mult),
                     reads=["pen"], writes=["pen"])
                p.op("dve", lambda e: e.tensor_tensor(out=selm.rearrange("q (g x) -> q g x", g=8), in0=sel.rearrange("q (g x) -> q g x", g=8),
                                                      in1=pen.unsqueeze(2).broadcast_to([128, 8, 32]), op=ALU.add), reads=["sel", "pen"], writes=["selm"])
                p.op("dve", lambda e: e.max(out=v8, in_=selm), reads=["selm"], writes=["v8"])
                p.op("dve", lambda e: e.tensor_scalar(out=A, in0=selm, scalar1=v8[:, 7:8], scalar2=None, op0=ALU.is_ge), reads=["selm", "v8"], writes=["A"])
                p.op("dve", lambda e: e.tensor_copy(out=A16, in_=A), reads=["A"], writes=["A16"])
                p.op("pe", lambda e: e.matmul(pbank[2][:, 0:256], lhsT=triU, rhs=A16, start=True, stop=True), reads=["triU", "A16"], writes=["pb2"])
                p.op("pe", lambda e: e.matmul(pbank[3][:, 0:256], lhsT=ones16, rhs=A16, start=True, stop=True), reads=["ones16", "A16"], writes=["pb3"])
                p.op("dve", lambda e: e.tensor_tensor(out=rankd, in0=pbank[2][:, 0:256], in1=cum, op=ALU.add), reads=["pb2", "cum"], writes=["rankd"])
                p.op("dve", lambda e: e.tensor_tensor(out=cum, in0=pbank[3][:, 0:256], in1=cum, op=ALU.add), reads=["pb3", "cum"], writes=["cum"])
                for k in range(8):
                    for (src, dstT, key) in ((rankd, R8, "R8"), (sc, w8.unsqueeze(1), "w8"), (eidx, E8, "E8")):
                        oap = dstT[:, t, k:k + 1] if key != "w8" else w8[:, k:k + 1]
                        p.op("dve", lambda e, k=k, src=src, oap=oap: e.scalar_tensor_tensor(
                            out=junk, in0=selm, scalar=v8[:, k:k + 1], in1=src, op0=ALU.is_equal, op1=ALU.mult, accum_out=oap),
                            reads=["selm", "v8", "rankd", "sc", "eidx"], writes=["junk", f"{key}_{t}"])
                p.op("dve", lambda e: e.reduce_sum(out=wsum, in_=w8, axis=AX.X), reads=[f"w8_{t}"], writes=["wsum"])
                p.op("dve", lambda e: e.reciprocal(out=wsum, in_=wsum), reads=["wsum"], writes=["wsum"])
                p.op("dve", lambda e: e.tensor_scalar(out=W8[:, t, :], in0=w8, scalar1=wsum[:, 0:1], scalar2=2.5, op0=ALU.mult, op1=ALU.mult),
                     reads=[f"w8_{t}", "wsum"], writes=[f"W8_{t}"])

            for t in range(ntiles):
                m1_tile(t, t % 2, 0 if t < 32 else 1)

            p.barrier()
            MAGIC_ = 12582912.0
            p.op("dve", lambda e: e.tensor_scalar(out=padded, in0=cum, scalar1=127.25, scalar2=1.0 / 256, op0=ALU.add, op1=ALU.mult),
                 reads=["cum"], writes=["padded"])
            p.op("dve", lambda e: e.tensor_scalar_add(out=padded, in0=padded, scalar1=MAGIC_), reads=["padded"], writes=["padded"])
            p.op("dve", lambda e: e.tensor_scalar(out=padded, in0=padded, scalar1=MAGIC_, scalar2=256.0, op0=ALU.subtract, op1=ALU.mult),
                 reads=["padded"], writes=["padded"])
            p.op("dve", lambda e: e.tensor_tensor_scan(out=pad_end, data0=onesr, data1=padded, initial=0.0, op0=ALU.mult, op1=ALU.add),
                 reads=["padded", "onesr"], writes=["pad_end"])
            p.op("dve", lambda e: e.tensor_tensor(out=pad_start, in0=pad_end, in1=padded, op=ALU.subtract), reads=["pad_end", "padded"], writes=["pad_start"])
            for bq in range(NPAIR):
                p.op("dve", lambda e, bq=bq: e.scalar_tensor_tensor(out=junk, in0=pad_end, scalar=float(256 * bq), in1=onesr, op0=ALU.is_le,
                                                                    op1=ALU.mult, accum_out=BE[:, bq:bq + 1]),
                     reads=["pad_end", "onesr"], writes=["junk", "BE"])
            p.op("dve", lambda e: e.tensor_scalar(out=BE, in0=BE, scalar1=255.0, scalar2=128.0, op0=ALU.min, op1=ALU.mult), reads=["BE"], writes=["BE"])
            p.op("dve", lambda e: e.tensor_scalar_add(out=BE, in0=BE, scalar1=qcf[:, 0:1]), reads=["BE", "qcf"], writes=["BE"])
            p.op("dve", lambda e: e.tensor_copy(out=WIDX, in_=BE), reads=["BE"], writes=["WIDX"])

            def m1b_tile(t):
                for k in range(8):
                    p.op("dve", lambda e, k=k: e.scalar_tensor_tensor(
                        out=junk, in0=eidx, scalar=E8[:, t, k:k + 1], in1=pad_start, op0=ALU.is_equal, op1=ALU.mult, accum_out=d8f[:, k:k + 1]),
                        reads=["eidx", f"E8_{t}", "pad_start"], writes=["junk", "d8f"])
                p.op("dve", lambda e: e.tensor_tensor(out=d8f, in0=d8f, in1=R8[:, t, :], op=ALU.add), reads=["d8f", f"R8_{t}"], writes=["d8f"])
                p.op("dve", lambda e: e.tensor_copy(out=D8[:, t, :], in_=d8f), reads=["d8f"], writes=[f"D8_{t}"])
                for k in range(8):
                    p.dma("pool", lambda e, k=k: e.indirect_dma_start(
                        out=TBL.ap()[:, :], out_offset=bass.IndirectOffsetOnAxis(ap=D8[:, t, k:k + 1], axis=0),
                        in_=tokf[:, t:t + 1], in_offset=None), reads=[f"D8_{t}", "tokf", "TBL"], writes=["TBLs"])

            for t in range(ntiles):
                m1b_tile(t)

            p.barrier()
            aoff[0] = m1_mark
            tbl_sb = alloc([7, 128], F32)
            idxc = alloc([NROW], I32)
            wst = [alloc([6144], F32) for _ in range(2)]
            wbf = [alloc([6144], BF16) for _ in range(2)]
            wgu = [w_[:, 0:4096].rearrange("q (c n) -> q c n", c=8) for w_ in wbf]
            wd = [w_[:, 4096:6144].rearrange("q (c n) -> q c n", c=2) for w_ in wbf]
            xg = [alloc([1024], BF16) for _ in range(3)]
            XT = alloc([8, 128], BF16)
            ostg = [alloc([1024], BF16) for _ in range(2)]
            p.dma("sp", lambda e: e.dma_start(out=tbl_sb, in_=TBL.ap().rearrange("(c r q) o -> r c (q o)", c=7, r=128)),
                  reads=["TBL", "TBLs"], writes=["tbl_sb"])
            for c in range(7):
                p.op("pe", lambda e, c=c: e.transpose(pbig[0][:, c * 128:(c + 1) * 128], tbl_sb[:, c, :], ident_f[:]),
                     reads=["tbl_sb", "ident_f"], writes=["pb0", "pb1"])
            p.op("dve", lambda e: e.tensor_copy(out=idxc, in_=pbig[0][:, 0:NROW]), reads=["pb0", "pb1"], writes=["idxc"])

            def load_block_w(pp):
                off = bass.IndirectOffsetOnAxis(ap=WIDX[:, pp:pp + 1], axis=0)
                p.dma("pool", lambda e: e.indirect_dma_start(out=wst[pp % 2], out_offset=None, in_=exp_w_all[l].ap(), in_offset=off),
                      reads=["WIDX"], writes=[f"wst{pp % 2}"])

            def cast_w(pp):
                b2 = pp % 2
                p.op("act", lambda e: e.copy(out=wbf[b2][:, 0:3072], in_=wst[b2][:, 0:3072]), reads=[f"wst{b2}"], writes=[f"wbfa{b2}"])
                p.op("dve", lambda e: e.tensor_copy(out=wbf[b2][:, 3072:6144], in_=wst[b2][:, 3072:6144]), reads=[f"wst{b2}"], writes=[f"wbfb{b2}"])

            XTs = [alloc([8, 128], BF16) for _ in range(3)]
            sgts = [alloc([256], F32) for _ in range(2)]
            hm16s = [alloc([256], BF16) for _ in range(2)]
            hmTs = [alloc([2, 128], BF16) for _ in range(2)]

            def stage1(bq):
                b3 = bq % 3
                p.dma("pool", lambda e: e.indirect_dma_start(
                    out=xg[b3], out_offset=None, in_=H16.ap()[:, :],
                    in_offset=bass.IndirectOffsetOnAxis(ap=idxc[:, bq:bq + 1], axis=0)),
                    reads=["idxc", "H16"], writes=[f"xg{b3}"])
                for k in range(8):
                    p.op("pe", lambda e, k=k: e.transpose(ptr[0][:, k * 128:(k + 1) * 128], xg[b3][:, k * 128:(k + 1) * 128], ident[:]),
                         reads=[f"xg{b3}", "ident"], writes=["ptr0"])
                copy_any(XTs[b3], ptr[0][:, :].rearrange("q (k n) -> q k n", k=8), ["ptr0"], [f"XT{b3}"])

            def stage2(bq):
                b3 = bq % 3
                w3 = (bq // 2) % 2
                b2 = bq % 2
                gb = pb45[b2]
                gk = f"pb{4 + b2}"
                for k in range(8):
                    p.op("pe", lambda e, k=k: e.matmul(gb[:, :], lhsT=XTs[b3][:, k, :], rhs=wgu[w3][:, k, :], start=(k == 0), stop=(k == 7)),
                         reads=[f"XT{b3}", f"wbfa{w3}", f"wbfb{w3}"], writes=[gk])
                p.op("act", lambda e: e.activation(out=sgts[b2], in_=gb[:, 0:256], func=AF.Silu), reads=[gk], writes=[f"sgt{b2}"])
                p.op("dve", lambda e: e.tensor_tensor(out=hm16s[b2], in0=sgts[b2], in1=gb[:, 256:512], op=ALU.mult),
                     reads=[f"sgt{b2}", gk], writes=[f"hm16{b2}"])

            def stage3(bq):
                b3 = (bq // 2) % 2
                b2 = bq % 2
                o2 = bq % 2
                for k in range(2):
                    p.op("pe", lambda e, k=k: e.transpose(ptr[1][:, k * 128:(k + 1) * 128], hm16s[b2][:, k * 128:(k + 1) * 128], ident[:]),
                         reads=[f"hm16{b2}", "ident"], writes=["ptr1"])
                p.op("act", lambda e: e.copy(out=hmTs[b2], in_=ptr[1][:, 0:256].rearrange("q (k n) -> q k n", k=2)), reads=["ptr1"], writes=[f"hmT{b2}"])
                for nb in range(2):
                    bank = pbank[2 * o2 + nb]
                    bkey = f"pb{2 * o2 + nb}"
                    for k in range(2):
                        p.op("pe", lambda e, k=k, nb=nb, bank=bank: e.matmul(bank, lhsT=hmTs[b2][:, k, :], rhs=wd[b3][:, k, nb * 512:(nb + 1) * 512],
                                                                             start=(k == 0), stop=(k == 1)), reads=[f"hmT{b2}", f"wbfb{b3}"], writes=[bkey])
                    copy_any(ostg[o2][:, nb * 512:(nb + 1) * 512], bank, [bkey], [f"ostg{o2}"])
                r0 = bq * 128
                p.dma("sp", lambda e: e.dma_start(out=OUTS.ap()[r0:r0 + 128, :], in_=ostg[o2]), reads=[f"ostg{o2}"], writes=["OUTS"])

            NB_ = cfg.get("nblk", NBLK)
            NP_ = (NB_ + 1) // 2
            load_block_w(0)
            load_block_w(1)
            cast_w(0)
            stage1(0)
            stage1(1)
            stage2(0)
            for bq in range(NB_):
                if bq % 2 == 0:
                    pp = bq // 2
                    if pp + 1 < NP_:
                        cast_w(pp + 1)
                    if pp + 2 < NP_:
                        load_block_w(pp + 2)
                if bq + 2 < NB_:
                    stage1(bq + 2)
                if bq + 1 < NB_:
                    stage2(bq + 1)
                stage3(bq)

            p.barrier()
            aoff[0] = m1_mark
            xt = [alloc([1024], F32) for _ in range(2)]
            stats = alloc([2, 6], F32)
            mv = alloc([2], F32)
            rstd = alloc([1], F32)
            og = [alloc([1024], BF16) for _ in range(3)]
            acc = alloc([1024], F32)
            lnw = alloc([1024], F32)
            lnb = alloc([1024], F32)
            sh_in = alloc([1024], BF16)
            xo = alloc([1024], F32)
            p.dma("sp", lambda e: e.dma_start(out=lnw, in_=ln_ffn_w.ap()[l, :].partition_broadcast(128)), writes=["lnw"])
            p.dma("sp", lambda e: e.dma_start(out=lnb, in_=ln_ffn_b.ap()[l, :].partition_broadcast(128)), writes=["lnb"])
            gc = [0]

            def m3_tile(t, b, j):
                tok = t * 128
                p.dma("sp", lambda e: e.dma_start(out=sh_in, in_=SHO.ap()[tok:tok + 128, :]), reads=["SHO"], writes=["sh_in"])
                p.dma("sp", lambda e: e.dma_start(out=xt[b], in_=XRES.ap()[tok:tok + 128, :]), reads=["xres", "xres_w"], writes=[f"xt{b}"])
                p.op("dve", lambda e: e.tensor_copy(out=acc, in_=sh_in), reads=["sh_in"], writes=["acc"])
                for k in range(8):
                    g3 = gc[0] % 3
                    gc[0] += 1
                    p.dma("pool", lambda e, k=k, g3=g3: e.indirect_dma_start(
                        out=og[g3], out_offset=None, in_=OUTS.ap()[:, :],
                        in_offset=bass.IndirectOffsetOnAxis(ap=D8[:, t, k:k + 1], axis=0)),
                        reads=[f"D8_{t}", "OUTS"], writes=[f"og{g3}"])
                    p.op("dve", lambda e, k=k, g3=g3: e.scalar_tensor_tensor(out=acc, in0=og[g3], scalar=W8[:, t, k:k + 1], in1=acc,
                                                                             op0=ALU.mult, op1=ALU.add),
                         reads=[f"og{g3}", f"W8_{t}", "acc"], writes=["acc"])
                p.op("dve", lambda e: e.tensor_tensor(out=xo, in0=acc, in1=mods[:, j, 5 * D:6 * D], op=ALU.mult), reads=["acc", "mods"], writes=["xo"])
                p.op("dve", lambda e: e.scalar_tensor_tensor(out=xo, in0=xt[b], scalar=DN_ALPHA, in1=xo, op0=ALU.mult, op1=ALU.add),
                     reads=[f"xt{b}", "xo"], writes=["xo"])
                ln_tile(xo, stats, mv, rstd, "xo", "st")
                p.op("dve", lambda e: e.tensor_tensor(out=xo, in0=xo, in1=lnw, op=ALU.mult), reads=["xo", "lnw"], writes=["xo"])
                p.op("dve", lambda e: e.tensor_tensor(out=xo, in0=xo, in1=lnb, op=ALU.add), reads=["xo", "lnb"], writes=["xo"])
                p.dma("sp", lambda e: e.dma_start(out=XRES.ap()[tok:tok + 128, :], in_=xo), reads=["xo"], writes=["xres_w2"])

            for t in range(ntiles):
                m3_tile(t, t % 2, 0 if t < 32 else 1)

        p.dma("sp", lambda e: e.dma_start(out=XRES.ap()[0:SEQ, :], in_=x_in.ap()), writes=["xres"])
        p.dma("sp", lambda e: e.dma_start(out=XRES.ap()[SEQ:NT, :], in_=ctx_in.ap()), writes=["xres"])
        for l in range(cfg.get("layers", DEPTH)):
            phase0(l)
            if stages >= 1 and not cfg.get("moe_only"):
                phase1(l, XRES.ap())
            if stages >= 5 and not cfg.get("moe_only"):
                nattn(l, l < DEPTH - 1)
            if stages >= 4 and not cfg.get("noret") and not cfg.get("moe_only"):
                retention(l)
            if stages >= 3 and not cfg.get("nofourier") and not cfg.get("moe_only"):
                fourier(SEQ, 0)
                if l < DEPTH - 1:
                    fourier(CTX, SEQ)
            if stages >= 6 and not cfg.get("moe_only"):
                merge(l, NTILE if l < DEPTH - 1 else 32)
            if stages >= 7:
                moe(l, NTILE if l < DEPTH - 1 else 32)

        p.barrier()
        for i in range(4):
            p.dma("sp", lambda e, i=i: e.dma_start(out=out_t.ap()[i * 1024:(i + 1) * 1024, :],
                                                   in_=XRES.ap()[i * 1024:(i + 1) * 1024, :]),
                  reads=["xres"], writes=["out"])
        p.barrier()
        with nc.Block() as block:
            p.emit(block)
    return nc


_NC_CACHE = {}


def _rope_table():
    t = np.arange(SEQ)
    row, col = t // 64, t % 64
    inv = (10000.0 ** (-np.arange(32, dtype=np.float32) / 32)).astype(np.float32)
    ar = row.astype(np.float32)[:, None] * inv[None, :]
    ac = col.astype(np.float32)[:, None] * inv[None, :]
    C = np.concatenate([np.cos(ar), np.cos(ar), np.cos(ac), np.cos(ac)], axis=1)
    S = np.concatenate([np.sin(ar), np.sin(ar), np.sin(ac), np.sin(ac)], axis=1)
    return np.ascontiguousarray(np.concatenate([C, S], axis=1), dtype=np.float32)


def _na_tables():
    DR = np.zeros((5, 128, 5, 128), np.int64)
    DC = np.zeros((5, 128, 5, 128), np.int64)
    M = np.zeros((5, 128, 5, 128), np.float32)
    pp = np.arange(128)
    for cls, tq in enumerate((0, 1, 2, 30, 31)):
        k0 = min(max(tq - 2, 0), 27)
        for j in range(5):
            kt = k0 + j
            kr = (2 * kt + pp // 64)[:, None]
            kc = (pp % 64)[:, None]
            qr = (2 * tq + pp // 64)[None, :]
            qc = (pp % 64)[None, :]
            r0 = np.clip(qr - 4, 0, 56)
            c0 = np.clip(qc - 8, 0, 48)
            valid = (kr >= r0) & (kr < r0 + 8) & (kc >= c0) & (kc < c0 + 16)
            DR[cls, :, j, :] = np.clip(kr - qr + 7, 0, 14)
            DC[cls, :, j, :] = np.clip(kc - qc, -15, 15) + 15
            M[cls, :, j, :] = valid
    return DR, DC, M


def _na_bias_layout(rpb):
    DR, DC, M = _na_tables()
    g = rpb[:, :, DR, DC]
    g = np.transpose(g, (0, 1, 3, 2, 4, 5))
    L = rpb.shape[0]
    return (np.ascontiguousarray(g.reshape(L, 8, 128, 3200), dtype=np.float32),
            np.ascontiguousarray(np.transpose(M, (1, 0, 2, 3)).reshape(128, 3200), dtype=np.float32))


def _wlay(w, c):
    L, E, K, N = w.shape
    return np.ascontiguousarray(w.reshape(L, E, c, 128, N).transpose(0, 1, 3, 2, 4)).reshape(L, E * 128, c * N)


def _gu_layout(gate, up):
    L, E = gate.shape[0], gate.shape[1]
    gu = np.stack([gate.reshape(L, E, 8, 128, 256), up.reshape(L, E, 8, 128, 256)], axis=4)
    gu = gu.transpose(0, 1, 3, 2, 4, 5).reshape(L, E, 128, 2, 4 * 512)
    return np.ascontiguousarray(gu.transpose(0, 3, 1, 2, 4)).reshape(L, 2, E * 128, 2048)


def _run(inputs, batches, cfg=None):
    f32 = lambda a: np.ascontiguousarray(np.asarray(a), dtype=np.float32)
    g = {k: f32(v) for k, v in inputs.items()}
    key = str(sorted((cfg or {}).items()))
    if key not in _NC_CACHE:
        _NC_CACHE[key] = build_program(dict(cfg or {}))
    nc = _NC_CACHE[key]
    nab, nam = _na_bias_layout(g["na_rpb"])
    shared = {
        "ada_w": g["ada_w"], "ada_b": g["ada_b"], "w_in": g["w_in"],
        "ret_decay": np.ascontiguousarray(np.concatenate([g["ret_decay_fwd"], g["ret_decay_bwd"]], axis=1)),
        "ret_gn_w": g["ret_gn_w"], "rope": _rope_table(), "na_bias": nab, "na_mask": nam,
        "w_o_na": g["w_o_na"], "w_fourier": g["w_fourier"], "w_o_ret": g["w_o_ret"], "w_out": g["w_out"],
        "ln_mix_w": g["ln_mix_w"], "ln_mix_b": g["ln_mix_b"], "router_w": g["router_w"], "router_bias": g["router_bias"],
        "sh_w_gate": g["sh_w_gate"], "sh_w_up": g["sh_w_up"], "sh_w_down": g["sh_w_down"],
        "ln_ffn_w": g["ln_ffn_w"], "ln_ffn_b": g["ln_ffn_b"],
    }
    wl = _wlay(g["exp_w_down"], 2)
    gu = _gu_layout(g["exp_w_gate"], g["exp_w_up"])
    for i in range(gu.shape[0]):
        shared[f"exp_w_all{i}"] = np.ascontiguousarray(np.concatenate([gu[i, 0], gu[i, 1], wl[i]], axis=1))
    del wl, gu
    in_maps = []
    for b in batches:
        m = dict(shared)
        m["x"] = g["x"][b]
        m["ctx"] = g["ctx"][b]
        m["cvec"] = np.ascontiguousarray(np.stack([g["c"][b], g["c_ctx"]]))
        in_maps.append(m)
    res = run_bass_kernel_spmd(nc, in_maps, core_ids=list(range(len(batches))))
    return np.stack([np.asarray(res.results[i]["out"], dtype=np.float32) for i in range(len(batches))], axis=0)


def kernel(**inputs):
    return _run(inputs, list(range(8)))
```
